# Optimizing a Trainium2 kernel written in Bass

```python
import jax, jax.numpy as jnp
from jax import lax
import numpy as np

D_MODEL = 1024
BATCH = 4
SEQ = 4096
DEPTH = 1

ML_HEADS = 4
ML_DQK = 128
ML_DV = 256
CONV_K = 4
RET_HEADS = 4
RET_DQK = 128
RET_DV = 256
ROPE_BASE = 10000.0
CHUNK = 128
N_EXPERTS = 32
TOP_K = 4
D_EXPERT = D_MODEL
SWIGLU_LIMIT = 7.0
SWIGLU_ALPHA = 1.702
EXPERT_BLOCK = 256
EPS = 1e-5

ML_QK_W = ML_HEADS * ML_DQK
ML_V_W = ML_HEADS * ML_DV
RET_QK_W = RET_HEADS * RET_DQK
RET_V_W = RET_HEADS * RET_DV
IN_SIZES = (2 * ML_QK_W, ML_V_W, ML_V_W, 2 * ML_HEADS, RET_QK_W, RET_QK_W, RET_V_W, RET_V_W, D_MODEL, D_MODEL)
D_IN = sum(IN_SIZES)

kernel_name = "hybrid_mlstm_retention_moe_adaln"


def rmsnorm(x, g):
    xf = x.astype(jnp.float32)
    y = xf * lax.rsqrt(jnp.mean(xf * xf, axis=-1, keepdims=True) + EPS)
    return (y * g.astype(jnp.float32)).astype(x.dtype)


def head_layernorm(h, n_heads, g):
    B, S, W = h.shape
    hf = h.astype(jnp.float32).reshape(B, S, n_heads, W // n_heads)
    mu = jnp.mean(hf, axis=-1, keepdims=True)
    var = jnp.mean(jnp.square(hf - mu), axis=-1, keepdims=True)
    y = ((hf - mu) * lax.rsqrt(var + EPS)).reshape(B, S, W)
    return (y * g.astype(jnp.float32)).astype(h.dtype)


def causal_dwconv(x, w, b):
    C = x.shape[-1]
    y = lax.conv_general_dilated(x, w[:, None, :].astype(x.dtype), window_strides=(1,), padding=((CONV_K - 1, 0),),
                                 dimension_numbers=('NWC', 'WIO', 'NWC'), feature_group_count=C)
    return y + b.astype(x.dtype)


def rotary(x, positions):
    half = x.shape[-1] // 2
    inv = ROPE_BASE ** (-jnp.arange(half, dtype=jnp.float32) / half)
    ang = positions.astype(jnp.float32)[..., None] * inv
    cos = jnp.cos(ang)[:, :, None, :]
    sin = jnp.sin(ang)[:, :, None, :]
    xf = x.astype(jnp.float32)
    x1, x2 = xf[..., :half], xf[..., half:]
    return jnp.concatenate([x1 * cos - x2 * sin, x2 * cos + x1 * sin], axis=-1).astype(x.dtype)


def to_chunks(t, n_heads):
    B, S, _ = t.shape
    return t.reshape(B, S // CHUNK, CHUNK, n_heads, -1).transpose(0, 3, 1, 2, 4)


def gate_chunks(t):
    B, S, H = t.shape
    return t.reshape(B, S // CHUNK, CHUNK, H).transpose(0, 3, 1, 2)


def from_chunks(t):
    B, H, NC, L, d = t.shape
    return t.transpose(0, 2, 3, 1, 4).reshape(B, NC * L, H * d)


def mlstm_chunkwise(q, k, v, i_pre, f_pre):
    L = q.shape[-2]
    dk = q.shape[-1]
    q = q.astype(jnp.float32) * (dk ** -0.5)
    k = k.astype(jnp.float32)
    v = v.astype(jnp.float32)
    a = jnp.cumsum(jax.nn.log_sigmoid(f_pre), axis=-1)
    A = a[..., -1]
    causal = jnp.tril(jnp.ones((L, L), dtype=bool))
    d_log = jnp.where(causal, a[..., :, None] - a[..., None, :] + i_pre[..., None, :], -jnp.inf)
    m_intra = jnp.max(d_log, axis=-1)
    w_state = A[..., None] - a + i_pre
    m_loc = jnp.max(w_state, axis=-1)
    ws = jnp.exp(w_state - m_loc[..., None])
    c_loc = jnp.einsum('bhcl,bhclk,bhclv->bhckv', ws, k, v)
    n_loc = jnp.einsum('bhcl,bhclk->bhck', ws, k)

    def step(carry, inp):
        c_st, n_st, m_st = carry
        a_c, m_l, c_l, n_l = inp
        m_new = jnp.maximum(a_c + m_st, m_l)
        s_prev = jnp.exp(a_c + m_st - m_new)
        s_loc = jnp.exp(m_l - m_new)
        c_new = s_prev[..., None, None] * c_st + s_loc[..., None, None] * c_l
        n_new = s_prev[..., None] * n_st + s_loc[..., None] * n_l
        return (c_new, n_new, m_new), (c_st, n_st, m_st)

    B, H = q.shape[0], q.shape[1]
    init = (jnp.zeros((B, H, dk, v.shape[-1]), jnp.float32), jnp.zeros((B, H, dk), jnp.float32), jnp.zeros((B, H), jnp.float32))
    xs = (jnp.moveaxis(A, 2, 0), jnp.moveaxis(m_loc, 2, 0), jnp.moveaxis(c_loc, 2, 0), jnp.moveaxis(n_loc, 2, 0))
    _, (c_prev, n_prev, m_prev) = lax.scan(step, init, xs)
    c_prev = jnp.moveaxis(c_prev, 0, 2)
    n_prev = jnp.moveaxis(n_prev, 0, 2)
    m_prev = jnp.moveaxis(m_prev, 0, 2)

    inter_log = a + m_prev[..., None]
    m_t = jnp.maximum(inter_log, m_intra)
    p = jnp.exp(d_log - m_t[..., None]) * jnp.einsum('bhcjk,bhcsk->bhcjs', q, k)
    inter_scale = jnp.exp(inter_log - m_t)
    num = jnp.einsum('bhcjs,bhcsv->bhcjv', p, v) + inter_scale[..., None] * jnp.einsum('bhcjk,bhckv->bhcjv', q, c_prev)
    den = jnp.sum(p, axis=-1) + inter_scale * jnp.einsum('bhcjk,bhck->bhcj', q, n_prev)
    return num / jnp.maximum(jnp.abs(den), jnp.exp(-m_t))[..., None]


def retention_chunkwise(q, k, v):
    H, L = q.shape[1], q.shape[-2]
    log_gamma = jnp.log(1.0 - 2.0 ** (-5.0 - jnp.arange(H, dtype=jnp.float32)))
    q = q.astype(jnp.float32)
    k = k.astype(jnp.float32) * (k.shape[-1] ** -0.5)
    v = v.astype(jnp.float32)
    pos = jnp.arange(L, dtype=jnp.float32)
    rel = pos[:, None] - pos[None, :]
    decay = jnp.where(rel >= 0, jnp.exp(log_gamma[:, None, None] * jnp.maximum(rel, 0.0)), 0.0)
    scores = jnp.einsum('bhcjk,bhcsk->bhcjs', q, k) * decay[None, :, None]
    intra = jnp.einsum('bhcjs,bhcsv->bhcjv', scores, v)
    k_decay = jnp.exp(log_gamma[:, None] * (L - 1 - pos))
    r_loc = jnp.einsum('bhcsk,hs,bhcsv->bhckv', k, k_decay, v)
    g_chunk = jnp.exp(log_gamma * L)[None, :, None, None]

    def step(r, r_l):
        return g_chunk * r + r_l, r

    init = jnp.zeros((q.shape[0], H, q.shape[-1], v.shape[-1]), jnp.float32)
    _, r_prev = lax.scan(step, init, jnp.moveaxis(r_loc, 2, 0))
    r_prev = jnp.moveaxis(r_prev, 0, 2)
    q_decay = jnp.exp(log_gamma[:, None] * (pos + 1.0))
    inter = jnp.einsum('bhcjk,bhckv->bhcjv', q, r_prev) * q_decay[None, :, None, :, None]
    return intra + inter


def mixer(xm, positions, w_in, conv_w, conv_b, b_if, ml_norm_g, ret_norm_g, w_branch_ml, w_branch_ret, w_out):
    B, S, _ = xm.shape
    split_idx = np.cumsum(IN_SIZES)[:-1].tolist()
    proj = xm @ w_in
    ml_qk, ml_v, ml_o, ml_if, ret_q, ret_k, ret_v, ret_g, gate_ml, gate_ret = jnp.split(proj, split_idx, axis=-1)

    ml_qk = jax.nn.silu(causal_dwconv(ml_qk, conv_w, conv_b))
    ml_q, ml_k = ml_qk[..., :ML_QK_W], ml_qk[..., ML_QK_W:]
    if_pre = (ml_if + b_if).astype(jnp.float32)
    h_ml = mlstm_chunkwise(to_chunks(ml_q, ML_HEADS), to_chunks(ml_k, ML_HEADS), to_chunks(ml_v, ML_HEADS),
                           gate_chunks(if_pre[..., :ML_HEADS]), gate_chunks(if_pre[..., ML_HEADS:]))
    h_ml = from_chunks(h_ml).astype(xm.dtype)
    h_ml = jax.nn.sigmoid(ml_o) * head_layernorm(h_ml, ML_HEADS, ml_norm_g)

    ret_q = rotary(ret_q.reshape(B, S, RET_HEADS, RET_DQK), positions).reshape(B, S, RET_QK_W)
    ret_k = rotary(ret_k.reshape(B, S, RET_HEADS, RET_DQK), positions).reshape(B, S, RET_QK_W)
    h_ret = retention_chunkwise(to_chunks(ret_q, RET_HEADS), to_chunks(ret_k, RET_HEADS), to_chunks(ret_v, RET_HEADS))
    h_ret = from_chunks(h_ret).astype(xm.dtype)
    h_ret = jax.nn.silu(ret_g) * head_layernorm(h_ret, RET_HEADS, ret_norm_g)

    y = jax.nn.sigmoid(gate_ml) * (h_ml @ w_branch_ml) + jax.nn.sigmoid(gate_ret) * (h_ret @ w_branch_ret)
    return y @ w_out


def clamped_swiglu(gu):
    x_glu = jnp.minimum(gu[..., ::2], SWIGLU_LIMIT)
    x_lin = jnp.clip(gu[..., 1::2], -SWIGLU_LIMIT, SWIGLU_LIMIT)
    return x_glu * jax.nn.sigmoid(SWIGLU_ALPHA * x_glu) * (x_lin + 1.0)


def moe(xm, w_router, b_router, w_gate_up, b_gate_up, w_down, b_down):
    B, S, D = xm.shape
    T = B * S
    xt = xm.reshape(T, D)
    logits = (xt @ w_router + b_router).astype(jnp.float32)
    top_val, top_idx = lax.top_k(logits, TOP_K)
    top_w = jax.nn.softmax(top_val, axis=-1)
    n_assign = T * TOP_K
    n_blocks = -(-n_assign // EXPERT_BLOCK) + N_EXPERTS
    n_slots = n_blocks * EXPERT_BLOCK
    e_flat = top_idx.reshape(n_assign)
    tok_flat = jnp.arange(n_assign, dtype=jnp.int32) // TOP_K
    w_flat = top_w.reshape(n_assign)
    counts = jnp.bincount(e_flat, length=N_EXPERTS)
    group_start = jnp.cumsum(counts) - counts
    blocks_per_e = (counts + EXPERT_BLOCK - 1) // EXPERT_BLOCK
    block_end = jnp.cumsum(blocks_per_e)
    pad_start = (block_end - blocks_per_e) * EXPERT_BLOCK
    order = jnp.argsort(e_flat)
    e_sorted = e_flat[order]
    rank = jnp.arange(n_assign, dtype=jnp.int32) - group_start[e_sorted]
    dest = pad_start[e_sorted] + rank
    slot_tok = jnp.full((n_slots,), T, jnp.int32).at[dest].set(tok_flat[order])
    slot_w = jnp.zeros((n_slots,), jnp.float32).at[dest].set(w_flat[order])
    block_expert = jnp.minimum(jnp.searchsorted(block_end, jnp.arange(n_blocks), side='right'), N_EXPERTS - 1)
    x_pad = jnp.concatenate([xt, jnp.zeros((1, D), xt.dtype)], axis=0)
    x_blocks = x_pad[slot_tok].reshape(n_blocks, EXPERT_BLOCK, D)

    def expert_block(args):
        xb, e = args
        gu = xb @ w_gate_up[e] + b_gate_up[e]
        return clamped_swiglu(gu) @ w_down[e] + b_down[e]

    y_blocks = lax.map(expert_block, (x_blocks, block_expert))
    y_slots = y_blocks.reshape(n_slots, D) * slot_w[:, None].astype(y_blocks.dtype)
    y = jax.ops.segment_sum(y_slots, slot_tok, num_segments=T + 1)[:T]
    return y.reshape(B, S, D)


def setup_inputs(seed: int = 0) -> dict:
    key = jax.random.key(seed)
    ks = jax.random.split(key, 24)
    D, L, E, F = D_MODEL, DEPTH, N_EXPERTS, D_EXPERT
    nrm = lambda k, shape, s: jax.random.normal(k, shape, jnp.float32) * s
    x = nrm(ks[0], (BATCH, SEQ, D), 1.0)
    c = nrm(ks[1], (BATCH, D), 1.0)
    offsets = jax.random.randint(ks[2], (BATCH, 1), 0, 1024, dtype=jnp.int32)
    positions = offsets + jnp.arange(SEQ, dtype=jnp.int32)[None, :]
    b_i = -1.0 + nrm(ks[8], (L, ML_HEADS), 0.1)
    b_f = jnp.linspace(3.0, 6.0, ML_HEADS, dtype=jnp.float32)[None, :] + nrm(ks[9], (L, ML_HEADS), 0.1)
    return {
        "x": x,
        "c": c,
        "positions": positions,
        "w_ada": nrm(ks[3], (L, D, 6 * D), 0.5 * D ** -0.5),
        "b_ada": nrm(ks[4], (L, 6 * D), 0.02),
        "norm_mix_g": 1.0 + nrm(ks[5], (L, D), 0.02),
        "w_in": nrm(ks[6], (L, D, D_IN), D ** -0.5),
        "conv_w": nrm(ks[7], (L, CONV_K, 2 * ML_QK_W), CONV_K ** -0.5),
        "conv_b": nrm(ks[10], (L, 2 * ML_QK_W), 0.02),
        "b_if": jnp.concatenate([b_i, b_f], axis=-1),
        "ml_norm_g": 1.0 + nrm(ks[11], (L, ML_V_W), 0.02),
        "ret_norm_g": 1.0 + nrm(ks[12], (L, RET_V_W), 0.02),
        "w_branch_ml": nrm(ks[13], (L, ML_V_W, D), ML_V_W ** -0.5),
        "w_branch_ret": nrm(ks[14], (L, RET_V_W, D), RET_V_W ** -0.5),
        "w_out": nrm(ks[15], (L, D, D), D ** -0.5),
        "norm_ffn_g": 1.0 + nrm(ks[16], (L, D), 0.02),
        "w_router": nrm(ks[17], (L, D, E), D ** -0.5),
        "b_router": nrm(ks[18], (L, E), 0.01),
        "w_gate_up": nrm(ks[19], (L, E, D, 2 * F), D ** -0.5),
        "b_gate_up": nrm(ks[20], (L, E, 2 * F), 0.02),
        "w_down": nrm(ks[21], (L, E, F, D), F ** -0.5),
        "b_down": nrm(ks[22], (L, E, D), 0.02),
        "norm_final_g": 1.0 + nrm(ks[23], (D,), 0.02),
    }


def reference(x, c, positions, w_ada, b_ada, norm_mix_g, w_in, conv_w, conv_b, b_if, ml_norm_g, ret_norm_g,
              w_branch_ml, w_branch_ret, w_out, norm_ffn_g, w_router, b_router, w_gate_up, b_gate_up, w_down, b_down,
              norm_final_g):
    c_act = jax.nn.silu(c)
    for l in range(DEPTH):
        mod = c_act @ w_ada[l] + b_ada[l]
        shift_m, scale_m, gate_m, shift_f, scale_f, gate_f = jnp.split(mod[:, None, :], 6, axis=-1)
        h = rmsnorm(x, norm_mix_g[l]) * (1.0 + scale_m) + shift_m
        x = x + gate_m * mixer(h, positions, w_in[l], conv_w[l], conv_b[l], b_if[l], ml_norm_g[l], ret_norm_g[l],
                               w_branch_ml[l], w_branch_ret[l], w_out[l])
        h = rmsnorm(x, norm_ffn_g[l]) * (1.0 + scale_f) + shift_f
        x = x + gate_f * moe(h, w_router[l], b_router[l], w_gate_up[l], b_gate_up[l], w_down[l], b_down[l])
    return rmsnorm(x, norm_final_g)
```

```python
import contextlib
import math
import numpy as np
import concourse.bass as bass
import concourse.mybir as mybir
from concourse.bass_utils import run_bass_kernel_spmd

F32 = mybir.dt.float32
F32R = mybir.dt.float32r
I32 = mybir.dt.int32
ALU = mybir.AluOpType
AF = mybir.ActivationFunctionType

D = 1024
SEQ = 4096
TOK = 2048
G = 256
NE = 32
EPS = 1e-5
LNSCALE = math.log(128.0 ** -0.5)
TWO_PI = 6.283185307179586

C_ID, C_TRI, C_MNEG, C_SEL, C_ONES, C_DECT, C_QD, C_KDEC, C_INVF = 0, 128, 256, 384, 896, 1408, 1920, 2432, 2436
C_IOB, C_CU = 2692, 2756
NC = 2764


class Buf:
    __slots__ = ("name", "w", "r")

    def __init__(self, name=""):
        self.name = name
        self.w = None
        self.r = {}


class DmaSem:
    __slots__ = ("sem", "value", "name")

    def __init__(self, name):
        self.name = name
        self.sem = None
        self.value = 0


class Op:
    __slots__ = ("eng", "fn", "deps", "needed", "tok", "dma")


class Sched:
    ENGS = ("pe", "dve", "act", "pool", "sp")

    def __init__(self, sync_same=True):
        self.ops = []
        self.sync_same = sync_same
        self.dmasems = []
        self.last = {}

    def dmasem(self, name):
        d = DmaSem(name)
        self.dmasems.append(d)
        return d

    def op(self, eng, fn, reads=(), writes=(), dma=None, extra=()):
        o = Op()
        o.eng, o.fn, o.dma = eng, fn, dma
        o.needed = dma is not None
        o.tok = None
        deps = {}
        for b in reads:
            if b.w is not None:
                deps[id(b.w)] = b.w
        for b in writes:
            if b.w is not None:
                deps[id(b.w)] = b.w
            for d in b.r.values():
                deps[id(d)] = d
        for d in extra:
            deps[id(d)] = d
        o.deps = []
        for d in deps.values():
            if d is o:
                continue
            if d.dma is None and d.eng == eng and (eng == "pe" or not self.sync_same):
                continue
            d.needed = True
            o.deps.append(d)
        key = ("dma", id(dma)) if dma is not None else eng
        for b in reads:
            b.r[key] = o
        for b in writes:
            b.w = o
            b.r = {}
        self.ops.append(o)
        self.last[key] = o
        return o

    def barrier(self):
        lasts = list(self.last.values())
        for e in self.ENGS:
            self.op(e, None, extra=[d for d in lasts if d.fn is not None])

    def emit(self, nc, es, final_waits=()):
        esem = {e: es.enter_context(nc.semaphore("s_" + e)) for e in self.ENGS}
        for d in self.dmasems:
            d.sem = es.enter_context(nc.semaphore("d_" + d.name))
            d.value = 0
        cnt = {e: 0 for e in self.ENGS}
        for o in self.ops:
            if o.fn is None:
                continue
            if o.dma is not None:
                o.dma.value += 16
                o.tok = (o.dma.sem, o.dma.value)
            elif o.needed:
                cnt[o.eng] += 1
                o.tok = (esem[o.eng], cnt[o.eng])
        block = es.enter_context(nc.Block())
        per = {e: [o for o in self.ops if o.eng == e] for e in self.ENGS}

        def run(e, h):
            waited = {}
            for o in per[e]:
                need = {}
                for d in o.deps:
                    s, v = d.tok
                    k = id(s)
                    if waited.get(k, 0) >= v:
                        continue
                    if k not in need or need[k][1] < v:
                        need[k] = (s, v)
                for k, (s, v) in need.items():
                    h.wait_ge(s, v)
                    waited[k] = v
                if o.fn is None:
                    continue
                ins = o.fn(h)
                if o.tok is not None:
                    ins.then_inc(o.tok[0], 16 if o.dma is not None else 1)
            if e == "sp":
                for d in final_waits:
                    h.wait_ge(d.sem, d.value)

        @block.tensor
        def _(h):
            run("pe", h)

        @block.vector
        def _(h):
            run("dve", h)

        @block.scalar
        def _(h):
            run("act", h)

        @block.gpsimd
        def _(h):
            run("pool", h)

        @block.sync
        def _(h):
            run("sp", h)


def build_nc(debug=False):
    nc = bass.Bass("TRN2", target_bir_lowering=False)

    def din(name, shape, dt=F32):
        return nc.dram_tensor(name, list(shape), dt, kind="ExternalInput").ap()

    xo = din("xo", [TOK, D]); xp = din("xp", [TOK, D])
    poso = din("poso", [128, 16], I32); posp = din("posp", [128, 16], I32)
    flag_d = din("flag", [128, 1]); cT_d = din("cT", [128, 8]); cst_d = din("cst", [128, NC])
    w_ada = din("w_ada", [D, 6 * D]); b_adaT = din("b_adaT", [128, 48])
    gmixT_d = din("gmixT", [128, 8]); gffnT_d = din("gffnT", [128, 8]); gfin_d = din("gfin", [1, D])
    w_in = din("w_in", [D, 8200]); convwT_d = din("convwT", [128, 32]); convbT_d = din("convbT", [128, 8])
    bif_d = din("bif", [1, 8]); mlgT_d = din("mlgT", [128, 8]); retgT_d = din("retgT", [128, 8])
    wbml = din("wbml", [D, D]); wbret = din("wbret", [D, D]); wout = din("wout", [D, D])
    wr_d = din("wr", [D, NE]); br_d = din("br", [1, NE])
    wgu = din("wgu", [NE, D, 2 * D]); bguT_d = din("bguT", [NE * 128, 16])
    wd = din("wd", [NE, D // 2, 2 * D]); bd_d = din("bd", [NE, D])
    out = nc.dram_tensor("out", [TOK, D], F32, kind="ExternalOutput").ap()
    x2s = nc.dram_tensor("x2s", [TOK, D], F32, kind="ExternalOutput" if debug else "Internal").ap()

    S = Sched()
    es = contextlib.ExitStack()
    NCOL = 53180
    pbanks = [es.enter_context(nc.psum_tensor("pb%d" % i, [128, 512], F32)) for i in range(8)]
    PB = [Buf("pb%d" % i) for i in range(8)]
    pctr = [0]

    def nextp():
        i = pctr[0] % 8
        pctr[0] += 1
        return pbanks[i][:, :], PB[i]

    top = [0]
    tcount = [0]

    def al(n):
        n = (n + 7) // 8 * 8
        a = top[0]
        top[0] += n
        assert top[0] <= NCOL, top[0]
        return a

    def T(n, dt=F32, name=""):
        a = al(n)
        tcount[0] += 1
        t = nc.alloc_sbuf_tensor_at("t%d_%s" % (tcount[0], name), [128, n], dt, offset=16640 + 4 * a)
        return t[:, :], Buf(name)

    def mm(o, lhsT, rhs, st, sp, rd, wr):
        S.op("pe", lambda h: h.matmul(o, lhsT=lhsT, rhs=rhs, start=st, stop=sp), rd, wr)

    def act(o, i, func, rd, wr, bias=None, scale=None, accum=None):
        kw = {}
        if bias is not None:
            kw["bias"] = bias
        if scale is not None:
            kw["scale"] = scale
        if accum is not None:
            kw["accum_out"] = accum
        S.op("act", lambda h: h.activation(o, i, func, **kw), rd, wr)

    def ts(o, i, s1, s2, op0, op1, rd, wr, eng="dve"):
        if op1 is None:
            S.op(eng, lambda h: h.tensor_scalar(o, i, s1, None, op0), rd, wr)
        else:
            S.op(eng, lambda h: h.tensor_scalar(o, i, s1, s2, op0, op1), rd, wr)

    def tt(o, a, b, op, rd, wr, eng="dve"):
        S.op(eng, lambda h: h.tensor_tensor(o, a, b, op), rd, wr)

    def stt(o, a, s, b, op0, op1, rd, wr):
        S.op("dve", lambda h: h.scalar_tensor_tensor(o, a, s, b, op0, op1), rd, wr)

    def cp(o, i, rd, wr, eng="dve"):
        if eng == "act":
            S.op(eng, lambda h: h.copy(o, i), rd, wr)
        else:
            S.op(eng, lambda h: h.tensor_copy(o, i), rd, wr)

    def dma(q, o, i, rd, wr, sem):
        S.op(q, lambda h: h.dma_start(out=o, in_=i), rd, wr, dma=sem)

    cst, Bcst = T(NC, name="cst")
    ident = cst[:, C_ID:C_ID + 128]
    tri = cst[:, C_TRI:C_TRI + 128]
    mneg = cst[:, C_MNEG:C_MNEG + 128]
    ones = cst[:, C_ONES:C_ONES + 512]
    onesR, BonesR = T(512, F32R, "onesR")
    modT, BmodT = T(48, name="modT")
    AB, BAB = T(32, name="AB")
    sm, Bsm = T(160, name="small")
    gmixT = sm[:, 0:8]; gffnT = sm[:, 8:16]; badaT = sm[:, 16:64]; convw = sm[:, 64:96]; convb = sm[:, 96:104]
    bifb = sm[:, 104:112]; flag = sm[:, 112:113]; epsc = sm[:, 113:114]; cTt = sm[:, 114:122]
    mlgT = sm[:, 122:130]; retgT = sm[:, 130:138]
    cact2, Bcact = T(16, F32R, "cact2")
    gfb, Bgfb = T(D, name="gate_f_bc")
    gfin, Bgfin = T(D, name="gfin_bc")
    wrt, Bwrt = T(8 * NE, F32R, "wr")
    brb, Bbrb = T(NE, name="br")
    posf, Bposf = T(32, name="posf")
    posi, Bposi = T(32, I32, "posi")
    PERS_END = top[0]

    dl = {n: S.dmasem(n) for n in ["c0", "c1", "c2", "c3", "c4", "c5", "c6", "c7", "c8", "c9", "c10", "c11", "c12", "c13", "c14", "c15", "c16"]}
    dma("sp", cst, cst_d[:, :], [], [Bcst], dl["c0"])
    dma("sp", gmixT, gmixT_d[:, :], [], [Bsm], dl["c1"])
    dma("sp", gffnT, gffnT_d[:, :], [], [Bsm], dl["c1"])
    dma("sp", badaT, b_adaT[:, :], [], [Bsm], dl["c1"])
    dma("sp", convw, convwT_d[:, :], [], [Bsm], dl["c1"])
    dma("sp", convb, convbT_d[:, :], [], [Bsm], dl["c1"])
    dma("sp", bifb, bif_d[0:1, :].partition_broadcast(128), [], [Bsm], dl["c1"])
    dma("sp", flag, flag_d[:, :], [], [Bsm], dl["c1"])
    dma("sp", cTt, cT_d[:, :], [], [Bsm], dl["c1"])
    dma("sp", mlgT, mlgT_d[:, :], [], [Bsm], dl["c1"])
    dma("sp", retgT, retgT_d[:, :], [], [Bsm], dl["c1"])
    S.op("dve", lambda h: h.memset(epsc, EPS), [], [Bsm])
    cp(onesR, ones, [Bcst], [BonesR])
    dma("sp", gfin, gfin_d[0:1, :].partition_broadcast(128), [], [Bgfin], dl["c2"])
    dma("pool", wrt.rearrange("p (k n) -> p k n", n=NE), wr_d.rearrange("(k p) n -> p k n", p=128), [], [Bwrt], dl["c3"])
    dma("sp", brb, br_d[0:1, :].partition_broadcast(128), [], [Bbrb], dl["c4"])
    dma("sp", posi[:, 0:16], posp[:, :], [], [Bposi], dl["c6"])
    dma("sp", posi[:, 16:32], poso[:, :], [], [Bposi], dl["c6"])
    cp(posf, posi, [Bposi], [Bposf])

    gmb, Bgmb = T(D, name="gate_m_bc")
    Cml = [T(257, F32, "Cml%d" % h) for h in range(4)]
    CmlR = [T(258, F32R, "CmlR%d" % h) for h in range(4)]
    Cret = [T(256, F32, "Cret%d" % h) for h in range(4)]
    CretR = [T(256, F32R, "CretR%d" % h) for h in range(4)]
    rowA, BrowA = T(600, name="rows")
    bTr = rowA[0:4, 0:256]; Mr = rowA[0:4, 256:512]; mpr = rowA[0:4, 512:530]; Ar = rowA[0:4, 530:532]
    ext = rowA[0:4, 532:540]
    xg = [T(D, name="xg%d" % c) for c in range(2)]
    xs, Bxs = T(D, name="xs")
    h1T, Bh1T = T(8 * G, F32R, "h1T")
    h1T3 = h1T.rearrange("p (k n) -> p k n", n=G)
    NW = 3
    wst = [T(8 * 256, F32R, "wst%d" % i) for i in range(NW)]
    wsem = [S.dmasem("wst%d" % i) for i in range(NW)]
    wctr = [0]
    qkpre, Bqkpre = T(8 * 259, name="qkpre")
    qkpre3 = qkpre.rearrange("p (f n) -> p f n", n=259)
    cacc, Bcacc = T(G, name="cacc")
    qkT, BqkT = T(8 * G, F32R, "qkT")
    qkT3 = qkT.rearrange("p (f n) -> p f n", n=G)
    vml = [T(4 * 258, F32R, "vml%d" % c) for c in range(2)]
    oml = [T(D, name="oml%d" % c) for c in range(2)]
    ifb = [T(8, name="if%d" % c) for c in range(2)]
    rq = [T(512, name="rq%d" % c) for c in range(2)]
    rk = [T(512, name="rk%d" % c) for c in range(2)]
    rv = [T(D, F32R, "rv%d" % c) for c in range(2)]
    rg = [T(D, name="rg%d" % c) for c in range(2)]
    gsm, Bgsm = T(104, name="gsm")
    NMh, BNM = T(4 * G, name="NM")
    hmT, BhmT = T(8 * G, F32R, "hmT")
    hrT, BhrT = T(8 * G, F32R, "hrT")
    yT, ByT = T(8 * G, F32R, "yT")
    hmT3 = hmT.rearrange("p (k n) -> p k n", n=G); hrT3 = hrT.rearrange("p (k n) -> p k n", n=G)
    yT3 = yT.rearrange("p (k n) -> p k n", n=G)
    DTs = [T(128, name="DT%d" % h) for h in range(4)]
    PTs = [T(128, F32R, "PT%d" % h) for h in range(4)]
    kws = [T(128, F32R, "kw%d" % h) for h in range(4)]
    inss = [T(258, name="inter_s%d" % h) for h in range(4)]
    tots = [T(258, name="tot%d" % h) for h in range(4)]
    hhs = [(inss[h][0][:, 0:256], inss[h][1]) for h in range(4)]
    sths = [T(16, name="sth%d" % h) for h in range(4)]
    qdTs = kws
    st6, Bst6 = T(16, name="stats")
    rot, Brot = T(768, name="rot")
    sg2, Bsg2 = rot[:, 0:256], Brot
    sct = [T(512, name="sincos%d" % c) for c in range(2)]
    kint, Bkint = T(256, I32, "kint")
    rtmp, Brtmp = xs, Bxs
    qrTs = [T(512, F32R, "qrT%d" % c) for c in range(2)]
    krTs = [T(512, F32R, "krT%d" % c) for c in range(2)]
    sg1, Bsg1 = cacc, Bcacc
    dgt, Bdgt = DTs[0]
    x2sem = S.dmasem("x2st")
    xsem = [S.dmasem("xld%d" % c) for c in range(2)]
    MIX_END = top[0]

    for h in range(4):
        S.op("dve", lambda hh_, a=Cml[h][0]: hh_.memset(a, 0.0), [], [Cml[h][1]])
        cp(CmlR[h][0][:, 0:257], Cml[h][0], [Cml[h][1]], [CmlR[h][1]])
        cp(CmlR[h][0][:, 257:258], Cml[h][0][:, 0:1], [Cml[h][1]], [CmlR[h][1]])
        S.op("dve", lambda hh_, a=Cret[h][0]: hh_.memset(a, 0.0), [], [Cret[h][1]])
        cp(CretR[h][0], Cret[h][0], [Cret[h][1]], [CretR[h][1]])
    S.op("dve", lambda h: h.memset(qkpre, 0.0), [], [Bqkpre])
    S.op("dve", lambda h: h.memset(rowA, 0.0), [], [BrowA])
    for c in range(2):
        for q_ in range(0, 1032, 512):
            n_ = min(512, 1032 - q_)
            cp(vml[c][0][:, q_:q_ + n_], ones[:, 0:n_], [Bcst], [vml[c][1]])

    def wload(src3, ncols):
        i = wctr[0] % NW
        wctr[0] += 1
        ap, b = wst[i]
        v3 = ap[:, 0:8 * ncols].rearrange("p (k n) -> p k n", n=ncols)
        dma("pool", v3, src3, [], [b], wsem[i])
        return v3, b

    def win_tile(c0, ncols=256):
        return wload(w_in.rearrange("(k p) c -> p k c", p=128)[:, :, c0:c0 + ncols], ncols)

    act(cact2.rearrange("p (k t) -> p k t", t=2)[:, :, 0], cTt, AF.Silu, [Bsm], [Bcact])
    act(cact2.rearrange("p (k t) -> p k t", t=2)[:, :, 1], cTt, AF.Silu, [Bsm], [Bcact])
    cact3 = cact2.rearrange("p (k t) -> p k t", t=2)
    pm, Bpm = nextp()
    wada3 = w_ada.rearrange("(k p) c -> p k c", p=128)
    for j in range(6):
        for sb_ in range(4):
            w3, wb = wload(wada3[:, :, j * D + sb_ * 256: j * D + (sb_ + 1) * 256], 256)
            for ft in range(2):
                col = (j * 8 + sb_ * 2 + ft) * 2
                for kc in range(8):
                    mm(pm[:, col:col + 2], w3[:, kc, ft * 128:(ft + 1) * 128], cact3[:, kc, :], kc == 0, kc == 7,
                       [wb, Bcact], [Bpm])
    tt(modT, pm[:, 0:96].rearrange("p (c t) -> p c t", t=2)[:, :, 0], badaT, ALU.add, [Bpm, Bsm], [BmodT])
    stt(AB[:, 0:8], modT[:, 8:16], 1.0, gmixT, ALU.add, ALU.mult, [BmodT, Bsm], [BAB])
    cp(AB[:, 8:16], modT[:, 0:8], [BmodT], [BAB])
    stt(AB[:, 16:24], modT[:, 32:40], 1.0, gffnT, ALU.add, ALU.mult, [BmodT, Bsm], [BAB])
    cp(AB[:, 24:32], modT[:, 24:32], [BmodT], [BAB])
    A1 = AB[:, 0:8]; B1 = AB[:, 8:16]; A2 = AB[:, 16:24]; B2 = AB[:, 24:32]

    def bcast_vec(dst, Bdst, col0):
        for hf in range(2):
            pb, Bp = nextp()
            for k4 in range(4):
                kc = hf * 4 + k4
                ts(dgt, ident, modT[:, col0 + kc:col0 + kc + 1], None, ALU.mult, None, [Bcst, BmodT], [Bdgt])
                mm(pb[:, k4 * 128:(k4 + 1) * 128], ones[:, 0:128], dgt, True, True, [Bcst, Bdgt], [Bp])
            cp(dst[:, hf * 512:(hf + 1) * 512], pb, [Bp], [Bdst])

    bcast_vec(gmb, Bgmb, 16)
    bcast_vec(gfb, Bgfb, 40)

    def norm_to_T(xsrc, Bx, dstT3, BdstT, c, Acol, Bcol):
        act(xs, xsrc, AF.Square, [Bx], [Bxs, Bst6], accum=st6[:, 0:1])
        act(st6[:, 1:2], st6[:, 0:1], AF.Sqrt, [Bst6, Bsm], [Bst6], bias=epsc, scale=1.0 / D)
        S.op("dve", lambda h: h.reciprocal(st6[:, 2:3], st6[:, 1:2]), [Bst6], [Bst6])
        ts(xs, xsrc, st6[:, 2:3], None, ALU.mult, None, [Bx, Bst6], [Bxs])
        for hf in range(2):
            pb, Bp = nextp()
            for k4 in range(4):
                kc = hf * 4 + k4
                S.op("pe", lambda h, o=pb[:, k4 * 128:(k4 + 1) * 128], i=xs[:, kc * 128:(kc + 1) * 128]: h.transpose(o, i, ident),
                     [Bxs, Bcst], [Bp])
            for k4 in range(4):
                kc = hf * 4 + k4
                act(dstT3[:, kc, c * 128:(c + 1) * 128], pb[:, k4 * 128:(k4 + 1) * 128], AF.Identity, [Bp, BAB], [BdstT],
                    bias=Bcol[:, kc:kc + 1], scale=Acol[:, kc:kc + 1])

    def proj_fm(c0, nft, evac):
        for t in range(nft // 2):
            w3, wb = win_tile(c0 + t * 256)
            for f in range(2):
                pb, Bp = nextp()
                for kc in range(8):
                    mm(pb[:, 0:G], w3[:, kc, f * 128:(f + 1) * 128], h1T3[:, kc, :], kc == 0, kc == 7, [wb, Bh1T], [Bp])
                evac(t * 2 + f, pb[:, 0:G], Bp)

    def proj_tm(c0, ntile, evac, ncols=256):
        for t in range(ntile):
            w3, wb = win_tile(c0 + t * ncols, ncols)
            for c in range(2):
                pb, Bp = nextp()
                for kc in range(8):
                    mm(pb[:, 0:ncols], h1T3[:, kc, c * 128:(c + 1) * 128], w3[:, kc, :], kc == 0, kc == 7, [Bh1T, wb], [Bp])
                evac(t, c, pb[:, 0:ncols], Bp)

    def ln_stages(items):
        for hv, Bh, st, Bst in items:
            S.op("dve", lambda h, st=st, hv=hv: h.bn_stats(st[:, 4:10], hv), [Bh], [Bst])
        yield
        for hv, Bh, st, Bst in items:
            S.op("dve", lambda h, st=st: h.bn_aggr(st[:, 10:12], st[:, 4:10]), [Bst], [Bst])
        yield
        for hv, Bh, st, Bst in items:
            act(st[:, 12:13], st[:, 11:12], AF.Sqrt, [Bst, Bsm], [Bst], bias=epsc, scale=1.0)
        yield
        for hv, Bh, st, Bst in items:
            S.op("dve", lambda h, st=st: h.reciprocal(st[:, 13:14], st[:, 12:13]), [Bst], [Bst])
        yield
        for hv, Bh, st, Bst in items:
            ts(hv, hv, st[:, 10:11], st[:, 13:14], ALU.subtract, ALU.mult, [Bh, Bst], [Bh])
        yield

    GAM = [1.0 - 2.0 ** (-5.0 - h) for h in range(4)]

    for g in range(16):
        own = g >= 8
        xsrc = xo if own else xp
        t0 = (g - 8) * G if own else g * G
        for c in range(2):
            dma("sp", xg[c][0], xsrc[t0 + c * 128:t0 + (c + 1) * 128, :], [], [xg[c][1]], xsem[c])
            norm_to_T(xg[c][0], xg[c][1], h1T3, Bh1T, c, A1, B1)
        ang = rot[:, 0:256]; red = rot[:, 256:512]; kf = rot[:, 512:768]
        for c in range(2):
            pcol = (16 if own else 0) + (g % 8) * 2 + c
            ts(ang, cst[:, C_INVF:C_INVF + 256], posf[:, pcol:pcol + 1], None, ALU.mult, None, [Bcst, Bposf], [Brot])
            for dst_, off in ((sct[c][0][:, 0:256], 0.0), (sct[c][0][:, 256:512], math.pi / 2)):
                ts(red, ang, off, None, ALU.add, None, [Brot], [Brot])
                ts(kint, red, 1.0 / TWO_PI, None, ALU.mult, None, [Brot], [Bkint])
                cp(kf, kint, [Bkint], [Brot])
                stt(red, kf, -TWO_PI, red, ALU.mult, ALU.add, [Brot], [Brot])
                ts(red, red, 3.14159, -3.14159, ALU.min, ALU.max, [Brot], [Brot])
                act(dst_, red, AF.Sin, [Brot], [sct[c][1]])
        if g == 8:
            ts(qkpre, qkpre, flag, None, ALU.mult, None, [Bqkpre, Bsm], [Bqkpre])
            ts(mpr[:, 0:1], mpr[:, 0:1], flag[0:4, :], None, ALU.mult, None, [BrowA, Bsm], [BrowA])
            for h in range(4):
                ts(Cml[h][0], Cml[h][0], flag, None, ALU.mult, None, [Cml[h][1], Bsm], [Cml[h][1]])
                cp(CmlR[h][0][:, 0:257], Cml[h][0], [Cml[h][1]], [CmlR[h][1]])
                ts(Cret[h][0], Cret[h][0], flag, None, ALU.mult, None, [Cret[h][1], Bsm], [Cret[h][1]])
                cp(CretR[h][0], Cret[h][0], [Cret[h][1]], [CretR[h][1]])

        need_q = own or g == 7

        def ev_qk(base):
            def f(ft, ps_, Bp):
                cp(qkpre3[:, base + ft, 3:259], ps_, [Bp], [Bqkpre], eng="act")
            return f
        proj_fm(512, 4, ev_qk(4))
        if need_q:
            proj_fm(0, 4, ev_qk(0))
        for ft in (range(8) if need_q else range(4, 8)):
            ts(cacc, qkpre3[:, ft, 0:G], convw[:, ft * 4:ft * 4 + 1], None, ALU.mult, None, [Bqkpre, Bsm], [Bcacc])
            for i in range(1, 4):
                stt(cacc, qkpre3[:, ft, i:i + G], convw[:, ft * 4 + i:ft * 4 + i + 1], cacc, ALU.mult, ALU.add,
                    [Bqkpre, Bsm, Bcacc], [Bcacc])
            act(qkT3[:, ft, :], cacc, AF.Silu, [Bcacc, Bsm], [BqkT], bias=convb[:, ft:ft + 1])
            cp(qkpre3[:, ft, 0:3], qkpre3[:, ft, G:G + 3], [Bqkpre], [Bqkpre])

        def ev_v(t, c, ps_, Bp):
            cp(vml[c][0][:, t * 258:t * 258 + 256], ps_, [Bp], [vml[c][1]], eng="act")
        proj_tm(1024, 4, ev_v)

        def ev_if(t, c, ps_, Bp):
            tt(ifb[c][0], ps_, bifb, ALU.add, [Bp, Bsm], [ifb[c][1]])
        proj_tm(3072, 1, ev_if, ncols=8)
        if own:
            def ev_o(t, c, ps_, Bp):
                act(oml[c][0][:, t * 256:(t + 1) * 256], ps_, AF.Sigmoid, [Bp], [oml[c][1]])
            proj_tm(2048, 4, ev_o)

        lf = gsm[:, 0:8]; a_ = gsm[:, 8:16]; b_ = gsm[:, 16:24]; tmp8 = gsm[:, 24:32]; bb = gsm[:, 32:40]
        u_ = gsm[:, 40:48]; isc = gsm[:, 48:56]; emt = gsm[:, 56:64]; spv = gsm[:, 64:72]; MT = gsm[:, 72:80]
        MLb = gsm[:, 80:88]; MPb = gsm[:, 88:96]; tmp8b = gsm[:, 96:104]
        for c in range(2):
            fp = ifb[c][0][:, 4:8]
            act(tmp8[:, c * 4:c * 4 + 4], fp, AF.Abs, [ifb[c][1]], [Bgsm])
            act(tmp8[:, c * 4:c * 4 + 4], tmp8[:, c * 4:c * 4 + 4], AF.Exp, [Bgsm], [Bgsm], scale=-1.0)
            act(tmp8[:, c * 4:c * 4 + 4], tmp8[:, c * 4:c * 4 + 4], AF.Ln, [Bgsm], [Bgsm], bias=1.0)
            ts(lf[:, c * 4:c * 4 + 4], fp, 0.0, None, ALU.min, None, [ifb[c][1]], [Bgsm])
            tt(lf[:, c * 4:c * 4 + 4], lf[:, c * 4:c * 4 + 4], tmp8[:, c * 4:c * 4 + 4], ALU.subtract, [Bgsm], [Bgsm])
        pa, Bpa = nextp()
        mm(pa[:, 0:8], tri, lf, True, True, [Bcst, Bgsm], [Bpa])
        for c in range(2):
            mm(pa[0:4, 16 + c:17 + c], lf[:, c * 4:c * 4 + 4], ones[:, 0:1], True, True, [Bgsm, Bcst], [Bpa])
        cp(a_, pa[:, 0:8], [Bpa], [Bgsm])
        cp(Ar, pa[0:4, 16:18], [Bpa], [BrowA])
        for c in range(2):
            tt(b_[:, c * 4:c * 4 + 4], ifb[c][0][:, 0:4], a_[:, c * 4:c * 4 + 4], ALU.subtract, [ifb[c][1], Bgsm], [Bgsm])
        pt_, Bpt = nextp()
        for c in range(2):
            S.op("pe", lambda h, o=pt_[0:4, c * 128:(c + 1) * 128], i=b_[:, c * 4:c * 4 + 4]: h.transpose(o, i, ident),
                 [Bgsm, Bcst], [Bpt])
        cp(bTr, pt_[0:4, 0:256], [Bpt], [BrowA])
        for c in range(2):
            ci = (g % 8) * 2 + c if False else c
            S.op("dve", lambda h, o=Mr[:, c * 128:(c + 1) * 128], d=bTr[:, c * 128:(c + 1) * 128], ini=mpr[:, c:c + 1]:
                 h.tensor_tensor_scan(o, d, d, ini, ALU.max, ALU.max), [BrowA], [BrowA])
            tt(mpr[:, c + 1:c + 2], Ar[:, c:c + 1], Mr[:, c * 128 + 127:c * 128 + 128], ALU.add, [BrowA], [BrowA])
            cp(ext[:, c:c + 1], Mr[:, c * 128 + 127:c * 128 + 128], [BrowA], [BrowA])
            cp(ext[:, 2 + c:3 + c], mpr[:, c:c + 1], [BrowA], [BrowA])
        cp(mpr[:, 0:1], mpr[:, 2:3], [BrowA], [BrowA])
        pe_, Bpe = nextp()
        for h in range(4):
            pb, Bp = nextp()
            mm(pb[:, 0:G], cst[0:4, C_SEL + h * 128:C_SEL + (h + 1) * 128], Mr, True, True, [Bcst, BrowA], [Bp])
            for c in range(2):
                tt(NMh[:, h * G + c * 128:h * G + (c + 1) * 128], mneg, pb[:, c * 128:(c + 1) * 128], ALU.subtract,
                   [Bcst, Bp], [BNM])
            mm(pe_[:, h * 4:h * 4 + 4], cst[0:4, C_SEL + h * 128:C_SEL + (h + 1) * 128], ext[:, 0:4], True, True,
               [Bcst, BrowA], [Bpe])
        for c in range(2):
            S.op("pe", lambda h, o=pe_[:, 32 + c * 4:36 + c * 4], i=Mr[:, c * 128:(c + 1) * 128]: h.transpose(o, i, ident[0:4, 0:4]),
                 [BrowA, Bcst], [Bpe])
        cp(MT, pe_[:, 32:40], [Bpe], [Bgsm])
        pe3 = pe_[:, 0:16].rearrange("p (h f) -> p h f", f=4)
        for c in range(2):
            cp(MLb[:, c * 4:c * 4 + 4], pe3[:, :, c], [Bpe], [Bgsm])
            cp(MPb[:, c * 4:c * 4 + 4], pe3[:, :, 2 + c], [Bpe], [Bgsm])
        ts(bb, b_, LNSCALE, None, ALU.add, None, [Bgsm], [Bgsm])
        tt(tmp8, b_, MLb, ALU.subtract, [Bgsm], [Bgsm])
        act(u_, tmp8, AF.Exp, [Bgsm], [Bgsm])
        tt(tmp8, MPb, MLb, ALU.subtract, [Bgsm], [Bgsm])
        act(spv, tmp8, AF.Exp, [Bgsm], [Bgsm])
        if own:
            tt(tmp8, MPb, MT, ALU.subtract, [Bgsm], [Bgsm])
            act(isc, tmp8, AF.Exp, [Bgsm], [Bgsm], bias=LNSCALE)
            tt(tmp8b, a_, MT, ALU.add, [Bgsm], [Bgsm])
            act(emt, tmp8b, AF.Exp, [Bgsm], [Bgsm], scale=-1.0)

        def ev_rk(t, c, ps_, Bp):
            cp(rk[c][0][:, t * 256:(t + 1) * 256], ps_, [Bp], [rk[c][1]], eng="act")
        proj_tm(3592, 2, ev_rk)

        def ev_rv(t, c, ps_, Bp):
            cp(rv[c][0][:, t * 256:(t + 1) * 256], ps_, [Bp], [rv[c][1]], eng="act")
        proj_tm(4104, 4, ev_rv)
        if own:
            def ev_rq(t, c, ps_, Bp):
                cp(rq[c][0][:, t * 256:(t + 1) * 256], ps_, [Bp], [rq[c][1]], eng="act")
            proj_tm(3080, 2, ev_rq)

            def ev_rg(t, c, ps_, Bp):
                act(rg[c][0][:, t * 256:(t + 1) * 256], ps_, AF.Silu, [Bp], [rg[c][1]])
            proj_tm(5128, 4, ev_rg)

        H4 = range(4)

        def rotary(src, Bsrc, c):
            s3 = sct[c][0][:, 0:256].rearrange("p (h d) -> p h d", d=64)
            c3 = sct[c][0][:, 256:512].rearrange("p (h d) -> p h d", d=64)
            Bsc = sct[c][1]
            x4 = src.rearrange("p (h t d) -> p h t d", t=2, d=64)
            r4 = rtmp[:, 0:512].rearrange("p (h t d) -> p h t d", t=2, d=64)
            q4 = rtmp[:, 512:1024].rearrange("p (h t d) -> p h t d", t=2, d=64)
            tt(r4[:, :, 0, :], x4[:, :, 0, :], c3, ALU.mult, [Bsrc, Bsc], [Brtmp])
            tt(r4[:, :, 1, :], x4[:, :, 1, :], c3, ALU.mult, [Bsrc, Bsc], [Brtmp])
            tt(q4[:, :, 0, :], x4[:, :, 1, :], s3, ALU.mult, [Bsrc, Bsc], [Brtmp])
            tt(q4[:, :, 1, :], x4[:, :, 0, :], s3, ALU.mult, [Bsrc, Bsc], [Brtmp])
            tt(x4[:, :, 0, :], r4[:, :, 0, :], q4[:, :, 0, :], ALU.subtract, [Brtmp], [Bsrc])
            tt(x4[:, :, 1, :], r4[:, :, 1, :], q4[:, :, 1, :], ALU.add, [Brtmp], [Bsrc])
        for c in range(2):
            rotary(rk[c][0], rk[c][1], c)
            if own:
                rotary(rq[c][0], rq[c][1], c)
        def ml_gen(c):
            kTs = [qkT3[:, 4 + h, c * 128:(c + 1) * 128] for h in H4]
            qTs = [qkT3[:, h, c * 128:(c + 1) * 128] for h in H4]
            vxs = [vml[c][0][:, h * 258:h * 258 + 258] for h in H4]
            cols = [c * 4 + h for h in H4]
            if own:
                pS = [nextp() for h in H4]
                for h in H4:
                    mm(pS[h][0][:, 0:128], kTs[h], qTs[h], True, True, [BqkT], [pS[h][1]])
                yield
                for h in H4:
                    act(DTs[h][0], NMh[:, h * G + c * 128:h * G + (c + 1) * 128], AF.Exp, [BNM, Bgsm], [DTs[h][1]],
                        bias=bb[:, cols[h]:cols[h] + 1])
                yield
                for h in H4:
                    tt(PTs[h][0], pS[h][0][:, 0:128], DTs[h][0], ALU.mult, [pS[h][1], DTs[h][1]], [PTs[h][1]])
                yield
                pI = [nextp() for h in H4]
                for h in H4:
                    mm(pI[h][0][:, 0:258], PTs[h][0], vxs[h], True, True, [PTs[h][1], vml[c][1]], [pI[h][1]])
                yield
                pJ = [nextp() for h in H4]
                for h in H4:
                    mm(pJ[h][0][:, 0:258], qTs[h], CmlR[h][0], True, True, [BqkT, CmlR[h][1]], [pJ[h][1]])
                yield
                for h in H4:
                    act(inss[h][0], pJ[h][0][:, 0:258], AF.Copy, [pJ[h][1], Bgsm], [inss[h][1]], scale=isc[:, cols[h]:cols[h] + 1])
                yield
                for h in H4:
                    tt(tots[h][0], pI[h][0][:, 0:258], inss[h][0], ALU.add, [pI[h][1], inss[h][1]], [tots[h][1]])
                yield
                for h in H4:
                    act(sths[h][0][:, 14:15], tots[h][0][:, 256:257], AF.Abs, [tots[h][1]], [sths[h][1]])
                yield
                for h in H4:
                    ts(sths[h][0][:, 14:15], sths[h][0][:, 14:15], emt[:, cols[h]:cols[h] + 1], None, ALU.max, None,
                       [sths[h][1], Bgsm], [sths[h][1]])
                yield
                for h in H4:
                    S.op("dve", lambda hd, st=sths[h][0]: hd.reciprocal(st[:, 15:16], st[:, 14:15]), [sths[h][1]], [sths[h][1]])
                yield
                for h in H4:
                    ts(hhs[h][0], tots[h][0][:, 0:256], sths[h][0][:, 15:16], None, ALU.mult, None, [tots[h][1], sths[h][1]], [hhs[h][1]])
                yield
                yield from ln_stages([(hhs[h][0], hhs[h][1], sths[h][0], sths[h][1]) for h in H4])
                for h in H4:
                    osl = oml[c][0][:, h * 256:(h + 1) * 256]
                    tt(osl, osl, hhs[h][0], ALU.mult, [oml[c][1], hhs[h][1]], [oml[c][1]])
            pK = [nextp() for h in H4]
            for h in H4:
                S.op("pe", lambda hd, o=pK[h][0][:, 0:128], i=kTs[h].bitcast(F32): hd.transpose(o, i, ident), [BqkT, Bcst], [pK[h][1]])
            yield
            for h in H4:
                act(kws[h][0], pK[h][0][:, 0:128], AF.Copy, [pK[h][1], Bgsm], [kws[h][1]], scale=u_[:, cols[h]:cols[h] + 1])
            yield
            pC = [nextp() for h in H4]
            for h in H4:
                mm(pC[h][0][:, 0:258], kws[h][0], vxs[h], True, True, [kws[h][1], vml[c][1]], [pC[h][1]])
            yield
            for h in H4:
                stt(Cml[h][0], Cml[h][0], spv[:, cols[h]:cols[h] + 1], pC[h][0][:, 0:257], ALU.mult, ALU.add,
                    [Cml[h][1], Bgsm, pC[h][1]], [Cml[h][1]])
            yield
            for h in H4:
                cp(CmlR[h][0][:, 0:257], Cml[h][0], [Cml[h][1]], [CmlR[h][1]], eng="act")

            yield

        def ret_gen(c):
            vvs = [rv[c][0][:, h * 256:(h + 1) * 256] for h in H4]
            if own:
                qrT, BqrT = qrTs[c]
                krT, BkrT = krTs[c]
                pq, Bpq = nextp()
                pk_, Bpk = nextp()
                for h in H4:
                    S.op("pe", lambda hd, o=pq[:, h * 128:(h + 1) * 128], i=rq[c][0][:, h * 128:(h + 1) * 128]: hd.transpose(o, i, ident),
                         [rq[c][1], Bcst], [Bpq])
                yield
                for h in H4:
                    S.op("pe", lambda hd, o=pk_[:, h * 128:(h + 1) * 128], i=rk[c][0][:, h * 128:(h + 1) * 128]: hd.transpose(o, i, ident),
                         [rk[c][1], Bcst], [Bpk])
                yield
                cp(qrT, pq, [Bpq], [BqrT], eng="act")
                cp(krT, pk_, [Bpk], [BkrT], eng="act")
                pS = [nextp() for h in H4]
                for h in H4:
                    mm(pS[h][0][:, 0:128], krT[:, h * 128:(h + 1) * 128], qrT[:, h * 128:(h + 1) * 128], True, True, [BkrT, BqrT], [pS[h][1]])
                yield
                for h in H4:
                    tt(PTs[h][0], pS[h][0][:, 0:128], cst[:, C_DECT + h * 128:C_DECT + (h + 1) * 128], ALU.mult, [pS[h][1], Bcst], [PTs[h][1]])
                yield
                for h in H4:
                    tt(qdTs[h][0], qrT[:, h * 128:(h + 1) * 128].bitcast(F32), cst[:, C_QD + h * 128:C_QD + (h + 1) * 128], ALU.mult,
                       [BqrT, Bcst], [qdTs[h][1]])
                yield
                pI = [nextp() for h in H4]
                for h in H4:
                    mm(pI[h][0][:, 0:256], PTs[h][0], vvs[h], True, False, [PTs[h][1], rv[c][1]], [pI[h][1]])
                    mm(pI[h][0][:, 0:256], qdTs[h][0], CretR[h][0], False, True, [qdTs[h][1], CretR[h][1]], [pI[h][1]])
                yield
                for h in H4:
                    cp(hhs[h][0], pI[h][0][:, 0:256], [pI[h][1]], [hhs[h][1]], eng="act")
                yield
                yield from ln_stages([(hhs[h][0], hhs[h][1], sths[h][0], sths[h][1]) for h in H4])
                for h in H4:
                    gsl = rg[c][0][:, h * 256:(h + 1) * 256]
                    tt(gsl, gsl, hhs[h][0], ALU.mult, [rg[c][1], hhs[h][1]], [rg[c][1]])
            for h in H4:
                ts(kws[h][0], rk[c][0][:, h * 128:(h + 1) * 128], cst[:, C_KDEC + h:C_KDEC + h + 1], None, ALU.mult, None,
                   [rk[c][1], Bcst], [kws[h][1]])
            yield
            pC = [nextp() for h in H4]
            for h in H4:
                mm(pC[h][0][:, 0:256], kws[h][0], vvs[h], True, True, [kws[h][1], rv[c][1]], [pC[h][1]])
            yield
            for h in H4:
                stt(Cret[h][0], Cret[h][0], GAM[h] ** 128, pC[h][0][:, 0:256], ALU.mult, ALU.add, [Cret[h][1], pC[h][1]], [Cret[h][1]])
            yield
            for h in H4:
                cp(CretR[h][0], Cret[h][0], [Cret[h][1]], [CretR[h][1]], eng="act")

            yield

        for c in range(2):
            for g_ in (ml_gen(c), ret_gen(c)):
                for _ in g_:
                    pass

        if not own:
            continue
        for c in range(2):
            for (src, Bsrc, dst3, Bdst, gT) in ((oml[c][0], oml[c][1], hmT3, BhmT, mlgT), (rg[c][0], rg[c][1], hrT3, BhrT, retgT)):
                for hf in range(2):
                    pb, Bp = nextp()
                    for k4 in range(4):
                        kc = hf * 4 + k4
                        S.op("pe", lambda hd, o=pb[:, k4 * 128:(k4 + 1) * 128], i=src[:, kc * 128:(kc + 1) * 128]: hd.transpose(o, i, ident),
                             [Bsrc, Bcst], [Bp])
                    for k4 in range(4):
                        kc = hf * 4 + k4
                        act(dst3[:, kc, c * 128:(c + 1) * 128], pb[:, k4 * 128:(k4 + 1) * 128], AF.Copy, [Bp, Bsm], [Bdst],
                            scale=gT[:, kc:kc + 1])
        for t in range(4):
            wm3, wmb = wload(wbml.rearrange("(k p) c -> p k c", p=128)[:, :, t * 256:(t + 1) * 256], 256)
            pbm = []
            for f in range(2):
                pb, Bp = nextp()
                for kc in range(8):
                    mm(pb[:, 0:G], wm3[:, kc, f * 128:(f + 1) * 128], hmT3[:, kc, :], kc == 0, kc == 7, [wmb, BhmT], [Bp])
                pbm.append((pb, Bp))
            wg3, wgb = win_tile(6152 + t * 256)
            for f in range(2):
                pb, Bp = nextp()
                for kc in range(8):
                    mm(pb[:, 0:G], wg3[:, kc, f * 128:(f + 1) * 128], h1T3[:, kc, :], kc == 0, kc == 7, [wgb, Bh1T], [Bp])
                act(sg1, pb[:, 0:G], AF.Sigmoid, [Bp], [Bsg1])
                tt(yT3[:, t * 2 + f, :], pbm[f][0][:, 0:G], sg1, ALU.mult, [pbm[f][1], Bsg1], [ByT])
            wr3, wrb = wload(wbret.rearrange("(k p) c -> p k c", p=128)[:, :, t * 256:(t + 1) * 256], 256)
            pbr = []
            for f in range(2):
                pb, Bp = nextp()
                for kc in range(8):
                    mm(pb[:, 0:G], wr3[:, kc, f * 128:(f + 1) * 128], hrT3[:, kc, :], kc == 0, kc == 7, [wrb, BhrT], [Bp])
                pbr.append((pb, Bp))
            wg3, wgb = win_tile(7176 + t * 256)
            for f in range(2):
                pb, Bp = nextp()
                for kc in range(8):
                    mm(pb[:, 0:G], wg3[:, kc, f * 128:(f + 1) * 128], h1T3[:, kc, :], kc == 0, kc == 7, [wgb, Bh1T], [Bp])
                act(sg1, pb[:, 0:G], AF.Sigmoid, [Bp], [Bsg1])
                tt(sg2, pbr[f][0][:, 0:G], sg1, ALU.mult, [pbr[f][1], Bsg1], [Bsg2])
                tt(yT3[:, t * 2 + f, :], yT3[:, t * 2 + f, :].bitcast(F32), sg2, ALU.add, [ByT, Bsg2], [ByT])
        for t in range(4):
            wo3, wob = wload(wout.rearrange("(k p) c -> p k c", p=128)[:, :, t * 256:(t + 1) * 256], 256)
            for c in range(2):
                pb, Bp = nextp()
                for kc in range(8):
                    mm(pb[:, 0:256], yT3[:, kc, c * 128:(c + 1) * 128], wo3[:, kc, :], kc == 0, kc == 7, [ByT, wob], [Bp])
                xsl = xg[c][0][:, t * 256:(t + 1) * 256]
                tt(sg2, pb[:, 0:256], gmb[:, t * 256:(t + 1) * 256], ALU.mult, [Bp, Bgmb], [Bsg2])
                tt(xsl, xsl, sg2, ALU.add, [xg[c][1], Bsg2], [xg[c][1]])
        for c in range(2):
            dma("sp", x2s[t0 + c * 128:t0 + (c + 1) * 128, :], xg[c][0], [xg[c][1]], [], x2sem)

    S.barrier()
    top[0] = PERS_END
    BLK = 256
    NJ = BLK // 128
    NBK = (TOK * 4) // BLK + NE
    NSLOT = NBK * BLK
    Xs = nc.dram_tensor("Xs", [NSLOT, D], F32, kind="Internal").ap()
    Ys = nc.dram_tensor("Ys", [NSLOT, D], F32, kind="Internal").ap()
    H2 = nc.dram_tensor("H2", [TOK, D], F32, kind="Internal").ap()
    A2bc, BA2bc = T(D, name="A2bc")
    B2bc, BB2bc = T(D, name="B2bc")
    xc, Bxc = T(D, name="xc")
    xs2, Bxs2 = T(D, name="xs2")
    h2tm, Bh2tm = T(D, name="h2tm")
    h2Tc, Bh2Tc = T(D, F32R, "h2Tc")
    h2Tc3 = h2Tc.rearrange("p (k n) -> p k n", n=128)
    st2, Bst2 = T(16, name="st2")
    lgt, Blgt = T(NE, name="lgt")
    t8, Bt8 = T(16, name="t8")
    maskall, Bmask = T(16 * NE, name="maskall")
    Gwall, BGw = T(16 * NE, name="Gwall")
    tris, Btris = T(128, name="tristrict")
    cntb, Bcnt = T(NE, name="cnt")
    nbi, Bnbi = T(NE, I32, "nbi")
    nbf, Bnbf = T(NE, name="nbf")
    bend, Bbend = T(NE, name="bend")
    sbase, Bsbase = T(NE, name="sbase")
    ones32, Bones32 = T(NE, name="ones32")
    key, Bkey = T(NE, name="key")
    eqt, Beqt = T(NE, name="eqt")
    s4, Bs4 = T(16, name="s4")
    sidxf, Bsidxf = T(64, name="sidxf")
    w4, Bw4 = T(64, name="w4")
    ebrow, Beb = T(NBK, name="ebrow")
    widxf, Bwidxf = T(NBK * 8, name="widxf")
    didxf, Bdidxf = T(NBK * 4, name="didxf")
    bidxf, Bbidxf = T(NBK * 2, name="bidxf")
    Xtm, BXtm = T(4 * D, name="Xtm")
    Xtm3 = Xtm.rearrange("p (j c) -> p j c", c=D)
    XT, BXT = T(8 * BLK, F32R, "XT")
    XT3 = XT.rearrange("p (k n) -> p k n", n=BLK)
    actT, BactT = T(8 * BLK, F32R, "actT")
    actT3 = actT.rearrange("p (k n) -> p k n", n=BLK)
    NU = 3
    wgt = [T(8 * 256, F32R, "wgu%d" % i) for i in range(NU)]
    wgsem = [S.dmasem("wgu%d" % i) for i in range(NU)]
    wq = [T(2 * 1024, F32R, "wd%d" % q) for q in range(4)]
    wq3 = [wq[q][0].rearrange("p (k n) -> p k n", n=1024) for q in range(4)]
    wqsem = [S.dmasem("wd%d" % q) for q in range(4)]
    bgt = [T(16, name="bgt%d" % i) for i in range(2)]
    bgsem = [S.dmasem("bgt%d" % i) for i in range(2)]
    bdb = [T(D, name="bdb%d" % i) for i in range(2)]
    bdsem = [S.dmasem("bdb%d" % i) for i in range(2)]
    gm_ = [T(512, name="gm%d" % i) for i in range(2)]
    sg_ = [T(512, name="sg%d" % i) for i in range(2)]
    lm_ = [T(512, name="lm%d" % i) for i in range(2)]
    dsb = [T(D, name="dsb%d" % i) for i in range(2)]
    acc, Bacc = T(D, name="acc")
    xcsem = S.dmasem("xc")
    h2sem = S.dmasem("h2st")
    h2lsem = S.dmasem("h2ld")
    scsem = S.dmasem("scat")
    xtsem = S.dmasem("xtm")
    yssem = S.dmasem("ysst")
    ygsem = S.dmasem("ygat")
    osem = S.dmasem("ost")
    uctr = [0]
    sctr = [0]
    wguh = wgu.rearrange("e (u q) c -> (e u q) c", u=8)
    wdh = wd.rearrange("e (q r) c -> (e q r) c", q=4)

    def ind(ap_):
        return bass.IndirectOffsetOnAxis(ap=ap_, axis=0)

    _bregs = {}

    def bnd(h, v):
        if v not in _bregs:
            _bregs[v] = h.to_reg(v)
        return _bregs[v]

    NIT = 40
    itl = [T(1, I32, "idx%d" % i) for i in range(NIT)]
    ictr = [0]

    def idx_tile(colap, Bsrc):
        i = ictr[0] % NIT
        ictr[0] += 1
        ap_, b_ = itl[i]
        cp(ap_, colap, [Bsrc], [b_])
        return ap_, b_

    def block_idx(b):
        d = {}
        d["bg"] = idx_tile(bidxf[:, b:b + 1], Bbidxf)
        d["bd"] = idx_tile(bidxf[:, NBK + b:NBK + b + 1], Bbidxf)
        for u in range(8):
            d["w%d" % u] = idx_tile(widxf[:, b * 8 + u:b * 8 + u + 1], Bwidxf)
        for q in range(4):
            d["d%d" % q] = idx_tile(didxf[:, b * 4 + q:b * 4 + q + 1], Bdidxf)
        return d

    def bcast_cols(dst, Bdst, colap, Bcol):
        for hf in range(2):
            pb, Bp = nextp()
            for k4 in range(4):
                kc = hf * 4 + k4
                ts(dgt, ident, colap[:, kc:kc + 1], None, ALU.mult, None, [Bcst, Bcol], [Bdgt])
                mm(pb[:, k4 * 128:(k4 + 1) * 128], ones[:, 0:128], dgt, True, True, [Bcst, Bdgt], [Bp])
            cp(dst[:, hf * 512:(hf + 1) * 512], pb, [Bp], [Bdst])
    dgt, Bdgt = T(128, name="dgt2")
    bcast_cols(A2bc, BA2bc, A2, BAB)
    bcast_cols(B2bc, BB2bc, B2, BAB)
    tt(tris, tri, ident, ALU.subtract, [Bcst], [Btris])
    S.op("dve", lambda h: h.memset(ones32, 1.0), [], [Bones32])
    wrt3 = wrt.rearrange("p (k n) -> p k n", n=NE)

    for c in range(16):
        dma("sp", xc, x2s[c * 128:(c + 1) * 128, :], [], [Bxc], xcsem)
        act(xs2, xc, AF.Square, [Bxc], [Bxs2, Bst2], accum=st2[:, 0:1])
        act(st2[:, 1:2], st2[:, 0:1], AF.Sqrt, [Bst2, Bsm], [Bst2], bias=epsc, scale=1.0 / D)
        S.op("dve", lambda h: h.reciprocal(st2[:, 2:3], st2[:, 1:2]), [Bst2], [Bst2])
        ts(xs2, xc, st2[:, 2:3], None, ALU.mult, None, [Bxc, Bst2], [Bxs2])
        tt(h2tm, xs2, A2bc, ALU.mult, [Bxs2, BA2bc], [Bh2tm])
        tt(h2tm, h2tm, B2bc, ALU.add, [Bh2tm, BB2bc], [Bh2tm])
        dma("sp", H2[c * 128:(c + 1) * 128, :], h2tm, [Bh2tm], [], h2sem)
        for hf in range(2):
            pb, Bp = nextp()
            for k4 in range(4):
                kc = hf * 4 + k4
                S.op("pe", lambda h, o=pb[:, k4 * 128:(k4 + 1) * 128], i=h2tm[:, kc * 128:(kc + 1) * 128]: h.transpose(o, i, ident),
                     [Bh2tm, Bcst], [Bp])
            cp(h2Tc[:, hf * 512:(hf + 1) * 512], pb, [Bp], [Bh2Tc], eng="act")
        pl, Bpl = nextp()
        for kc in range(8):
            mm(pl[:, 0:NE], h2Tc3[:, kc, :], wrt3[:, kc, :], kc == 0, kc == 7, [Bh2Tc, Bwrt], [Bpl])
        tt(lgt, pl[:, 0:NE], brb, ALU.add, [Bpl, Bbrb], [Blgt])
        S.op("dve", lambda h: h.max(t8[:, 0:8], lgt), [Blgt], [Bt8])
        ts(maskall[:, c * NE:(c + 1) * NE], lgt, t8[:, 3:4], None, ALU.is_ge, None, [Blgt, Bt8], [Bmask])
        ts(t8[:, 8:9], t8[:, 0:1], -1.0, None, ALU.mult, None, [Bt8], [Bt8])
        act(lgt, lgt, AF.Exp, [Blgt, Bt8], [Blgt], bias=t8[:, 8:9])
        tt(lgt, lgt, maskall[:, c * NE:(c + 1) * NE], ALU.mult, [Blgt, Bmask], [Blgt])
        S.op("dve", lambda h: h.reduce_sum(t8[:, 9:10], lgt, mybir.AxisListType.X), [Blgt], [Bt8])
        S.op("dve", lambda h: h.reciprocal(t8[:, 10:11], t8[:, 9:10]), [Bt8], [Bt8])
        ts(Gwall[:, c * NE:(c + 1) * NE], lgt, t8[:, 10:11], None, ALU.mult, None, [Blgt, Bt8], [BGw])

    pcn, Bpcn = nextp()
    for c in range(16):
        mm(pcn[:, 0:NE], ones[:, 0:128], maskall[:, c * NE:(c + 1) * NE], c == 0, c == 15, [Bcst, Bmask], [Bpcn])
    cp(cntb, pcn[:, 0:NE], [Bpcn], [Bcnt])
    ts(nbi, cntb, 1.0 / BLK, (BLK - 1.0) / BLK - 0.49951171875, ALU.mult, ALU.add, [Bcnt], [Bnbi])
    cp(nbf, nbi, [Bnbi], [Bnbf])
    S.op("dve", lambda h: h.tensor_tensor_scan(bend, ones32, nbf, 0.0, ALU.mult, ALU.add), [Bones32, Bnbf], [Bbend])
    tt(sbase, bend, nbf, ALU.subtract, [Bbend, Bnbf], [Bsbase])
    ts(sbase, sbase, float(BLK), None, ALU.mult, None, [Bsbase], [Bsbase])
    iob = cst[:, C_IOB:C_IOB + NBK]
    S.op("dve", lambda h: h.memset(ebrow, 0.0), [], [Beb])
    for e in range(NE):
        stt(ebrow, iob, bend[:, e:e + 1], ebrow, ALU.is_ge, ALU.add, [Bcst, Bbend, Beb], [Beb])
    ts(ebrow, ebrow, float(NE - 1), None, ALU.min, None, [Beb], [Beb])
    widxf3 = widxf.rearrange("p (b u) -> p b u", u=8)
    for u in range(8):
        ts(widxf3[:, :, u], ebrow, 1024.0, cst[:, C_CU + u:C_CU + u + 1], ALU.mult, ALU.add, [Beb, Bcst], [Bwidxf])
    didxf3 = didxf.rearrange("p (b q) -> p b q", q=4)
    for q in range(4):
        ts(didxf3[:, :, q], ebrow, 512.0, cst[:, C_CU + q:C_CU + q + 1], ALU.mult, ALU.add, [Beb, Bcst], [Bdidxf])
    ts(bidxf[:, 0:NBK], ebrow, 128.0, cst[:, C_CU:C_CU + 1], ALU.mult, ALU.add, [Beb, Bcst], [Bbidxf])
    cp(bidxf[:, NBK:2 * NBK], ebrow, [Beb], [Bbidxf])

    prk, Bprk = nextp()
    for c in range(16):
        for c2 in range(c):
            mm(prk[:, c * NE:(c + 1) * NE], ones[:, 0:128], maskall[:, c2 * NE:(c2 + 1) * NE], c2 == 0, False, [Bcst, Bmask], [Bprk])
        mm(prk[:, c * NE:(c + 1) * NE], tris, maskall[:, c * NE:(c + 1) * NE], c == 0, True, [Btris, Bmask], [Bprk])
    for c in range(16):
        mk = maskall[:, c * NE:(c + 1) * NE]
        tt(key, prk[:, c * NE:(c + 1) * NE], sbase, ALU.add, [Bprk, Bsbase], [Bkey])
        ts(key, key, 1.0, None, ALU.add, None, [Bkey], [Bkey])
        tt(key, key, mk, ALU.mult, [Bkey, Bmask], [Bkey])
        S.op("dve", lambda h: h.max(s4[:, 0:8], key), [Bkey], [Bs4])
        ts(sidxf[:, c * 4:(c + 1) * 4], s4[:, 0:4], -1.0, None, ALU.add, None, [Bs4], [Bsidxf])
        for k in range(4):
            ts(eqt, key, s4[:, k:k + 1], None, ALU.is_equal, None, [Bkey, Bs4], [Beqt])
            tt(eqt, eqt, Gwall[:, c * NE:(c + 1) * NE], ALU.mult, [Beqt, BGw], [Beqt])
            S.op("dve", lambda h, o=w4[:, c * 4 + k:c * 4 + k + 1]: h.reduce_sum(o, eqt, mybir.AxisListType.X), [Beqt], [Bw4])
    lasth2 = S.last[("dma", id(h2sem))]
    for c in range(16):
        S.op("sp", lambda h, c=c: h.dma_start(out=h2tm, in_=H2[c * 128:(c + 1) * 128, :]), [], [Bh2tm], dma=h2lsem, extra=[lasth2])
        for k in range(4):
            ia, Bia = idx_tile(sidxf[:, c * 4 + k:c * 4 + k + 1], Bsidxf)
            S.op("pool", lambda h, ia=ia: h.indirect_dma_start(
                out=Xs[:, :], out_offset=ind(ia), in_=h2tm, in_offset=None, bounds_check=bnd(h, NSLOT - 1), oob_is_err=False),
                [Bh2tm, Bia], [], dma=scsem)

    lastsc = S.last[("dma", id(scsem))]
    nxt_ix = block_idx(0)
    for b in range(NBK):
        bix = nxt_ix
        if b + 1 < NBK:
            nxt_ix = block_idx(b + 1)
        if b == 0:
            S.op("sp", lambda h, b=b: h.dma_start(out=Xtm3[:, 0:NJ, :], in_=Xs[b * BLK:(b + 1) * BLK, :].rearrange("(j p) c -> p j c", p=128)),
                 [], [BXtm], dma=xtsem, extra=[lastsc])
        for j in range(NJ):
            for hf in range(2):
                pb, Bp = nextp()
                for k4 in range(4):
                    kc = hf * 4 + k4
                    S.op("pe", lambda h, o=pb[:, k4 * 128:(k4 + 1) * 128], i=Xtm3[:, j, kc * 128:(kc + 1) * 128]: h.transpose(o, i, ident),
                         [BXtm, Bcst], [Bp])
                for k4 in range(4):
                    kc = hf * 4 + k4
                    cp(XT3[:, kc, j * 128:(j + 1) * 128], pb[:, k4 * 128:(k4 + 1) * 128], [Bp], [BXT], eng="act")
        if b + 1 < NBK:
            S.op("sp", lambda h, b=b + 1: h.dma_start(out=Xtm3[:, 0:NJ, :], in_=Xs[b * BLK:(b + 1) * BLK, :].rearrange("(j p) c -> p j c", p=128)),
                 [], [BXtm], dma=xtsem, extra=[lastsc])
        bi = b % 2
        ia, Bia = bix["bg"]
        S.op("pool", lambda h, o=bgt[bi][0], ia=ia: h.indirect_dma_start(
            out=o, out_offset=None, in_=bguT_d[:, :], in_offset=ind(ia), bounds_check=bnd(h, NE * 128 - 1), oob_is_err=False),
            [Bia], [bgt[bi][1]], dma=bgsem[bi])
        ia, Bia = bix["bd"]
        S.op("pool", lambda h, o=bdb[bi][0], ia=ia: h.indirect_dma_start(
            out=o, out_offset=None, in_=bd_d[:, :], in_offset=ind(ia), bounds_check=bnd(h, NE - 1), oob_is_err=False),
            [Bia], [bdb[bi][1]], dma=bdsem[bi])
        for u in range(8):
            i = uctr[0] % NU
            uctr[0] += 1
            wv, wb = wgt[i]
            w3 = wv.rearrange("p (k n) -> p k n", n=256)
            ia, Bia = bix["w%d" % u]
            S.op("pool", lambda h, o=wv, ia=ia: h.indirect_dma_start(
                out=o, out_offset=None, in_=wguh[:, :], in_offset=ind(ia), bounds_check=bnd(h, NE * 1024 - 1), oob_is_err=False),
                [Bia], [wb], dma=wgsem[i])
            pg, Bpg = nextp()
            pg = pg[:, 0:BLK]
            for kc in range(8):
                mm(pg, w3[:, kc, 0:128], XT3[:, kc, :], kc == 0, kc == 7, [wb, BXT], [Bpg])
            plin, Bplin = nextp()
            plin = plin[:, 0:BLK]
            for kc in range(8):
                mm(plin, w3[:, kc, 128:256], XT3[:, kc, :], kc == 0, kc == 7, [wb, BXT], [Bplin])
            si = sctr[0] % 2
            sctr[0] += 1
            gmv, Bgm = gm_[si]; sgv, Bsg = sg_[si]; lmv, Blm = lm_[si]
            gmv = gmv[:, 0:BLK]; sgv = sgv[:, 0:BLK]; lmv = lmv[:, 0:BLK]
            bg = bgt[bi][0]
            ts(gmv, pg, bg[:, u * 2:u * 2 + 1], 7.0, ALU.add, ALU.min, [Bpg, bgt[bi][1]], [Bgm])
            act(sgv, gmv, AF.Sigmoid, [Bgm], [Bsg], scale=1.702)
            act(lmv, plin, AF.Identity, [Bplin, bgt[bi][1]], [Blm], bias=bg[:, u * 2 + 1:u * 2 + 2])
            ts(lmv, lmv, 7.0, -7.0, ALU.min, ALU.max, [Blm], [Blm])
            tt(gmv, gmv, sgv, ALU.mult, [Bgm, Bsg], [Bgm])
            stt(actT3[:, u, :], lmv, 1.0, gmv, ALU.add, ALU.mult, [Blm, Bgm], [BactT])
        for q in range(4):
            ia, Bia = bix["d%d" % q]
            S.op("pool", lambda h, o=wq[q][0], ia=ia: h.indirect_dma_start(
                out=o, out_offset=None, in_=wdh[:, :], in_offset=ind(ia), bounds_check=bnd(h, NE * 512 - 1), oob_is_err=False),
                [Bia], [wq[q][1]], dma=wqsem[q])
        for j in range(NJ):
            dv, Bd = dsb[j % 2]
            for nt in range(2):
                pd_, Bpd = nextp()
                for ft in range(8):
                    mm(pd_, actT3[:, ft, j * 128:(j + 1) * 128], wq3[ft // 2][:, ft % 2, nt * 512:(nt + 1) * 512], ft == 0, ft == 7,
                       [BactT, wq[ft // 2][1]], [Bpd])
                tt(dv[:, nt * 512:(nt + 1) * 512], pd_, bdb[bi][0][:, nt * 512:(nt + 1) * 512], ALU.add, [Bpd, bdb[bi][1]], [Bd])
            dma("sp", Ys[b * BLK + j * 128:b * BLK + (j + 1) * 128, :], dv, [Bd], [], yssem)

    lastys = S.last[("dma", id(yssem))]
    for c in range(16):
        dma("sp", xc, x2s[c * 128:(c + 1) * 128, :], [], [Bxc], xcsem)
        for k in range(4):
            ia, Bia = idx_tile(sidxf[:, c * 4 + k:c * 4 + k + 1], Bsidxf)
            S.op("pool", lambda h, o=Xtm3[:, k, :], ia=ia: h.indirect_dma_start(
                out=o, out_offset=None, in_=Ys[:, :], in_offset=ind(ia), bounds_check=bnd(h, NSLOT - 1), oob_is_err=False),
                [Bia], [BXtm], dma=ygsem, extra=[lastys])
        ts(acc, Xtm3[:, 0, :], w4[:, c * 4:c * 4 + 1], None, ALU.mult, None, [BXtm, Bw4], [Bacc])
        for k in range(1, 4):
            stt(acc, Xtm3[:, k, :], w4[:, c * 4 + k:c * 4 + k + 1], acc, ALU.mult, ALU.add, [BXtm, Bw4, Bacc], [Bacc])
        tt(acc, acc, gfb, ALU.mult, [Bacc, Bgfb], [Bacc])
        tt(xc, xc, acc, ALU.add, [Bxc, Bacc], [Bxc])
        act(xs2, xc, AF.Square, [Bxc], [Bxs2, Bst2], accum=st2[:, 0:1])
        act(st2[:, 1:2], st2[:, 0:1], AF.Sqrt, [Bst2, Bsm], [Bst2], bias=epsc, scale=1.0 / D)
        S.op("dve", lambda h: h.reciprocal(st2[:, 2:3], st2[:, 1:2]), [Bst2], [Bst2])
        stt(xs2, xc, st2[:, 2:3], gfin, ALU.mult, ALU.mult, [Bxc, Bst2, Bgfin], [Bxs2])
        dma("sp", out[c * 128:(c + 1) * 128, :], xs2, [Bxs2], [], osem)

    print("arena cols: pers", PERS_END, "mix", MIX_END, "moe", top[0], "ops", len(S.ops))
    S.emit(nc, es, final_waits=[osem, x2sem, yssem, h2sem, scsem])
    es.close()
    return nc


def _consts():
    c = np.zeros((128, NC), np.float64)
    idx = np.arange(128)
    c[:, C_ID:C_ID + 128] = np.eye(128)
    s = idx[:, None]; j = idx[None, :]
    c[:, C_TRI:C_TRI + 128] = (s <= j)
    c[:, C_MNEG:C_MNEG + 128] = np.where(s <= j, 0.0, -30000.0)
    for h in range(4):
        c[h, C_SEL + h * 128:C_SEL + (h + 1) * 128] = 1.0
        lg = math.log(1.0 - 2.0 ** (-5.0 - h))
        c[:, C_DECT + h * 128:C_DECT + (h + 1) * 128] = np.where(j >= s, np.exp(lg * np.maximum(j - s, 0)), 0.0) * 128.0 ** -0.5
        c[:, C_QD + h * 128:C_QD + (h + 1) * 128] = np.exp(lg * (j + 1.0)) * np.ones((128, 1))
        c[:, C_KDEC + h] = np.exp(lg * (127.0 - idx)) * 128.0 ** -0.5
    c[:, C_ONES:C_ONES + 512] = 1.0
    inv = (10000.0 ** (-np.arange(64, dtype=np.float32) / np.float32(64))).astype(np.float32)
    c[:, C_INVF:C_INVF + 256] = np.tile(inv, 4)[None, :]
    c[:, C_IOB:C_IOB + 64] = np.arange(64)[None, :]
    c[:, C_CU:C_CU + 8] = np.arange(8)[None, :] * 128 + idx[:, None]
    return c.astype(np.float32)


_NC_CACHE = {}


def _colT(v, n):
    return np.ascontiguousarray(np.asarray(v, np.float32).reshape(n, 128).T)


def kernel(x, c, positions, w_ada, b_ada, norm_mix_g, w_in, conv_w, conv_b, b_if, ml_norm_g, ret_norm_g,
           w_branch_ml, w_branch_ret, w_out, norm_ffn_g, w_router, b_router, w_gate_up, b_gate_up, w_down, b_down,
           norm_final_g, _debug=False):
    f = lambda a: np.ascontiguousarray(np.asarray(a, np.float32))
    x = f(x); c = f(c)
    positions = np.asarray(positions).astype(np.int32)
    key = bool(_debug)
    if key not in _NC_CACHE:
        _NC_CACHE[key] = build_nc(debug=key)
    nc = _NC_CACHE[key]
    wgu_p = f(w_gate_up)[0].reshape(NE, 8, 128, 8, 128, 2)
    wgu_p = np.ascontiguousarray(wgu_p.transpose(0, 3, 2, 1, 5, 4)).reshape(NE, D, 2 * D)
    wd_p = f(w_down)[0].reshape(NE, 4, 2, 128, D)
    wd_p = np.ascontiguousarray(wd_p.transpose(0, 1, 3, 2, 4)).reshape(NE, D // 2, 2 * D)
    bgu = f(b_gate_up)[0].reshape(NE, D, 2)
    bguT = np.stack([bgu[..., 0].reshape(NE, 8, 128), bgu[..., 1].reshape(NE, 8, 128)], axis=2)
    bguT = np.ascontiguousarray(bguT.transpose(0, 3, 1, 2).reshape(NE * 128, 16))
    shared = {
        "cst": _consts(),
        "w_ada": f(w_ada)[0], "b_adaT": _colT(f(b_ada)[0], 48),
        "gmixT": _colT(f(norm_mix_g)[0], 8), "gffnT": _colT(f(norm_ffn_g)[0], 8), "gfin": f(norm_final_g).reshape(1, D),
        "w_in": f(w_in)[0],
        "convwT": np.ascontiguousarray(f(conv_w)[0].T.reshape(8, 128, 4).transpose(1, 0, 2).reshape(128, 32)),
        "convbT": _colT(f(conv_b)[0], 8), "bif": f(b_if)[0].reshape(1, 8),
        "mlgT": _colT(f(ml_norm_g)[0], 8), "retgT": _colT(f(ret_norm_g)[0], 8),
        "wbml": f(w_branch_ml)[0], "wbret": f(w_branch_ret)[0], "wout": f(w_out)[0],
        "wr": f(w_router)[0], "br": f(b_router)[0].reshape(1, NE),
        "wgu": wgu_p, "bguT": bguT, "wd": wd_p, "bd": f(b_down)[0],
    }
    in_maps = []
    for i in range(8):
        b, half = i // 2, i % 2
        m = dict(shared)
        m["xo"] = np.ascontiguousarray(x[b, half * TOK:(half + 1) * TOK])
        m["xp"] = np.ascontiguousarray(x[b, 0:TOK])
        m["poso"] = np.ascontiguousarray(positions[b, half * TOK:(half + 1) * TOK].reshape(16, 128).T)
        m["posp"] = np.ascontiguousarray(positions[b, 0:TOK].reshape(16, 128).T)
        m["flag"] = np.full((128, 1), float(half), np.float32)
        m["cT"] = _colT(c[b], 8)
        in_maps.append(m)
    res = run_bass_kernel_spmd(nc, in_maps, core_ids=list(range(8)))
    outp = np.zeros((4, SEQ, D), np.float32)
    for i in range(8):
        b, half = i // 2, i % 2
        outp[b, half * TOK:(half + 1) * TOK] = res.results[i]["out"]
    if _debug:
        dbg = np.zeros((4, SEQ, D), np.float32)
        for i in range(8):
            b, half = i // 2, i % 2
            dbg[b, half * TOK:(half + 1) * TOK] = res.results[i]["x2s"]
        return outp, dbg
    return outp
```

```python
import contextlib
import math
import numpy as np
import concourse.bass as bass
import concourse.mybir as mybir
from concourse.bass_utils import run_bass_kernel_spmd

F32 = mybir.dt.float32
F32R = mybir.dt.float32r
I32 = mybir.dt.int32
ALU = mybir.AluOpType
AF = mybir.ActivationFunctionType

D = 1024
SEQ = 4096
TOK = 2048
G = 256
NE = 32
EPS = 1e-5
LNSCALE = math.log(128.0 ** -0.5)
TWO_PI = 6.283185307179586

C_ID, C_TRI, C_MNEG, C_SEL, C_ONES, C_DECT, C_QD, C_KDEC, C_INVF = 0, 128, 256, 384, 896, 1408, 1920, 2432, 2436
C_IOB, C_CU = 2692, 2756
NC = 2764


class Buf:
    __slots__ = ("name", "w", "r")

    def __init__(self, name=""):
        self.name = name
        self.w = None
        self.r = {}


class DmaSem:
    __slots__ = ("sem", "value", "name")

    def __init__(self, name):
        self.name = name
        self.sem = None
        self.value = 0


class Op:
    __slots__ = ("eng", "fn", "deps", "needed", "tok", "dma")


class Sched:
    ENGS = ("pe", "dve", "act", "pool", "sp")

    def __init__(self, sync_same=True):
        self.ops = []
        self.sync_same = sync_same
        self.dmasems = []
        self.last = {}

    def dmasem(self, name):
        d = DmaSem(name)
        self.dmasems.append(d)
        return d

    def op(self, eng, fn, reads=(), writes=(), dma=None, extra=()):
        o = Op()
        o.eng, o.fn, o.dma = eng, fn, dma
        o.needed = dma is not None
        o.tok = None
        deps = {}
        for b in reads:
            if b.w is not None:
                deps[id(b.w)] = b.w
        for b in writes:
            if b.w is not None:
                deps[id(b.w)] = b.w
            for d in b.r.values():
                deps[id(d)] = d
        for d in extra:
            deps[id(d)] = d
        o.deps = []
        for d in deps.values():
            if d is o:
                continue
            if d.dma is None and d.eng == eng and (eng == "pe" or not self.sync_same):
                continue
            d.needed = True
            o.deps.append(d)
        key = ("dma", id(dma)) if dma is not None else eng
        for b in reads:
            b.r[key] = o
        for b in writes:
            b.w = o
            b.r = {}
        self.ops.append(o)
        self.last[key] = o
        return o

    def barrier(self):
        lasts = list(self.last.values())
        for e in self.ENGS:
            self.op(e, None, extra=[d for d in lasts if d.fn is not None])

    def emit(self, nc, es, final_waits=()):
        esem = {e: es.enter_context(nc.semaphore("s_" + e)) for e in self.ENGS}
        for d in self.dmasems:
            d.sem = es.enter_context(nc.semaphore("d_" + d.name))
            d.value = 0
        cnt = {e: 0 for e in self.ENGS}
        for o in self.ops:
            if o.fn is None:
                continue
            if o.dma is not None:
                o.dma.value += 16
                o.tok = (o.dma.sem, o.dma.value)
            elif o.needed:
                cnt[o.eng] += 1
                o.tok = (esem[o.eng], cnt[o.eng])
        block = es.enter_context(nc.Block())
        per = {e: [o for o in self.ops if o.eng == e] for e in self.ENGS}

        def run(e, h):
            waited = {}
            for o in per[e]:
                need = {}
                for d in o.deps:
                    s, v = d.tok
                    k = id(s)
                    if waited.get(k, 0) >= v:
                        continue
                    if k not in need or need[k][1] < v:
                        need[k] = (s, v)
                for k, (s, v) in need.items():
                    h.wait_ge(s, v)
                    waited[k] = v
                if o.fn is None:
                    continue
                ins = o.fn(h)
                if o.tok is not None:
                    ins.then_inc(o.tok[0], 16 if o.dma is not None else 1)
            if e == "sp":
                for d in final_waits:
                    h.wait_ge(d.sem, d.value)

        @block.tensor
        def _(h):
            run("pe", h)

        @block.vector
        def _(h):
            run("dve", h)

        @block.scalar
        def _(h):
            run("act", h)

        @block.gpsimd
        def _(h):
            run("pool", h)

        @block.sync
        def _(h):
            run("sp", h)


def build_nc(debug=False):
    nc = bass.Bass("TRN2", target_bir_lowering=False)

    def din(name, shape, dt=F32):
        return nc.dram_tensor(name, list(shape), dt, kind="ExternalInput").ap()

    xo = din("xo", [TOK, D]); xp = din("xp", [TOK, D])
    poso = din("poso", [128, 16], I32); posp = din("posp", [128, 16], I32)
    flag_d = din("flag", [128, 1]); cT_d = din("cT", [128, 8]); cst_d = din("cst", [128, NC])
    w_ada = din("w_ada", [D, 6 * D]); b_adaT = din("b_adaT", [128, 48])
    gmixT_d = din("gmixT", [128, 8]); gffnT_d = din("gffnT", [128, 8]); gfin_d = din("gfin", [1, D])
    w_in = din("w_in", [D, 8200]); convwT_d = din("convwT", [128, 32]); convbT_d = din("convbT", [128, 8])
    bif_d = din("bif", [1, 8]); mlgT_d = din("mlgT", [128, 8]); retgT_d = din("retgT", [128, 8])
    wbml = din("wbml", [D, D]); wbret = din("wbret", [D, D]); wout = din("wout", [D, D])
    wr_d = din("wr", [D, NE]); br_d = din("br", [1, NE])
    wgu = din("wgu", [NE, D, 2 * D]); bguT_d = din("bguT", [NE * 128, 16])
    wd = din("wd", [NE, D // 2, 2 * D]); bd_d = din("bd", [NE, D])
    out = nc.dram_tensor("out", [TOK, D], F32, kind="ExternalOutput").ap()
    x2s = nc.dram_tensor("x2s", [TOK, D], F32, kind="ExternalOutput" if debug else "Internal").ap()

    S = Sched()
    es = contextlib.ExitStack()
    NCOL = 53180
    pbanks = [es.enter_context(nc.psum_tensor("pb%d" % i, [128, 512], F32)) for i in range(8)]
    PB = [Buf("pb%d" % i) for i in range(8)]
    pctr = [0]

    def nextp():
        i = pctr[0] % 8
        pctr[0] += 1
        return pbanks[i][:, :], PB[i]

    top = [0]
    tcount = [0]

    def al(n):
        n = (n + 7) // 8 * 8
        a = top[0]
        top[0] += n
        assert top[0] <= NCOL, top[0]
        return a

    def T(n, dt=F32, name=""):
        a = al(n)
        tcount[0] += 1
        t = nc.alloc_sbuf_tensor_at("t%d_%s" % (tcount[0], name), [128, n], dt, offset=16640 + 4 * a)
        return t[:, :], Buf(name)

    def mm(o, lhsT, rhs, st, sp, rd, wr):
        S.op("pe", lambda h: h.matmul(o, lhsT=lhsT, rhs=rhs, start=st, stop=sp), rd, wr)

    def act(o, i, func, rd, wr, bias=None, scale=None, accum=None):
        kw = {}
        if bias is not None:
            kw["bias"] = bias
        if scale is not None:
            kw["scale"] = scale
        if accum is not None:
            kw["accum_out"] = accum
        S.op("act", lambda h: h.activation(o, i, func, **kw), rd, wr)

    def ts(o, i, s1, s2, op0, op1, rd, wr, eng="dve"):
        if op1 is None:
            S.op(eng, lambda h: h.tensor_scalar(o, i, s1, None, op0), rd, wr)
        else:
            S.op(eng, lambda h: h.tensor_scalar(o, i, s1, s2, op0, op1), rd, wr)

    def tt(o, a, b, op, rd, wr, eng="dve"):
        S.op(eng, lambda h: h.tensor_tensor(o, a, b, op), rd, wr)

    def stt(o, a, s, b, op0, op1, rd, wr):
        S.op("dve", lambda h: h.scalar_tensor_tensor(o, a, s, b, op0, op1), rd, wr)

    def cp(o, i, rd, wr, eng="dve"):
        if eng == "act":
            S.op(eng, lambda h: h.copy(o, i), rd, wr)
        else:
            S.op(eng, lambda h: h.tensor_copy(o, i), rd, wr)

    def dma(q, o, i, rd, wr, sem):
        S.op(q, lambda h: h.dma_start(out=o, in_=i), rd, wr, dma=sem)

    cst, Bcst = T(NC, name="cst")
    ident = cst[:, C_ID:C_ID + 128]
    tri = cst[:, C_TRI:C_TRI + 128]
    mneg = cst[:, C_MNEG:C_MNEG + 128]
    ones = cst[:, C_ONES:C_ONES + 512]
    onesR, BonesR = T(512, F32R, "onesR")
    modT, BmodT = T(48, name="modT")
    AB, BAB = T(32, name="AB")
    sm, Bsm = T(160, name="small")
    gmixT = sm[:, 0:8]; gffnT = sm[:, 8:16]; badaT = sm[:, 16:64]; convw = sm[:, 64:96]; convb = sm[:, 96:104]
    bifb = sm[:, 104:112]; flag = sm[:, 112:113]; epsc = sm[:, 113:114]; cTt = sm[:, 114:122]
    mlgT = sm[:, 122:130]; retgT = sm[:, 130:138]
    cact2, Bcact = T(16, F32R, "cact2")
    gfb, Bgfb = T(D, name="gate_f_bc")
    gfin, Bgfin = T(D, name="gfin_bc")
    wrt, Bwrt = T(8 * NE, F32R, "wr")
    brb, Bbrb = T(NE, name="br")
    posf, Bposf = T(32, name="posf")
    posi, Bposi = T(32, I32, "posi")
    PERS_END = top[0]

    dl = {n: S.dmasem(n) for n in ["c0", "c1", "c2", "c3", "c4", "c5", "c6", "c7", "c8", "c9", "c10", "c11", "c12", "c13", "c14", "c15", "c16"]}
    dma("sp", cst, cst_d[:, :], [], [Bcst], dl["c0"])
    dma("sp", gmixT, gmixT_d[:, :], [], [Bsm], dl["c1"])
    dma("sp", gffnT, gffnT_d[:, :], [], [Bsm], dl["c1"])
    dma("sp", badaT, b_adaT[:, :], [], [Bsm], dl["c1"])
    dma("sp", convw, convwT_d[:, :], [], [Bsm], dl["c1"])
    dma("sp", convb, convbT_d[:, :], [], [Bsm], dl["c1"])
    dma("sp", bifb, bif_d[0:1, :].partition_broadcast(128), [], [Bsm], dl["c1"])
    dma("sp", flag, flag_d[:, :], [], [Bsm], dl["c1"])
    dma("sp", cTt, cT_d[:, :], [], [Bsm], dl["c1"])
    dma("sp", mlgT, mlgT_d[:, :], [], [Bsm], dl["c1"])
    dma("sp", retgT, retgT_d[:, :], [], [Bsm], dl["c1"])
    S.op("dve", lambda h: h.memset(epsc, EPS), [], [Bsm])
    cp(onesR, ones, [Bcst], [BonesR])
    dma("sp", gfin, gfin_d[0:1, :].partition_broadcast(128), [], [Bgfin], dl["c2"])
    dma("pool", wrt.rearrange("p (k n) -> p k n", n=NE), wr_d.rearrange("(k p) n -> p k n", p=128), [], [Bwrt], dl["c3"])
    dma("sp", brb, br_d[0:1, :].partition_broadcast(128), [], [Bbrb], dl["c4"])
    dma("sp", posi[:, 0:16], posp[:, :], [], [Bposi], dl["c6"])
    dma("sp", posi[:, 16:32], poso[:, :], [], [Bposi], dl["c6"])
    cp(posf, posi, [Bposi], [Bposf])

    gmb, Bgmb = T(D, name="gate_m_bc")
    Cml = [T(257, F32, "Cml%d" % h) for h in range(4)]
    CmlR = [T(258, F32R, "CmlR%d" % h) for h in range(4)]
    Cret = [T(256, F32, "Cret%d" % h) for h in range(4)]
    CretR = [T(256, F32R, "CretR%d" % h) for h in range(4)]
    rowA, BrowA = T(600, name="rows")
    bTr = rowA[0:4, 0:256]; Mr = rowA[0:4, 256:512]; mpr = rowA[0:4, 512:530]; Ar = rowA[0:4, 530:532]
    ext = rowA[0:4, 532:540]
    xg = [T(D, name="xg%d" % c) for c in range(2)]
    xs, Bxs = T(D, name="xs")
    h1T, Bh1T = T(8 * G, F32R, "h1T")
    h1T3 = h1T.rearrange("p (k n) -> p k n", n=G)
    NW = 3
    wst = [T(8 * 256, F32R, "wst%d" % i) for i in range(NW)]
    wsem = [S.dmasem("wst%d" % i) for i in range(NW)]
    wctr = [0]
    qkpre, Bqkpre = T(8 * 259, name="qkpre")
    qkpre3 = qkpre.rearrange("p (f n) -> p f n", n=259)
    cacc, Bcacc = T(G, name="cacc")
    qkT, BqkT = T(8 * G, F32R, "qkT")
    qkT3 = qkT.rearrange("p (f n) -> p f n", n=G)
    vml = [T(4 * 258, F32R, "vml%d" % c) for c in range(2)]
    oml = [T(D, name="oml%d" % c) for c in range(2)]
    ifb = [T(8, name="if%d" % c) for c in range(2)]
    rq = [T(512, name="rq%d" % c) for c in range(2)]
    rk = [T(512, name="rk%d" % c) for c in range(2)]
    rv = [T(D, F32R, "rv%d" % c) for c in range(2)]
    rg = [T(D, name="rg%d" % c) for c in range(2)]
    gsm, Bgsm = T(104, name="gsm")
    NMh, BNM = T(4 * G, name="NM")
    hmT, BhmT = T(8 * G, F32R, "hmT")
    hrT, BhrT = T(8 * G, F32R, "hrT")
    yT, ByT = T(8 * G, F32R, "yT")
    hmT3 = hmT.rearrange("p (k n) -> p k n", n=G); hrT3 = hrT.rearrange("p (k n) -> p k n", n=G)
    yT3 = yT.rearrange("p (k n) -> p k n", n=G)
    DTs = [T(128, name="DT%d" % h) for h in range(4)]
    PTs = [T(128, F32R, "PT%d" % h) for h in range(4)]
    kws = [T(128, F32R, "kw%d" % h) for h in range(4)]
    inss = [T(258, name="inter_s%d" % h) for h in range(4)]
    tots = [T(258, name="tot%d" % h) for h in range(4)]
    hhs = [(inss[h][0][:, 0:256], inss[h][1]) for h in range(4)]
    sths = [T(16, name="sth%d" % h) for h in range(4)]
    qdTs = kws
    st6, Bst6 = T(16, name="stats")
    rot, Brot = T(768, name="rot")
    sg2, Bsg2 = rot[:, 0:256], Brot
    sct = [T(512, name="sincos%d" % c) for c in range(2)]
    kint, Bkint = T(256, I32, "kint")
    rtmp, Brtmp = xs, Bxs
    qrTs = [T(512, F32R, "qrT%d" % c) for c in range(2)]
    krTs = [T(512, F32R, "krT%d" % c) for c in range(2)]
    sg1, Bsg1 = cacc, Bcacc
    dgt, Bdgt = DTs[0]
    x2sem = S.dmasem("x2st")
    xsem = [S.dmasem("xld%d" % c) for c in range(2)]
    MIX_END = top[0]

    for h in range(4):
        S.op("dve", lambda hh_, a=Cml[h][0]: hh_.memset(a, 0.0), [], [Cml[h][1]])
        cp(CmlR[h][0][:, 0:257], Cml[h][0], [Cml[h][1]], [CmlR[h][1]])
        cp(CmlR[h][0][:, 257:258], Cml[h][0][:, 0:1], [Cml[h][1]], [CmlR[h][1]])
        S.op("dve", lambda hh_, a=Cret[h][0]: hh_.memset(a, 0.0), [], [Cret[h][1]])
        cp(CretR[h][0], Cret[h][0], [Cret[h][1]], [CretR[h][1]])
    S.op("dve", lambda h: h.memset(qkpre, 0.0), [], [Bqkpre])
    S.op("dve", lambda h: h.memset(rowA, 0.0), [], [BrowA])
    for c in range(2):
        for q_ in range(0, 1032, 512):
            n_ = min(512, 1032 - q_)
            cp(vml[c][0][:, q_:q_ + n_], ones[:, 0:n_], [Bcst], [vml[c][1]])

    def wload(src3, ncols):
        i = wctr[0] % NW
        wctr[0] += 1
        ap, b = wst[i]
        v3 = ap[:, 0:8 * ncols].rearrange("p (k n) -> p k n", n=ncols)
        dma("pool", v3, src3, [], [b], wsem[i])
        return v3, b

    def win_tile(c0, ncols=256):
        return wload(w_in.rearrange("(k p) c -> p k c", p=128)[:, :, c0:c0 + ncols], ncols)

    act(cact2.rearrange("p (k t) -> p k t", t=2)[:, :, 0], cTt, AF.Silu, [Bsm], [Bcact])
    act(cact2.rearrange("p (k t) -> p k t", t=2)[:, :, 1], cTt, AF.Silu, [Bsm], [Bcact])
    cact3 = cact2.rearrange("p (k t) -> p k t", t=2)
    pm, Bpm = nextp()
    wada3 = w_ada.rearrange("(k p) c -> p k c", p=128)
    for j in range(6):
        for sb_ in range(4):
            w3, wb = wload(wada3[:, :, j * D + sb_ * 256: j * D + (sb_ + 1) * 256], 256)
            for ft in range(2):
                col = (j * 8 + sb_ * 2 + ft) * 2
                for kc in range(8):
                    mm(pm[:, col:col + 2], w3[:, kc, ft * 128:(ft + 1) * 128], cact3[:, kc, :], kc == 0, kc == 7,
                       [wb, Bcact], [Bpm])
    tt(modT, pm[:, 0:96].rearrange("p (c t) -> p c t", t=2)[:, :, 0], badaT, ALU.add, [Bpm, Bsm], [BmodT])
    stt(AB[:, 0:8], modT[:, 8:16], 1.0, gmixT, ALU.add, ALU.mult, [BmodT, Bsm], [BAB])
    cp(AB[:, 8:16], modT[:, 0:8], [BmodT], [BAB])
    stt(AB[:, 16:24], modT[:, 32:40], 1.0, gffnT, ALU.add, ALU.mult, [BmodT, Bsm], [BAB])
    cp(AB[:, 24:32], modT[:, 24:32], [BmodT], [BAB])
    A1 = AB[:, 0:8]; B1 = AB[:, 8:16]; A2 = AB[:, 16:24]; B2 = AB[:, 24:32]

    def bcast_vec(dst, Bdst, col0):
        for hf in range(2):
            pb, Bp = nextp()
            for k4 in range(4):
                kc = hf * 4 + k4
                ts(dgt, ident, modT[:, col0 + kc:col0 + kc + 1], None, ALU.mult, None, [Bcst, BmodT], [Bdgt])
                mm(pb[:, k4 * 128:(k4 + 1) * 128], ones[:, 0:128], dgt, True, True, [Bcst, Bdgt], [Bp])
            cp(dst[:, hf * 512:(hf + 1) * 512], pb, [Bp], [Bdst])

    bcast_vec(gmb, Bgmb, 16)
    bcast_vec(gfb, Bgfb, 40)

    def norm_to_T(xsrc, Bx, dstT3, BdstT, c, Acol, Bcol):
        act(xs, xsrc, AF.Square, [Bx], [Bxs, Bst6], accum=st6[:, 0:1])
        act(st6[:, 1:2], st6[:, 0:1], AF.Sqrt, [Bst6, Bsm], [Bst6], bias=epsc, scale=1.0 / D)
        S.op("dve", lambda h: h.reciprocal(st6[:, 2:3], st6[:, 1:2]), [Bst6], [Bst6])
        ts(xs, xsrc, st6[:, 2:3], None, ALU.mult, None, [Bx, Bst6], [Bxs])
        for hf in range(2):
            pb, Bp = nextp()
            for k4 in range(4):
                kc = hf * 4 + k4
                S.op("pe", lambda h, o=pb[:, k4 * 128:(k4 + 1) * 128], i=xs[:, kc * 128:(kc + 1) * 128]: h.transpose(o, i, ident),
                     [Bxs, Bcst], [Bp])
            for k4 in range(4):
                kc = hf * 4 + k4
                act(dstT3[:, kc, c * 128:(c + 1) * 128], pb[:, k4 * 128:(k4 + 1) * 128], AF.Identity, [Bp, BAB], [BdstT],
                    bias=Bcol[:, kc:kc + 1], scale=Acol[:, kc:kc + 1])

    def proj_fm(c0, nft, evac):
        for t in range(nft // 2):
            w3, wb = win_tile(c0 + t * 256)
            for f in range(2):
                pb, Bp = nextp()
                for kc in range(8):
                    mm(pb[:, 0:G], w3[:, kc, f * 128:(f + 1) * 128], h1T3[:, kc, :], kc == 0, kc == 7, [wb, Bh1T], [Bp])
                evac(t * 2 + f, pb[:, 0:G], Bp)

    def proj_tm(c0, ntile, evac, ncols=256):
        for t in range(ntile):
            w3, wb = win_tile(c0 + t * ncols, ncols)
            for c in range(2):
                pb, Bp = nextp()
                for kc in range(8):
                    mm(pb[:, 0:ncols], h1T3[:, kc, c * 128:(c + 1) * 128], w3[:, kc, :], kc == 0, kc == 7, [Bh1T, wb], [Bp])
                evac(t, c, pb[:, 0:ncols], Bp)

    def ln_stages(items):
        for hv, Bh, st, Bst in items:
            S.op("dve", lambda h, st=st, hv=hv: h.bn_stats(st[:, 4:10], hv), [Bh], [Bst])
        yield
        for hv, Bh, st, Bst in items:
            S.op("dve", lambda h, st=st: h.bn_aggr(st[:, 10:12], st[:, 4:10]), [Bst], [Bst])
        yield
        for hv, Bh, st, Bst in items:
            act(st[:, 12:13], st[:, 11:12], AF.Sqrt, [Bst, Bsm], [Bst], bias=epsc, scale=1.0)
        yield
        for hv, Bh, st, Bst in items:
            S.op("dve", lambda h, st=st: h.reciprocal(st[:, 13:14], st[:, 12:13]), [Bst], [Bst])
        yield
        for hv, Bh, st, Bst in items:
            ts(hv, hv, st[:, 10:11], st[:, 13:14], ALU.subtract, ALU.mult, [Bh, Bst], [Bh])
        yield

    GAM = [1.0 - 2.0 ** (-5.0 - h) for h in range(4)]

    for g in range(16):
        own = g >= 8
        xsrc = xo if own else xp
        t0 = (g - 8) * G if own else g * G
        for c in range(2):
            dma("sp", xg[c][0], xsrc[t0 + c * 128:t0 + (c + 1) * 128, :], [], [xg[c][1]], xsem[c])
            norm_to_T(xg[c][0], xg[c][1], h1T3, Bh1T, c, A1, B1)
        ang = rot[:, 0:256]; red = rot[:, 256:512]; kf = rot[:, 512:768]
        for c in range(2):
            pcol = (16 if own else 0) + (g % 8) * 2 + c
            ts(ang, cst[:, C_INVF:C_INVF + 256], posf[:, pcol:pcol + 1], None, ALU.mult, None, [Bcst, Bposf], [Brot])
            for dst_, off in ((sct[c][0][:, 0:256], 0.0), (sct[c][0][:, 256:512], math.pi / 2)):
                ts(red, ang, off, None, ALU.add, None, [Brot], [Brot])
                ts(kint, red, 1.0 / TWO_PI, None, ALU.mult, None, [Brot], [Bkint])
                cp(kf, kint, [Bkint], [Brot])
                stt(red, kf, -TWO_PI, red, ALU.mult, ALU.add, [Brot], [Brot])
                ts(red, red, 3.14159, -3.14159, ALU.min, ALU.max, [Brot], [Brot])
                act(dst_, red, AF.Sin, [Brot], [sct[c][1]])
        if g == 8:
            ts(qkpre, qkpre, flag, None, ALU.mult, None, [Bqkpre, Bsm], [Bqkpre])
            ts(mpr[:, 0:1], mpr[:, 0:1], flag[0:4, :], None, ALU.mult, None, [BrowA, Bsm], [BrowA])
            for h in range(4):
                ts(Cml[h][0], Cml[h][0], flag, None, ALU.mult, None, [Cml[h][1], Bsm], [Cml[h][1]])
                cp(CmlR[h][0][:, 0:257], Cml[h][0], [Cml[h][1]], [CmlR[h][1]])
                ts(Cret[h][0], Cret[h][0], flag, None, ALU.mult, None, [Cret[h][1], Bsm], [Cret[h][1]])
                cp(CretR[h][0], Cret[h][0], [Cret[h][1]], [CretR[h][1]])

        need_q = own or g == 7

        def ev_qk(base):
            def f(ft, ps_, Bp):
                cp(qkpre3[:, base + ft, 3:259], ps_, [Bp], [Bqkpre], eng="act")
            return f
        proj_fm(512, 4, ev_qk(4))
        if need_q:
            proj_fm(0, 4, ev_qk(0))
        for ft in (range(8) if need_q else range(4, 8)):
            ts(cacc, qkpre3[:, ft, 0:G], convw[:, ft * 4:ft * 4 + 1], None, ALU.mult, None, [Bqkpre, Bsm], [Bcacc])
            for i in range(1, 4):
                stt(cacc, qkpre3[:, ft, i:i + G], convw[:, ft * 4 + i:ft * 4 + i + 1], cacc, ALU.mult, ALU.add,
                    [Bqkpre, Bsm, Bcacc], [Bcacc])
            act(qkT3[:, ft, :], cacc, AF.Silu, [Bcacc, Bsm], [BqkT], bias=convb[:, ft:ft + 1])
            cp(qkpre3[:, ft, 0:3], qkpre3[:, ft, G:G + 3], [Bqkpre], [Bqkpre])

        def ev_v(t, c, ps_, Bp):
            cp(vml[c][0][:, t * 258:t * 258 + 256], ps_, [Bp], [vml[c][1]], eng="act")
        proj_tm(1024, 4, ev_v)

        def ev_if(t, c, ps_, Bp):
            tt(ifb[c][0], ps_, bifb, ALU.add, [Bp, Bsm], [ifb[c][1]])
        proj_tm(3072, 1, ev_if, ncols=8)
        if own:
            def ev_o(t, c, ps_, Bp):
                act(oml[c][0][:, t * 256:(t + 1) * 256], ps_, AF.Sigmoid, [Bp], [oml[c][1]])
            proj_tm(2048, 4, ev_o)

        lf = gsm[:, 0:8]; a_ = gsm[:, 8:16]; b_ = gsm[:, 16:24]; tmp8 = gsm[:, 24:32]; bb = gsm[:, 32:40]
        u_ = gsm[:, 40:48]; isc = gsm[:, 48:56]; emt = gsm[:, 56:64]; spv = gsm[:, 64:72]; MT = gsm[:, 72:80]
        MLb = gsm[:, 80:88]; MPb = gsm[:, 88:96]; tmp8b = gsm[:, 96:104]
        for c in range(2):
            fp = ifb[c][0][:, 4:8]
            act(tmp8[:, c * 4:c * 4 + 4], fp, AF.Abs, [ifb[c][1]], [Bgsm])
            act(tmp8[:, c * 4:c * 4 + 4], tmp8[:, c * 4:c * 4 + 4], AF.Exp, [Bgsm], [Bgsm], scale=-1.0)
            act(tmp8[:, c * 4:c * 4 + 4], tmp8[:, c * 4:c * 4 + 4], AF.Ln, [Bgsm], [Bgsm], bias=1.0)
            ts(lf[:, c * 4:c * 4 + 4], fp, 0.0, None, ALU.min, None, [ifb[c][1]], [Bgsm])
            tt(lf[:, c * 4:c * 4 + 4], lf[:, c * 4:c * 4 + 4], tmp8[:, c * 4:c * 4 + 4], ALU.subtract, [Bgsm], [Bgsm])
        pa, Bpa = nextp()
        mm(pa[:, 0:8], tri, lf, True, True, [Bcst, Bgsm], [Bpa])
        for c in range(2):
            mm(pa[0:4, 16 + c:17 + c], lf[:, c * 4:c * 4 + 4], ones[:, 0:1], True, True, [Bgsm, Bcst], [Bpa])
        cp(a_, pa[:, 0:8], [Bpa], [Bgsm])
        cp(Ar, pa[0:4, 16:18], [Bpa], [BrowA])
        for c in range(2):
            tt(b_[:, c * 4:c * 4 + 4], ifb[c][0][:, 0:4], a_[:, c * 4:c * 4 + 4], ALU.subtract, [ifb[c][1], Bgsm], [Bgsm])
        pt_, Bpt = nextp()
        for c in range(2):
            S.op("pe", lambda h, o=pt_[0:4, c * 128:(c + 1) * 128], i=b_[:, c * 4:c * 4 + 4]: h.transpose(o, i, ident),
                 [Bgsm, Bcst], [Bpt])
        cp(bTr, pt_[0:4, 0:256], [Bpt], [BrowA])
        for c in range(2):
            ci = (g % 8) * 2 + c if False else c
            S.op("dve", lambda h, o=Mr[:, c * 128:(c + 1) * 128], d=bTr[:, c * 128:(c + 1) * 128], ini=mpr[:, c:c + 1]:
                 h.tensor_tensor_scan(o, d, d, ini, ALU.max, ALU.max), [BrowA], [BrowA])
            tt(mpr[:, c + 1:c + 2], Ar[:, c:c + 1], Mr[:, c * 128 + 127:c * 128 + 128], ALU.add, [BrowA], [BrowA])
            cp(ext[:, c:c + 1], Mr[:, c * 128 + 127:c * 128 + 128], [BrowA], [BrowA])
            cp(ext[:, 2 + c:3 + c], mpr[:, c:c + 1], [BrowA], [BrowA])
        cp(mpr[:, 0:1], mpr[:, 2:3], [BrowA], [BrowA])
        pe_, Bpe = nextp()
        for h in range(4):
            pb, Bp = nextp()
            mm(pb[:, 0:G], cst[0:4, C_SEL + h * 128:C_SEL + (h + 1) * 128], Mr, True, True, [Bcst, BrowA], [Bp])
            for c in range(2):
                tt(NMh[:, h * G + c * 128:h * G + (c + 1) * 128], mneg, pb[:, c * 128:(c + 1) * 128], ALU.subtract,
                   [Bcst, Bp], [BNM])
            mm(pe_[:, h * 4:h * 4 + 4], cst[0:4, C_SEL + h * 128:C_SEL + (h + 1) * 128], ext[:, 0:4], True, True,
               [Bcst, BrowA], [Bpe])
        for c in range(2):
            S.op("pe", lambda h, o=pe_[:, 32 + c * 4:36 + c * 4], i=Mr[:, c * 128:(c + 1) * 128]: h.transpose(o, i, ident[0:4, 0:4]),
                 [BrowA, Bcst], [Bpe])
        cp(MT, pe_[:, 32:40], [Bpe], [Bgsm])
        pe3 = pe_[:, 0:16].rearrange("p (h f) -> p h f", f=4)
        for c in range(2):
            cp(MLb[:, c * 4:c * 4 + 4], pe3[:, :, c], [Bpe], [Bgsm])
            cp(MPb[:, c * 4:c * 4 + 4], pe3[:, :, 2 + c], [Bpe], [Bgsm])
        ts(bb, b_, LNSCALE, None, ALU.add, None, [Bgsm], [Bgsm])
        tt(tmp8, b_, MLb, ALU.subtract, [Bgsm], [Bgsm])
        act(u_, tmp8, AF.Exp, [Bgsm], [Bgsm])
        tt(tmp8, MPb, MLb, ALU.subtract, [Bgsm], [Bgsm])
        act(spv, tmp8, AF.Exp, [Bgsm], [Bgsm])
        if own:
            tt(tmp8, MPb, MT, ALU.subtract, [Bgsm], [Bgsm])
            act(isc, tmp8, AF.Exp, [Bgsm], [Bgsm], bias=LNSCALE)
            tt(tmp8b, a_, MT, ALU.add, [Bgsm], [Bgsm])
            act(emt, tmp8b, AF.Exp, [Bgsm], [Bgsm], scale=-1.0)

        def ev_rk(t, c, ps_, Bp):
            cp(rk[c][0][:, t * 256:(t + 1) * 256], ps_, [Bp], [rk[c][1]], eng="act")
        proj_tm(3592, 2, ev_rk)

        def ev_rv(t, c, ps_, Bp):
            cp(rv[c][0][:, t * 256:(t + 1) * 256], ps_, [Bp], [rv[c][1]], eng="act")
        proj_tm(4104, 4, ev_rv)
        if own:
            def ev_rq(t, c, ps_, Bp):
                cp(rq[c][0][:, t * 256:(t + 1) * 256], ps_, [Bp], [rq[c][1]], eng="act")
            proj_tm(3080, 2, ev_rq)

            def ev_rg(t, c, ps_, Bp):
                act(rg[c][0][:, t * 256:(t + 1) * 256], ps_, AF.Silu, [Bp], [rg[c][1]])
            proj_tm(5128, 4, ev_rg)

        H4 = range(4)

        def rotary(src, Bsrc, c):
            s3 = sct[c][0][:, 0:256].rearrange("p (h d) -> p h d", d=64)
            c3 = sct[c][0][:, 256:512].rearrange("p (h d) -> p h d", d=64)
            Bsc = sct[c][1]
            x4 = src.rearrange("p (h t d) -> p h t d", t=2, d=64)
            r4 = rtmp[:, 0:512].rearrange("p (h t d) -> p h t d", t=2, d=64)
            q4 = rtmp[:, 512:1024].rearrange("p (h t d) -> p h t d", t=2, d=64)
            tt(r4[:, :, 0, :], x4[:, :, 0, :], c3, ALU.mult, [Bsrc, Bsc], [Brtmp])
            tt(r4[:, :, 1, :], x4[:, :, 1, :], c3, ALU.mult, [Bsrc, Bsc], [Brtmp])
            tt(q4[:, :, 0, :], x4[:, :, 1, :], s3, ALU.mult, [Bsrc, Bsc], [Brtmp])
            tt(q4[:, :, 1, :], x4[:, :, 0, :], s3, ALU.mult, [Bsrc, Bsc], [Brtmp])
            tt(x4[:, :, 0, :], r4[:, :, 0, :], q4[:, :, 0, :], ALU.subtract, [Brtmp], [Bsrc])
            tt(x4[:, :, 1, :], r4[:, :, 1, :], q4[:, :, 1, :], ALU.add, [Brtmp], [Bsrc])
        for c in range(2):
            rotary(rk[c][0], rk[c][1], c)
            if own:
                rotary(rq[c][0], rq[c][1], c)
        def ml_gen(c):
            kTs = [qkT3[:, 4 + h, c * 128:(c + 1) * 128] for h in H4]
            qTs = [qkT3[:, h, c * 128:(c + 1) * 128] for h in H4]
            vxs = [vml[c][0][:, h * 258:h * 258 + 258] for h in H4]
            cols = [c * 4 + h for h in H4]
            if own:
                pS = [nextp() for h in H4]
                for h in H4:
                    mm(pS[h][0][:, 0:128], kTs[h], qTs[h], True, True, [BqkT], [pS[h][1]])
                yield
                for h in H4:
                    act(DTs[h][0], NMh[:, h * G + c * 128:h * G + (c + 1) * 128], AF.Exp, [BNM, Bgsm], [DTs[h][1]],
                        bias=bb[:, cols[h]:cols[h] + 1])
                yield
                for h in H4:
                    tt(PTs[h][0], pS[h][0][:, 0:128], DTs[h][0], ALU.mult, [pS[h][1], DTs[h][1]], [PTs[h][1]])
                yield
                pI = [nextp() for h in H4]
                for h in H4:
                    mm(pI[h][0][:, 0:258], PTs[h][0], vxs[h], True, True, [PTs[h][1], vml[c][1]], [pI[h][1]])
                yield
                pJ = [nextp() for h in H4]
                for h in H4:
                    mm(pJ[h][0][:, 0:258], qTs[h], CmlR[h][0], True, True, [BqkT, CmlR[h][1]], [pJ[h][1]])
                yield
                for h in H4:
                    act(inss[h][0], pJ[h][0][:, 0:258], AF.Copy, [pJ[h][1], Bgsm], [inss[h][1]], scale=isc[:, cols[h]:cols[h] + 1])
                yield
                for h in H4:
                    tt(tots[h][0], pI[h][0][:, 0:258], inss[h][0], ALU.add, [pI[h][1], inss[h][1]], [tots[h][1]])
                yield
                for h in H4:
                    act(sths[h][0][:, 14:15], tots[h][0][:, 256:257], AF.Abs, [tots[h][1]], [sths[h][1]])
                yield
                for h in H4:
                    ts(sths[h][0][:, 14:15], sths[h][0][:, 14:15], emt[:, cols[h]:cols[h] + 1], None, ALU.max, None,
                       [sths[h][1], Bgsm], [sths[h][1]])
                yield
                for h in H4:
                    S.op("dve", lambda hd, st=sths[h][0]: hd.reciprocal(st[:, 15:16], st[:, 14:15]), [sths[h][1]], [sths[h][1]])
                yield
                for h in H4:
                    ts(hhs[h][0], tots[h][0][:, 0:256], sths[h][0][:, 15:16], None, ALU.mult, None, [tots[h][1], sths[h][1]], [hhs[h][1]])
                yield
                yield from ln_stages([(hhs[h][0], hhs[h][1], sths[h][0], sths[h][1]) for h in H4])
                for h in H4:
                    osl = oml[c][0][:, h * 256:(h + 1) * 256]
                    tt(osl, osl, hhs[h][0], ALU.mult, [oml[c][1], hhs[h][1]], [oml[c][1]])
            pK = [nextp() for h in H4]
            for h in H4:
                S.op("pe", lambda hd, o=pK[h][0][:, 0:128], i=kTs[h].bitcast(F32): hd.transpose(o, i, ident), [BqkT, Bcst], [pK[h][1]])
            yield
            for h in H4:
                act(kws[h][0], pK[h][0][:, 0:128], AF.Copy, [pK[h][1], Bgsm], [kws[h][1]], scale=u_[:, cols[h]:cols[h] + 1])
            yield
            pC = [nextp() for h in H4]
            for h in H4:
                mm(pC[h][0][:, 0:258], kws[h][0], vxs[h], True, True, [kws[h][1], vml[c][1]], [pC[h][1]])
            yield
            for h in H4:
                stt(Cml[h][0], Cml[h][0], spv[:, cols[h]:cols[h] + 1], pC[h][0][:, 0:257], ALU.mult, ALU.add,
                    [Cml[h][1], Bgsm, pC[h][1]], [Cml[h][1]])
            yield
            for h in H4:
                cp(CmlR[h][0][:, 0:257], Cml[h][0], [Cml[h][1]], [CmlR[h][1]], eng="act")

            yield

        def ret_gen(c):
            vvs = [rv[c][0][:, h * 256:(h + 1) * 256] for h in H4]
            if own:
                qrT, BqrT = qrTs[c]
                krT, BkrT = krTs[c]
                pq, Bpq = nextp()
                pk_, Bpk = nextp()
                for h in H4:
                    S.op("pe", lambda hd, o=pq[:, h * 128:(h + 1) * 128], i=rq[c][0][:, h * 128:(h + 1) * 128]: hd.transpose(o, i, ident),
                         [rq[c][1], Bcst], [Bpq])
                yield
                for h in H4:
                    S.op("pe", lambda hd, o=pk_[:, h * 128:(h + 1) * 128], i=rk[c][0][:, h * 128:(h + 1) * 128]: hd.transpose(o, i, ident),
                         [rk[c][1], Bcst], [Bpk])
                yield
                cp(qrT, pq, [Bpq], [BqrT], eng="act")
                cp(krT, pk_, [Bpk], [BkrT], eng="act")
                pS = [nextp() for h in H4]
                for h in H4:
                    mm(pS[h][0][:, 0:128], krT[:, h * 128:(h + 1) * 128], qrT[:, h * 128:(h + 1) * 128], True, True, [BkrT, BqrT], [pS[h][1]])
                yield
                for h in H4:
                    tt(PTs[h][0], pS[h][0][:, 0:128], cst[:, C_DECT + h * 128:C_DECT + (h + 1) * 128], ALU.mult, [pS[h][1], Bcst], [PTs[h][1]])
                yield
                for h in H4:
                    tt(qdTs[h][0], qrT[:, h * 128:(h + 1) * 128].bitcast(F32), cst[:, C_QD + h * 128:C_QD + (h + 1) * 128], ALU.mult,
                       [BqrT, Bcst], [qdTs[h][1]])
                yield
                pI = [nextp() for h in H4]
                for h in H4:
                    mm(pI[h][0][:, 0:256], PTs[h][0], vvs[h], True, False, [PTs[h][1], rv[c][1]], [pI[h][1]])
                    mm(pI[h][0][:, 0:256], qdTs[h][0], CretR[h][0], False, True, [qdTs[h][1], CretR[h][1]], [pI[h][1]])
                yield
                for h in H4:
                    cp(hhs[h][0], pI[h][0][:, 0:256], [pI[h][1]], [hhs[h][1]], eng="act")
                yield
                yield from ln_stages([(hhs[h][0], hhs[h][1], sths[h][0], sths[h][1]) for h in H4])
                for h in H4:
                    gsl = rg[c][0][:, h * 256:(h + 1) * 256]
                    tt(gsl, gsl, hhs[h][0], ALU.mult, [rg[c][1], hhs[h][1]], [rg[c][1]])
            for h in H4:
                ts(kws[h][0], rk[c][0][:, h * 128:(h + 1) * 128], cst[:, C_KDEC + h:C_KDEC + h + 1], None, ALU.mult, None,
                   [rk[c][1], Bcst], [kws[h][1]])
            yield
            pC = [nextp() for h in H4]
            for h in H4:
                mm(pC[h][0][:, 0:256], kws[h][0], vvs[h], True, True, [kws[h][1], rv[c][1]], [pC[h][1]])
            yield
            for h in H4:
                stt(Cret[h][0], Cret[h][0], GAM[h] ** 128, pC[h][0][:, 0:256], ALU.mult, ALU.add, [Cret[h][1], pC[h][1]], [Cret[h][1]])
            yield
            for h in H4:
                cp(CretR[h][0], Cret[h][0], [Cret[h][1]], [CretR[h][1]], eng="act")

            yield

        for c in range(2):
            for g_ in (ml_gen(c), ret_gen(c)):
                for _ in g_:
                    pass

        if not own:
            continue
        for c in range(2):
            for (src, Bsrc, dst3, Bdst, gT) in ((oml[c][0], oml[c][1], hmT3, BhmT, mlgT), (rg[c][0], rg[c][1], hrT3, BhrT, retgT)):
                for hf in range(2):
                    pb, Bp = nextp()
                    for k4 in range(4):
                        kc = hf * 4 + k4
                        S.op("pe", lambda hd, o=pb[:, k4 * 128:(k4 + 1) * 128], i=src[:, kc * 128:(kc + 1) * 128]: hd.transpose(o, i, ident),
                             [Bsrc, Bcst], [Bp])
                    for k4 in range(4):
                        kc = hf * 4 + k4
                        act(dst3[:, kc, c * 128:(c + 1) * 128], pb[:, k4 * 128:(k4 + 1) * 128], AF.Copy, [Bp, Bsm], [Bdst],
                            scale=gT[:, kc:kc + 1])
        for t in range(4):
            wm3, wmb = wload(wbml.rearrange("(k p) c -> p k c", p=128)[:, :, t * 256:(t + 1) * 256], 256)
            pbm = []
            for f in range(2):
                pb, Bp = nextp()
                for kc in range(8):
                    mm(pb[:, 0:G], wm3[:, kc, f * 128:(f + 1) * 128], hmT3[:, kc, :], kc == 0, kc == 7, [wmb, BhmT], [Bp])
                pbm.append((pb, Bp))
            wg3, wgb = win_tile(6152 + t * 256)
            for f in range(2):
                pb, Bp = nextp()
                for kc in range(8):
                    mm(pb[:, 0:G], wg3[:, kc, f * 128:(f + 1) * 128], h1T3[:, kc, :], kc == 0, kc == 7, [wgb, Bh1T], [Bp])
                act(sg1, pb[:, 0:G], AF.Sigmoid, [Bp], [Bsg1])
                tt(yT3[:, t * 2 + f, :], pbm[f][0][:, 0:G], sg1, ALU.mult, [pbm[f][1], Bsg1], [ByT])
            wr3, wrb = wload(wbret.rearrange("(k p) c -> p k c", p=128)[:, :, t * 256:(t + 1) * 256], 256)
            pbr = []
            for f in range(2):
                pb, Bp = nextp()
                for kc in range(8):
                    mm(pb[:, 0:G], wr3[:, kc, f * 128:(f + 1) * 128], hrT3[:, kc, :], kc == 0, kc == 7, [wrb, BhrT], [Bp])
                pbr.append((pb, Bp))
            wg3, wgb = win_tile(7176 + t * 256)
            for f in range(2):
                pb, Bp = nextp()
                for kc in range(8):
                    mm(pb[:, 0:G], wg3[:, kc, f * 128:(f + 1) * 128], h1T3[:, kc, :], kc == 0, kc == 7, [wgb, Bh1T], [Bp])
                act(sg1, pb[:, 0:G], AF.Sigmoid, [Bp], [Bsg1])
                tt(sg2, pbr[f][0][:, 0:G], sg1, ALU.mult, [pbr[f][1], Bsg1], [Bsg2])
                tt(yT3[:, t * 2 + f, :], yT3[:, t * 2 + f, :].bitcast(F32), sg2, ALU.add, [ByT, Bsg2], [ByT])
        for t in range(4):
            wo3, wob = wload(wout.rearrange("(k p) c -> p k c", p=128)[:, :, t * 256:(t + 1) * 256], 256)
            for c in range(2):
                pb, Bp = nextp()
                for kc in range(8):
                    mm(pb[:, 0:256], yT3[:, kc, c * 128:(c + 1) * 128], wo3[:, kc, :], kc == 0, kc == 7, [ByT, wob], [Bp])
                xsl = xg[c][0][:, t * 256:(t + 1) * 256]
                tt(sg2, pb[:, 0:256], gmb[:, t * 256:(t + 1) * 256], ALU.mult, [Bp, Bgmb], [Bsg2])
                tt(xsl, xsl, sg2, ALU.add, [xg[c][1], Bsg2], [xg[c][1]])
        for c in range(2):
            dma("sp", x2s[t0 + c * 128:t0 + (c + 1) * 128, :], xg[c][0], [xg[c][1]], [], x2sem)

    S.barrier()
    top[0] = PERS_END
    BLK = 512
    NBK = (TOK * 4) // BLK + NE
    NSLOT = NBK * BLK
    Xs = nc.dram_tensor("Xs", [NSLOT, D], F32, kind="Internal").ap()
    Ys = nc.dram_tensor("Ys", [NSLOT, D], F32, kind="Internal").ap()
    H2 = nc.dram_tensor("H2", [TOK, D], F32, kind="Internal").ap()
    A2bc, BA2bc = T(D, name="A2bc")
    B2bc, BB2bc = T(D, name="B2bc")
    xc, Bxc = T(D, name="xc")
    xs2, Bxs2 = T(D, name="xs2")
    h2tm, Bh2tm = T(D, name="h2tm")
    h2Tc, Bh2Tc = T(D, F32R, "h2Tc")
    h2Tc3 = h2Tc.rearrange("p (k n) -> p k n", n=128)
    st2, Bst2 = T(16, name="st2")
    lgt, Blgt = T(NE, name="lgt")
    t8, Bt8 = T(16, name="t8")
    maskall, Bmask = T(16 * NE, name="maskall")
    Gwall, BGw = T(16 * NE, name="Gwall")
    tris, Btris = T(128, name="tristrict")
    cntb, Bcnt = T(NE, name="cnt")
    nbi, Bnbi = T(NE, I32, "nbi")
    nbf, Bnbf = T(NE, name="nbf")
    bend, Bbend = T(NE, name="bend")
    sbase, Bsbase = T(NE, name="sbase")
    ones32, Bones32 = T(NE, name="ones32")
    key, Bkey = T(NE, name="key")
    eqt, Beqt = T(NE, name="eqt")
    s4, Bs4 = T(16, name="s4")
    sidxf, Bsidxf = T(64, name="sidxf")
    w4, Bw4 = T(64, name="w4")
    ebrow, Beb = T(NBK, name="ebrow")
    widxf, Bwidxf = T(NBK * 8, name="widxf")
    didxf, Bdidxf = T(NBK * 4, name="didxf")
    bidxf, Bbidxf = T(NBK * 2, name="bidxf")
    Xtm, BXtm = T(4 * D, name="Xtm")
    Xtm3 = Xtm.rearrange("p (j c) -> p j c", c=D)
    XT, BXT = T(8 * BLK, F32R, "XT")
    XT3 = XT.rearrange("p (k n) -> p k n", n=BLK)
    actT, BactT = T(8 * BLK, F32R, "actT")
    actT3 = actT.rearrange("p (k n) -> p k n", n=BLK)
    NU = 3
    wgt = [T(8 * 256, F32R, "wgu%d" % i) for i in range(NU)]
    wgsem = [S.dmasem("wgu%d" % i) for i in range(NU)]
    wq = [T(2 * 1024, F32R, "wd%d" % q) for q in range(4)]
    wq3 = [wq[q][0].rearrange("p (k n) -> p k n", n=1024) for q in range(4)]
    wqsem = [S.dmasem("wd%d" % q) for q in range(4)]
    bgt = [T(16, name="bgt%d" % i) for i in range(2)]
    bgsem = [S.dmasem("bgt%d" % i) for i in range(2)]
    bdb = [T(D, name="bdb%d" % i) for i in range(2)]
    bdsem = [S.dmasem("bdb%d" % i) for i in range(2)]
    gm_ = [T(512, name="gm%d" % i) for i in range(2)]
    sg_ = [T(512, name="sg%d" % i) for i in range(2)]
    lm_ = [T(512, name="lm%d" % i) for i in range(2)]
    dsb = [T(D, name="dsb%d" % i) for i in range(2)]
    acc, Bacc = T(D, name="acc")
    xcsem = S.dmasem("xc")
    h2sem = S.dmasem("h2st")
    h2lsem = S.dmasem("h2ld")
    scsem = S.dmasem("scat")
    xtsem = S.dmasem("xtm")
    yssem = S.dmasem("ysst")
    ygsem = S.dmasem("ygat")
    osem = S.dmasem("ost")
    uctr = [0]
    sctr = [0]
    wguh = wgu.rearrange("e (u q) c -> (e u q) c", u=8)
    wdh = wd.rearrange("e (q r) c -> (e q r) c", q=4)

    def ind(ap_):
        return bass.IndirectOffsetOnAxis(ap=ap_, axis=0)

    _bregs = {}

    def bnd(h, v):
        if v not in _bregs:
            _bregs[v] = h.to_reg(v)
        return _bregs[v]

    NIT = 40
    itl = [T(1, I32, "idx%d" % i) for i in range(NIT)]
    ictr = [0]

    def idx_tile(colap, Bsrc):
        i = ictr[0] % NIT
        ictr[0] += 1
        ap_, b_ = itl[i]
        cp(ap_, colap, [Bsrc], [b_])
        return ap_, b_

    def block_idx(b):
        d = {}
        d["bg"] = idx_tile(bidxf[:, b:b + 1], Bbidxf)
        d["bd"] = idx_tile(bidxf[:, NBK + b:NBK + b + 1], Bbidxf)
        for u in range(8):
            d["w%d" % u] = idx_tile(widxf[:, b * 8 + u:b * 8 + u + 1], Bwidxf)
        for q in range(4):
            d["d%d" % q] = idx_tile(didxf[:, b * 4 + q:b * 4 + q + 1], Bdidxf)
        return d

    def bcast_cols(dst, Bdst, colap, Bcol):
        for hf in range(2):
            pb, Bp = nextp()
            for k4 in range(4):
                kc = hf * 4 + k4
                ts(dgt, ident, colap[:, kc:kc + 1], None, ALU.mult, None, [Bcst, Bcol], [Bdgt])
                mm(pb[:, k4 * 128:(k4 + 1) * 128], ones[:, 0:128], dgt, True, True, [Bcst, Bdgt], [Bp])
            cp(dst[:, hf * 512:(hf + 1) * 512], pb, [Bp], [Bdst])
    dgt, Bdgt = T(128, name="dgt2")
    bcast_cols(A2bc, BA2bc, A2, BAB)
    bcast_cols(B2bc, BB2bc, B2, BAB)
    tt(tris, tri, ident, ALU.subtract, [Bcst], [Btris])
    S.op("dve", lambda h: h.memset(ones32, 1.0), [], [Bones32])
    wrt3 = wrt.rearrange("p (k n) -> p k n", n=NE)

    for c in range(16):
        dma("sp", xc, x2s[c * 128:(c + 1) * 128, :], [], [Bxc], xcsem)
        act(xs2, xc, AF.Square, [Bxc], [Bxs2, Bst2], accum=st2[:, 0:1])
        act(st2[:, 1:2], st2[:, 0:1], AF.Sqrt, [Bst2, Bsm], [Bst2], bias=epsc, scale=1.0 / D)
        S.op("dve", lambda h: h.reciprocal(st2[:, 2:3], st2[:, 1:2]), [Bst2], [Bst2])
        ts(xs2, xc, st2[:, 2:3], None, ALU.mult, None, [Bxc, Bst2], [Bxs2])
        tt(h2tm, xs2, A2bc, ALU.mult, [Bxs2, BA2bc], [Bh2tm])
        tt(h2tm, h2tm, B2bc, ALU.add, [Bh2tm, BB2bc], [Bh2tm])
        dma("sp", H2[c * 128:(c + 1) * 128, :], h2tm, [Bh2tm], [], h2sem)
        for hf in range(2):
            pb, Bp = nextp()
            for k4 in range(4):
                kc = hf * 4 + k4
                S.op("pe", lambda h, o=pb[:, k4 * 128:(k4 + 1) * 128], i=h2tm[:, kc * 128:(kc + 1) * 128]: h.transpose(o, i, ident),
                     [Bh2tm, Bcst], [Bp])
            cp(h2Tc[:, hf * 512:(hf + 1) * 512], pb, [Bp], [Bh2Tc], eng="act")
        pl, Bpl = nextp()
        for kc in range(8):
            mm(pl[:, 0:NE], h2Tc3[:, kc, :], wrt3[:, kc, :], kc == 0, kc == 7, [Bh2Tc, Bwrt], [Bpl])
        tt(lgt, pl[:, 0:NE], brb, ALU.add, [Bpl, Bbrb], [Blgt])
        S.op("dve", lambda h: h.max(t8[:, 0:8], lgt), [Blgt], [Bt8])
        ts(maskall[:, c * NE:(c + 1) * NE], lgt, t8[:, 3:4], None, ALU.is_ge, None, [Blgt, Bt8], [Bmask])
        ts(t8[:, 8:9], t8[:, 0:1], -1.0, None, ALU.mult, None, [Bt8], [Bt8])
        act(lgt, lgt, AF.Exp, [Blgt, Bt8], [Blgt], bias=t8[:, 8:9])
        tt(lgt, lgt, maskall[:, c * NE:(c + 1) * NE], ALU.mult, [Blgt, Bmask], [Blgt])
        S.op("dve", lambda h: h.reduce_sum(t8[:, 9:10], lgt, mybir.AxisListType.X), [Blgt], [Bt8])
        S.op("dve", lambda h: h.reciprocal(t8[:, 10:11], t8[:, 9:10]), [Bt8], [Bt8])
        ts(Gwall[:, c * NE:(c + 1) * NE], lgt, t8[:, 10:11], None, ALU.mult, None, [Blgt, Bt8], [BGw])

    pcn, Bpcn = nextp()
    for c in range(16):
        mm(pcn[:, 0:NE], ones[:, 0:128], maskall[:, c * NE:(c + 1) * NE], c == 0, c == 15, [Bcst, Bmask], [Bpcn])
    cp(cntb, pcn[:, 0:NE], [Bpcn], [Bcnt])
    ts(nbi, cntb, 1.0 / BLK, (BLK - 1.0) / BLK - 0.49951171875, ALU.mult, ALU.add, [Bcnt], [Bnbi])
    cp(nbf, nbi, [Bnbi], [Bnbf])
    S.op("dve", lambda h: h.tensor_tensor_scan(bend, ones32, nbf, 0.0, ALU.mult, ALU.add), [Bones32, Bnbf], [Bbend])
    tt(sbase, bend, nbf, ALU.subtract, [Bbend, Bnbf], [Bsbase])
    ts(sbase, sbase, float(BLK), None, ALU.mult, None, [Bsbase], [Bsbase])
    iob = cst[:, C_IOB:C_IOB + NBK]
    S.op("dve", lambda h: h.memset(ebrow, 0.0), [], [Beb])
    for e in range(NE):
        stt(ebrow, iob, bend[:, e:e + 1], ebrow, ALU.is_ge, ALU.add, [Bcst, Bbend, Beb], [Beb])
    ts(ebrow, ebrow, float(NE - 1), None, ALU.min, None, [Beb], [Beb])
    widxf3 = widxf.rearrange("p (b u) -> p b u", u=8)
    for u in range(8):
        ts(widxf3[:, :, u], ebrow, 1024.0, cst[:, C_CU + u:C_CU + u + 1], ALU.mult, ALU.add, [Beb, Bcst], [Bwidxf])
    didxf3 = didxf.rearrange("p (b q) -> p b q", q=4)
    for q in range(4):
        ts(didxf3[:, :, q], ebrow, 512.0, cst[:, C_CU + q:C_CU + q + 1], ALU.mult, ALU.add, [Beb, Bcst], [Bdidxf])
    ts(bidxf[:, 0:NBK], ebrow, 128.0, cst[:, C_CU:C_CU + 1], ALU.mult, ALU.add, [Beb, Bcst], [Bbidxf])
    cp(bidxf[:, NBK:2 * NBK], ebrow, [Beb], [Bbidxf])

    prk, Bprk = nextp()
    for c in range(16):
        for c2 in range(c):
            mm(prk[:, c * NE:(c + 1) * NE], ones[:, 0:128], maskall[:, c2 * NE:(c2 + 1) * NE], c2 == 0, False, [Bcst, Bmask], [Bprk])
        mm(prk[:, c * NE:(c + 1) * NE], tris, maskall[:, c * NE:(c + 1) * NE], c == 0, True, [Btris, Bmask], [Bprk])
    for c in range(16):
        mk = maskall[:, c * NE:(c + 1) * NE]
        tt(key, prk[:, c * NE:(c + 1) * NE], sbase, ALU.add, [Bprk, Bsbase], [Bkey])
        ts(key, key, 1.0, None, ALU.add, None, [Bkey], [Bkey])
        tt(key, key, mk, ALU.mult, [Bkey, Bmask], [Bkey])
        S.op("dve", lambda h: h.max(s4[:, 0:8], key), [Bkey], [Bs4])
        ts(sidxf[:, c * 4:(c + 1) * 4], s4[:, 0:4], -1.0, None, ALU.add, None, [Bs4], [Bsidxf])
        for k in range(4):
            ts(eqt, key, s4[:, k:k + 1], None, ALU.is_equal, None, [Bkey, Bs4], [Beqt])
            tt(eqt, eqt, Gwall[:, c * NE:(c + 1) * NE], ALU.mult, [Beqt, BGw], [Beqt])
            S.op("dve", lambda h, o=w4[:, c * 4 + k:c * 4 + k + 1]: h.reduce_sum(o, eqt, mybir.AxisListType.X), [Beqt], [Bw4])
    lasth2 = S.last[("dma", id(h2sem))]
    for c in range(16):
        S.op("sp", lambda h, c=c: h.dma_start(out=h2tm, in_=H2[c * 128:(c + 1) * 128, :]), [], [Bh2tm], dma=h2lsem, extra=[lasth2])
        for k in range(4):
            ia, Bia = idx_tile(sidxf[:, c * 4 + k:c * 4 + k + 1], Bsidxf)
            S.op("pool", lambda h, ia=ia: h.indirect_dma_start(
                out=Xs[:, :], out_offset=ind(ia), in_=h2tm, in_offset=None, bounds_check=bnd(h, NSLOT - 1), oob_is_err=False),
                [Bh2tm, Bia], [], dma=scsem)

    lastsc = S.last[("dma", id(scsem))]
    nxt_ix = block_idx(0)
    for b in range(NBK):
        bix = nxt_ix
        if b + 1 < NBK:
            nxt_ix = block_idx(b + 1)
        if b == 0:
            S.op("sp", lambda h, b=b: h.dma_start(out=Xtm3, in_=Xs[b * BLK:(b + 1) * BLK, :].rearrange("(j p) c -> p j c", p=128)),
                 [], [BXtm], dma=xtsem, extra=[lastsc])
        for j in range(4):
            for hf in range(2):
                pb, Bp = nextp()
                for k4 in range(4):
                    kc = hf * 4 + k4
                    S.op("pe", lambda h, o=pb[:, k4 * 128:(k4 + 1) * 128], i=Xtm3[:, j, kc * 128:(kc + 1) * 128]: h.transpose(o, i, ident),
                         [BXtm, Bcst], [Bp])
                for k4 in range(4):
                    kc = hf * 4 + k4
                    cp(XT3[:, kc, j * 128:(j + 1) * 128], pb[:, k4 * 128:(k4 + 1) * 128], [Bp], [BXT], eng="act")
        if b + 1 < NBK:
            S.op("sp", lambda h, b=b + 1: h.dma_start(out=Xtm3, in_=Xs[b * BLK:(b + 1) * BLK, :].rearrange("(j p) c -> p j c", p=128)),
                 [], [BXtm], dma=xtsem, extra=[lastsc])
        bi = b % 2
        ia, Bia = bix["bg"]
        S.op("pool", lambda h, o=bgt[bi][0], ia=ia: h.indirect_dma_start(
            out=o, out_offset=None, in_=bguT_d[:, :], in_offset=ind(ia), bounds_check=bnd(h, NE * 128 - 1), oob_is_err=False),
            [Bia], [bgt[bi][1]], dma=bgsem[bi])
        ia, Bia = bix["bd"]
        S.op("pool", lambda h, o=bdb[bi][0], ia=ia: h.indirect_dma_start(
            out=o, out_offset=None, in_=bd_d[:, :], in_offset=ind(ia), bounds_check=bnd(h, NE - 1), oob_is_err=False),
            [Bia], [bdb[bi][1]], dma=bdsem[bi])
        for u in range(8):
            i = uctr[0] % NU
            uctr[0] += 1
            wv, wb = wgt[i]
            w3 = wv.rearrange("p (k n) -> p k n", n=256)
            ia, Bia = bix["w%d" % u]
            S.op("pool", lambda h, o=wv, ia=ia: h.indirect_dma_start(
                out=o, out_offset=None, in_=wguh[:, :], in_offset=ind(ia), bounds_check=bnd(h, NE * 1024 - 1), oob_is_err=False),
                [Bia], [wb], dma=wgsem[i])
            pg, Bpg = nextp()
            for kc in range(8):
                mm(pg, w3[:, kc, 0:128], XT3[:, kc, :], kc == 0, kc == 7, [wb, BXT], [Bpg])
            plin, Bplin = nextp()
            for kc in range(8):
                mm(plin, w3[:, kc, 128:256], XT3[:, kc, :], kc == 0, kc == 7, [wb, BXT], [Bplin])
            si = sctr[0] % 2
            sctr[0] += 1
            gmv, Bgm = gm_[si]; sgv, Bsg = sg_[si]; lmv, Blm = lm_[si]
            bg = bgt[bi][0]
            ts(gmv, pg, bg[:, u * 2:u * 2 + 1], 7.0, ALU.add, ALU.min, [Bpg, bgt[bi][1]], [Bgm])
            act(sgv, gmv, AF.Sigmoid, [Bgm], [Bsg], scale=1.702)
            act(lmv, plin, AF.Identity, [Bplin, bgt[bi][1]], [Blm], bias=bg[:, u * 2 + 1:u * 2 + 2])
            ts(lmv, lmv, 7.0, -7.0, ALU.min, ALU.max, [Blm], [Blm])
            tt(gmv, gmv, sgv, ALU.mult, [Bgm, Bsg], [Bgm])
            stt(actT3[:, u, :], lmv, 1.0, gmv, ALU.add, ALU.mult, [Blm, Bgm], [BactT])
        for q in range(4):
            ia, Bia = bix["d%d" % q]
            S.op("pool", lambda h, o=wq[q][0], ia=ia: h.indirect_dma_start(
                out=o, out_offset=None, in_=wdh[:, :], in_offset=ind(ia), bounds_check=bnd(h, NE * 512 - 1), oob_is_err=False),
                [Bia], [wq[q][1]], dma=wqsem[q])
        for j in range(4):
            dv, Bd = dsb[j % 2]
            for nt in range(2):
                pd_, Bpd = nextp()
                for ft in range(8):
                    mm(pd_, actT3[:, ft, j * 128:(j + 1) * 128], wq3[ft // 2][:, ft % 2, nt * 512:(nt + 1) * 512], ft == 0, ft == 7,
                       [BactT, wq[ft // 2][1]], [Bpd])
                tt(dv[:, nt * 512:(nt + 1) * 512], pd_, bdb[bi][0][:, nt * 512:(nt + 1) * 512], ALU.add, [Bpd, bdb[bi][1]], [Bd])
            dma("sp", Ys[b * BLK + j * 128:b * BLK + (j + 1) * 128, :], dv, [Bd], [], yssem)

    lastys = S.last[("dma", id(yssem))]
    xtm_prev = [BXtm.w] + list(BXtm.r.values())
    BY = [Buf("Yk%d" % k) for k in range(4)]
    ygs = [S.dmasem("ygat%d" % k) for k in range(4)]
    for c in range(16):
        dma("sp", xc, x2s[c * 128:(c + 1) * 128, :], [], [Bxc], xcsem)
        for k in range(4):
            ia, Bia = idx_tile(sidxf[:, c * 4 + k:c * 4 + k + 1], Bsidxf)
            S.op("pool", lambda h, o=Xtm3[:, k, :], ia=ia: h.indirect_dma_start(
                out=o, out_offset=None, in_=Ys[:, :], in_offset=ind(ia), bounds_check=bnd(h, NSLOT - 1), oob_is_err=False),
                [Bia], [BY[k]], dma=ygs[k], extra=[lastys] + [d_ for d_ in xtm_prev if d_ is not None])
        ts(acc, Xtm3[:, 0, :], w4[:, c * 4:c * 4 + 1], None, ALU.mult, None, [BY[0], Bw4], [Bacc])
        for k in range(1, 4):
            stt(acc, Xtm3[:, k, :], w4[:, c * 4 + k:c * 4 + k + 1], acc, ALU.mult, ALU.add, [BY[k], Bw4, Bacc], [Bacc])
        tt(acc, acc, gfb, ALU.mult, [Bacc, Bgfb], [Bacc])
        tt(xc, xc, acc, ALU.add, [Bxc, Bacc], [Bxc])
        act(xs2, xc, AF.Square, [Bxc], [Bxs2, Bst2], accum=st2[:, 0:1])
        act(st2[:, 1:2], st2[:, 0:1], AF.Sqrt, [Bst2, Bsm], [Bst2], bias=epsc, scale=1.0 / D)
        S.op("dve", lambda h: h.reciprocal(st2[:, 2:3], st2[:, 1:2]), [Bst2], [Bst2])
        stt(xs2, xc, st2[:, 2:3], gfin, ALU.mult, ALU.mult, [Bxc, Bst2, Bgfin], [Bxs2])
        dma("sp", out[c * 128:(c + 1) * 128, :], xs2, [Bxs2], [], osem)

    print("arena cols: pers", PERS_END, "mix", MIX_END, "moe", top[0], "ops", len(S.ops))
    S.emit(nc, es, final_waits=[osem, x2sem, yssem, h2sem, scsem])
    es.close()
    return nc


def _consts():
    c = np.zeros((128, NC), np.float64)
    idx = np.arange(128)
    c[:, C_ID:C_ID + 128] = np.eye(128)
    s = idx[:, None]; j = idx[None, :]
    c[:, C_TRI:C_TRI + 128] = (s <= j)
    c[:, C_MNEG:C_MNEG + 128] = np.where(s <= j, 0.0, -30000.0)
    for h in range(4):
        c[h, C_SEL + h * 128:C_SEL + (h + 1) * 128] = 1.0
        lg = math.log(1.0 - 2.0 ** (-5.0 - h))
        c[:, C_DECT + h * 128:C_DECT + (h + 1) * 128] = np.where(j >= s, np.exp(lg * np.maximum(j - s, 0)), 0.0) * 128.0 ** -0.5
        c[:, C_QD + h * 128:C_QD + (h + 1) * 128] = np.exp(lg * (j + 1.0)) * np.ones((128, 1))
        c[:, C_KDEC + h] = np.exp(lg * (127.0 - idx)) * 128.0 ** -0.5
    c[:, C_ONES:C_ONES + 512] = 1.0
    inv = (10000.0 ** (-np.arange(64, dtype=np.float32) / np.float32(64))).astype(np.float32)
    c[:, C_INVF:C_INVF + 256] = np.tile(inv, 4)[None, :]
    c[:, C_IOB:C_IOB + 64] = np.arange(64)[None, :]
    c[:, C_CU:C_CU + 8] = np.arange(8)[None, :] * 128 + idx[:, None]
    return c.astype(np.float32)


_NC_CACHE = {}


def _colT(v, n):
    return np.ascontiguousarray(np.asarray(v, np.float32).reshape(n, 128).T)


def kernel(x, c, positions, w_ada, b_ada, norm_mix_g, w_in, conv_w, conv_b, b_if, ml_norm_g, ret_norm_g,
           w_branch_ml, w_branch_ret, w_out, norm_ffn_g, w_router, b_router, w_gate_up, b_gate_up, w_down, b_down,
           norm_final_g, _debug=False):
    f = lambda a: np.ascontiguousarray(np.asarray(a, np.float32))
    x = f(x); c = f(c)
    positions = np.asarray(positions).astype(np.int32)
    key = bool(_debug)
    if key not in _NC_CACHE:
        _NC_CACHE[key] = build_nc(debug=key)
    nc = _NC_CACHE[key]
    wgu_p = f(w_gate_up)[0].reshape(NE, 8, 128, 8, 128, 2)
    wgu_p = np.ascontiguousarray(wgu_p.transpose(0, 3, 2, 1, 5, 4)).reshape(NE, D, 2 * D)
    wd_p = f(w_down)[0].reshape(NE, 4, 2, 128, D)
    wd_p = np.ascontiguousarray(wd_p.transpose(0, 1, 3, 2, 4)).reshape(NE, D // 2, 2 * D)
    bgu = f(b_gate_up)[0].reshape(NE, D, 2)
    bguT = np.stack([bgu[..., 0].reshape(NE, 8, 128), bgu[..., 1].reshape(NE, 8, 128)], axis=2)
    bguT = np.ascontiguousarray(bguT.transpose(0, 3, 1, 2).reshape(NE * 128, 16))
    shared = {
        "cst": _consts(),
        "w_ada": f(w_ada)[0], "b_adaT": _colT(f(b_ada)[0], 48),
        "gmixT": _colT(f(norm_mix_g)[0], 8), "gffnT": _colT(f(norm_ffn_g)[0], 8), "gfin": f(norm_final_g).reshape(1, D),
        "w_in": f(w_in)[0],
        "convwT": np.ascontiguousarray(f(conv_w)[0].T.reshape(8, 128, 4).transpose(1, 0, 2).reshape(128, 32)),
        "convbT": _colT(f(conv_b)[0], 8), "bif": f(b_if)[0].reshape(1, 8),
        "mlgT": _colT(f(ml_norm_g)[0], 8), "retgT": _colT(f(ret_norm_g)[0], 8),
        "wbml": f(w_branch_ml)[0], "wbret": f(w_branch_ret)[0], "wout": f(w_out)[0],
        "wr": f(w_router)[0], "br": f(b_router)[0].reshape(1, NE),
        "wgu": wgu_p, "bguT": bguT, "wd": wd_p, "bd": f(b_down)[0],
    }
    in_maps = []
    for i in range(8):
        b, half = i // 2, i % 2
        m = dict(shared)
        m["xo"] = np.ascontiguousarray(x[b, half * TOK:(half + 1) * TOK])
        m["xp"] = np.ascontiguousarray(x[b, 0:TOK])
        m["poso"] = np.ascontiguousarray(positions[b, half * TOK:(half + 1) * TOK].reshape(16, 128).T)
        m["posp"] = np.ascontiguousarray(positions[b, 0:TOK].reshape(16, 128).T)
        m["flag"] = np.full((128, 1), float(half), np.float32)
        m["cT"] = _colT(c[b], 8)
        in_maps.append(m)
    res = run_bass_kernel_spmd(nc, in_maps, core_ids=list(range(8)))
    outp = np.zeros((4, SEQ, D), np.float32)
    for i in range(8):
        b, half = i // 2, i % 2
        outp[b, half * TOK:(half + 1) * TOK] = res.results[i]["out"]
    if _debug:
        dbg = np.zeros((4, SEQ, D), np.float32)
        for i in range(8):
            b, half = i // 2, i % 2
            dbg[b, half * TOK:(half + 1) * TOK] = res.results[i]["x2s"]
        return outp, dbg
    return outp
```

```python
import contextlib
import math
import numpy as np
import concourse.bass as bass
import concourse.mybir as mybir
from concourse.bass_utils import run_bass_kernel_spmd

F32 = mybir.dt.float32
F32R = mybir.dt.float32r
I32 = mybir.dt.int32
ALU = mybir.AluOpType
AF = mybir.ActivationFunctionType

D = 1024
SEQ = 4096
TOK = 2048
G = 256
NE = 32
EPS = 1e-5
LNSCALE = math.log(128.0 ** -0.5)
TWO_PI = 6.283185307179586

C_ID, C_TRI, C_MNEG, C_SEL, C_ONES, C_DECT, C_QD, C_KDEC, C_INVF = 0, 128, 256, 384, 896, 1408, 1920, 2432, 2436
C_IOB, C_CU = 2692, 2756
NC = 2764


class Buf:
    __slots__ = ("name", "w", "r")

    def __init__(self, name=""):
        self.name = name
        self.w = None
        self.r = {}


class DmaSem:
    __slots__ = ("sem", "value", "name")

    def __init__(self, name):
        self.name = name
        self.sem = None
        self.value = 0


class Op:
    __slots__ = ("eng", "fn", "deps", "needed", "tok", "dma")


class Sched:
    ENGS = ("pe", "dve", "act", "pool", "sp")

    def __init__(self, sync_same=True):
        self.ops = []
        self.sync_same = sync_same
        self.dmasems = []
        self.last = {}

    def dmasem(self, name):
        d = DmaSem(name)
        self.dmasems.append(d)
        return d

    def op(self, eng, fn, reads=(), writes=(), dma=None, extra=()):
        o = Op()
        o.eng, o.fn, o.dma = eng, fn, dma
        o.needed = dma is not None
        o.tok = None
        deps = {}
        for b in reads:
            if b.w is not None:
                deps[id(b.w)] = b.w
        for b in writes:
            if b.w is not None:
                deps[id(b.w)] = b.w
            for d in b.r.values():
                deps[id(d)] = d
        for d in extra:
            deps[id(d)] = d
        o.deps = []
        for d in deps.values():
            if d is o:
                continue
            if d.dma is None and d.eng == eng and (eng == "pe" or not self.sync_same):
                continue
            d.needed = True
            o.deps.append(d)
        key = ("dma", id(dma)) if dma is not None else eng
        for b in reads:
            b.r[key] = o
        for b in writes:
            b.w = o
            b.r = {}
        self.ops.append(o)
        self.last[key] = o
        return o

    def barrier(self):
        lasts = list(self.last.values())
        for e in self.ENGS:
            self.op(e, None, extra=[d for d in lasts if d.fn is not None])

    def emit(self, nc, es, final_waits=()):
        esem = {e: es.enter_context(nc.semaphore("s_" + e)) for e in self.ENGS}
        for d in self.dmasems:
            d.sem = es.enter_context(nc.semaphore("d_" + d.name))
            d.value = 0
        cnt = {e: 0 for e in self.ENGS}
        for o in self.ops:
            if o.fn is None:
                continue
            if o.dma is not None:
                o.dma.value += 16
                o.tok = (o.dma.sem, o.dma.value)
            elif o.needed:
                cnt[o.eng] += 1
                o.tok = (esem[o.eng], cnt[o.eng])
        block = es.enter_context(nc.Block())
        per = {e: [o for o in self.ops if o.eng == e] for e in self.ENGS}

        def run(e, h):
            waited = {}
            for o in per[e]:
                need = {}
                for d in o.deps:
                    s, v = d.tok
                    k = id(s)
                    if waited.get(k, 0) >= v:
                        continue
                    if k not in need or need[k][1] < v:
                        need[k] = (s, v)
                for k, (s, v) in need.items():
                    h.wait_ge(s, v)
                    waited[k] = v
                if o.fn is None:
                    continue
                ins = o.fn(h)
                if o.tok is not None:
                    ins.then_inc(o.tok[0], 16 if o.dma is not None else 1)
            if e == "sp":
                for d in final_waits:
                    h.wait_ge(d.sem, d.value)

        @block.tensor
        def _(h):
            run("pe", h)

        @block.vector
        def _(h):
            run("dve", h)

        @block.scalar
        def _(h):
            run("act", h)

        @block.gpsimd
        def _(h):
            run("pool", h)

        @block.sync
        def _(h):
            run("sp", h)


def build_nc(debug=False):
    nc = bass.Bass("TRN2", target_bir_lowering=False)

    def din(name, shape, dt=F32):
        return nc.dram_tensor(name, list(shape), dt, kind="ExternalInput").ap()

    xo = din("xo", [TOK, D]); xp = din("xp", [TOK, D])
    poso = din("poso", [128, 16], I32); posp = din("posp", [128, 16], I32)
    flag_d = din("flag", [128, 1]); cT_d = din("cT", [128, 8]); cst_d = din("cst", [128, NC])
    w_ada = din("w_ada", [D, 6 * D]); b_adaT = din("b_adaT", [128, 48])
    gmixT_d = din("gmixT", [128, 8]); gffnT_d = din("gffnT", [128, 8]); gfin_d = din("gfin", [1, D])
    w_in = din("w_in", [D, 8200]); convwT_d = din("convwT", [128, 32]); convbT_d = din("convbT", [128, 8])
    bif_d = din("bif", [1, 8]); mlgT_d = din("mlgT", [128, 8]); retgT_d = din("retgT", [128, 8])
    wbml = din("wbml", [D, D]); wbret = din("wbret", [D, D]); wout = din("wout", [D, D])
    wr_d = din("wr", [D, NE]); br_d = din("br", [1, NE])
    wgu = din("wgu", [NE, D, 2 * D]); bguT_d = din("bguT", [NE * 128, 16])
    wd = din("wd", [NE, D // 2, 2 * D]); bd_d = din("bd", [NE, D])
    out = nc.dram_tensor("out", [TOK, D], F32, kind="ExternalOutput").ap()
    x2s = nc.dram_tensor("x2s", [TOK, D], F32, kind="ExternalOutput" if debug else "Internal").ap()

    S = Sched()
    es = contextlib.ExitStack()
    NCOL = 53180
    pbanks = [es.enter_context(nc.psum_tensor("pb%d" % i, [128, 512], F32)) for i in range(8)]
    PB = [Buf("pb%d" % i) for i in range(8)]
    pctr = [0]

    def nextp():
        i = pctr[0] % 8
        pctr[0] += 1
        return pbanks[i][:, :], PB[i]

    top = [0]
    tcount = [0]

    def al(n):
        n = (n + 7) // 8 * 8
        a = top[0]
        top[0] += n
        assert top[0] <= NCOL, top[0]
        return a

    def T(n, dt=F32, name=""):
        a = al(n)
        tcount[0] += 1
        t = nc.alloc_sbuf_tensor_at("t%d_%s" % (tcount[0], name), [128, n], dt, offset=16640 + 4 * a)
        return t[:, :], Buf(name)

    def mm(o, lhsT, rhs, st, sp, rd, wr):
        S.op("pe", lambda h: h.matmul(o, lhsT=lhsT, rhs=rhs, start=st, stop=sp), rd, wr)

    def act(o, i, func, rd, wr, bias=None, scale=None, accum=None):
        kw = {}
        if bias is not None:
            kw["bias"] = bias
        if scale is not None:
            kw["scale"] = scale
        if accum is not None:
            kw["accum_out"] = accum
        S.op("act", lambda h: h.activation(o, i, func, **kw), rd, wr)

    def ts(o, i, s1, s2, op0, op1, rd, wr, eng="dve"):
        if op1 is None:
            S.op(eng, lambda h: h.tensor_scalar(o, i, s1, None, op0), rd, wr)
        else:
            S.op(eng, lambda h: h.tensor_scalar(o, i, s1, s2, op0, op1), rd, wr)

    def tt(o, a, b, op, rd, wr, eng="dve"):
        S.op(eng, lambda h: h.tensor_tensor(o, a, b, op), rd, wr)

    def stt(o, a, s, b, op0, op1, rd, wr):
        S.op("dve", lambda h: h.scalar_tensor_tensor(o, a, s, b, op0, op1), rd, wr)

    def cp(o, i, rd, wr, eng="dve"):
        if eng == "act":
            S.op(eng, lambda h: h.copy(o, i), rd, wr)
        else:
            S.op(eng, lambda h: h.tensor_copy(o, i), rd, wr)

    def dma(q, o, i, rd, wr, sem):
        S.op(q, lambda h: h.dma_start(out=o, in_=i), rd, wr, dma=sem)

    cst, Bcst = T(NC, name="cst")
    ident = cst[:, C_ID:C_ID + 128]
    tri = cst[:, C_TRI:C_TRI + 128]
    mneg = cst[:, C_MNEG:C_MNEG + 128]
    ones = cst[:, C_ONES:C_ONES + 512]
    onesR, BonesR = T(512, F32R, "onesR")
    modT, BmodT = T(48, name="modT")
    AB, BAB = T(32, name="AB")
    sm, Bsm = T(160, name="small")
    gmixT = sm[:, 0:8]; gffnT = sm[:, 8:16]; badaT = sm[:, 16:64]; convw = sm[:, 64:96]; convb = sm[:, 96:104]
    bifb = sm[:, 104:112]; flag = sm[:, 112:113]; epsc = sm[:, 113:114]; cTt = sm[:, 114:122]
    mlgT = sm[:, 122:130]; retgT = sm[:, 130:138]
    cact2, Bcact = T(16, F32R, "cact2")
    gfb, Bgfb = T(D, name="gate_f_bc")
    gfin, Bgfin = T(D, name="gfin_bc")
    wrt, Bwrt = T(8 * NE, F32R, "wr")
    brb, Bbrb = T(NE, name="br")
    posf, Bposf = T(32, name="posf")
    posi, Bposi = T(32, I32, "posi")
    PERS_END = top[0]

    dl = {n: S.dmasem(n) for n in ["c0", "c1", "c2", "c3", "c4", "c5", "c6", "c7", "c8", "c9", "c10", "c11", "c12", "c13", "c14", "c15", "c16"]}
    dma("sp", cst, cst_d[:, :], [], [Bcst], dl["c0"])
    dma("sp", gmixT, gmixT_d[:, :], [], [Bsm], dl["c1"])
    dma("sp", gffnT, gffnT_d[:, :], [], [Bsm], dl["c1"])
    dma("sp", badaT, b_adaT[:, :], [], [Bsm], dl["c1"])
    dma("sp", convw, convwT_d[:, :], [], [Bsm], dl["c1"])
    dma("sp", convb, convbT_d[:, :], [], [Bsm], dl["c1"])
    dma("sp", bifb, bif_d[0:1, :].partition_broadcast(128), [], [Bsm], dl["c1"])
    dma("sp", flag, flag_d[:, :], [], [Bsm], dl["c1"])
    dma("sp", cTt, cT_d[:, :], [], [Bsm], dl["c1"])
    dma("sp", mlgT, mlgT_d[:, :], [], [Bsm], dl["c1"])
    dma("sp", retgT, retgT_d[:, :], [], [Bsm], dl["c1"])
    S.op("dve", lambda h: h.memset(epsc, EPS), [], [Bsm])
    cp(onesR, ones, [Bcst], [BonesR])
    dma("sp", gfin, gfin_d[0:1, :].partition_broadcast(128), [], [Bgfin], dl["c2"])
    dma("pool", wrt.rearrange("p (k n) -> p k n", n=NE), wr_d.rearrange("(k p) n -> p k n", p=128), [], [Bwrt], dl["c3"])
    dma("sp", brb, br_d[0:1, :].partition_broadcast(128), [], [Bbrb], dl["c4"])
    dma("sp", posi[:, 0:16], posp[:, :], [], [Bposi], dl["c6"])
    dma("sp", posi[:, 16:32], poso[:, :], [], [Bposi], dl["c6"])
    cp(posf, posi, [Bposi], [Bposf])

    gmb, Bgmb = T(D, name="gate_m_bc")
    Cml = [T(257, F32, "Cml%d" % h) for h in range(4)]
    CmlR = [T(258, F32R, "CmlR%d" % h) for h in range(4)]
    Cret = [T(256, F32, "Cret%d" % h) for h in range(4)]
    CretR = [T(256, F32R, "CretR%d" % h) for h in range(4)]
    rowA, BrowA = T(600, name="rows")
    bTr = rowA[0:4, 0:256]; Mr = rowA[0:4, 256:512]; mpr = rowA[0:4, 512:530]; Ar = rowA[0:4, 530:532]
    ext = rowA[0:4, 532:540]
    xg = [T(D, name="xg%d" % c) for c in range(2)]
    xs, Bxs = T(D, name="xs")
    h1T, Bh1T = T(8 * G, F32R, "h1T")
    h1T3 = h1T.rearrange("p (k n) -> p k n", n=G)
    NW = 3
    wst = [T(8 * 256, F32R, "wst%d" % i) for i in range(NW)]
    wsem = [S.dmasem("wst%d" % i) for i in range(NW)]
    wctr = [0]
    qkpre, Bqkpre = T(8 * 259, name="qkpre")
    qkpre3 = qkpre.rearrange("p (f n) -> p f n", n=259)
    cacc, Bcacc = T(G, name="cacc")
    qkT, BqkT = T(8 * G, F32R, "qkT")
    qkT3 = qkT.rearrange("p (f n) -> p f n", n=G)
    vml = [T(4 * 258, F32R, "vml%d" % c) for c in range(2)]
    oml = [T(D, name="oml%d" % c) for c in range(2)]
    ifb = [T(8, name="if%d" % c) for c in range(2)]
    rq = [T(512, name="rq%d" % c) for c in range(2)]
    rk = [T(512, name="rk%d" % c) for c in range(2)]
    rv = [T(D, F32R, "rv%d" % c) for c in range(2)]
    rg = [T(D, name="rg%d" % c) for c in range(2)]
    gsm, Bgsm = T(104, name="gsm")
    NMh, BNM = T(4 * G, name="NM")
    hmT, BhmT = T(8 * G, F32R, "hmT")
    hrT, BhrT = T(8 * G, F32R, "hrT")
    yT, ByT = T(8 * G, F32R, "yT")
    hmT3 = hmT.rearrange("p (k n) -> p k n", n=G); hrT3 = hrT.rearrange("p (k n) -> p k n", n=G)
    yT3 = yT.rearrange("p (k n) -> p k n", n=G)
    DTs = [T(128, name="DT%d" % h) for h in range(4)]
    PTs = [T(128, F32R, "PT%d" % h) for h in range(4)]
    kws = [T(128, F32R, "kw%d" % h) for h in range(4)]
    inss = [T(258, name="inter_s%d" % h) for h in range(4)]
    tots = [T(258, name="tot%d" % h) for h in range(4)]
    hhs = [(inss[h][0][:, 0:256], inss[h][1]) for h in range(4)]
    sths = [T(16, name="sth%d" % h) for h in range(4)]
    qdTs = kws
    st6, Bst6 = T(16, name="stats")
    rot, Brot = T(768, name="rot")
    sg2, Bsg2 = rot[:, 0:256], Brot
    sct = [T(512, name="sincos%d" % c) for c in range(2)]
    kint, Bkint = T(256, I32, "kint")
    rtmp, Brtmp = xs, Bxs
    qrTs = [T(512, F32R, "qrT%d" % c) for c in range(2)]
    krTs = [T(512, F32R, "krT%d" % c) for c in range(2)]
    sg1, Bsg1 = cacc, Bcacc
    dgt, Bdgt = DTs[0]
    x2sems = [S.dmasem("x2st%d" % c) for c in range(2)]
    xsem = [S.dmasem("xld%d" % c) for c in range(2)]
    MIX_END = top[0]

    for h in range(4):
        S.op("dve", lambda hh_, a=Cml[h][0]: hh_.memset(a, 0.0), [], [Cml[h][1]])
        cp(CmlR[h][0][:, 0:257], Cml[h][0], [Cml[h][1]], [CmlR[h][1]])
        cp(CmlR[h][0][:, 257:258], Cml[h][0][:, 0:1], [Cml[h][1]], [CmlR[h][1]])
        S.op("dve", lambda hh_, a=Cret[h][0]: hh_.memset(a, 0.0), [], [Cret[h][1]])
        cp(CretR[h][0], Cret[h][0], [Cret[h][1]], [CretR[h][1]])
    S.op("dve", lambda h: h.memset(qkpre, 0.0), [], [Bqkpre])
    S.op("dve", lambda h: h.memset(rowA, 0.0), [], [BrowA])
    for c in range(2):
        for q_ in range(0, 1032, 512):
            n_ = min(512, 1032 - q_)
            cp(vml[c][0][:, q_:q_ + n_], ones[:, 0:n_], [Bcst], [vml[c][1]])

    def wload(src3, ncols):
        i = wctr[0] % NW
        wctr[0] += 1
        ap, b = wst[i]
        v3 = ap[:, 0:8 * ncols].rearrange("p (k n) -> p k n", n=ncols)
        dma("pool", v3, src3, [], [b], wsem[i])
        return v3, b

    def win_tile(c0, ncols=256):
        return wload(w_in.rearrange("(k p) c -> p k c", p=128)[:, :, c0:c0 + ncols], ncols)

    act(cact2.rearrange("p (k t) -> p k t", t=2)[:, :, 0], cTt, AF.Silu, [Bsm], [Bcact])
    act(cact2.rearrange("p (k t) -> p k t", t=2)[:, :, 1], cTt, AF.Silu, [Bsm], [Bcact])
    cact3 = cact2.rearrange("p (k t) -> p k t", t=2)
    pm, Bpm = nextp()
    wada3 = w_ada.rearrange("(k p) c -> p k c", p=128)
    for j in range(6):
        for sb_ in range(4):
            w3, wb = wload(wada3[:, :, j * D + sb_ * 256: j * D + (sb_ + 1) * 256], 256)
            for ft in range(2):
                col = (j * 8 + sb_ * 2 + ft) * 2
                for kc in range(8):
                    mm(pm[:, col:col + 2], w3[:, kc, ft * 128:(ft + 1) * 128], cact3[:, kc, :], kc == 0, kc == 7,
                       [wb, Bcact], [Bpm])
    tt(modT, pm[:, 0:96].rearrange("p (c t) -> p c t", t=2)[:, :, 0], badaT, ALU.add, [Bpm, Bsm], [BmodT])
    stt(AB[:, 0:8], modT[:, 8:16], 1.0, gmixT, ALU.add, ALU.mult, [BmodT, Bsm], [BAB])
    cp(AB[:, 8:16], modT[:, 0:8], [BmodT], [BAB])
    stt(AB[:, 16:24], modT[:, 32:40], 1.0, gffnT, ALU.add, ALU.mult, [BmodT, Bsm], [BAB])
    cp(AB[:, 24:32], modT[:, 24:32], [BmodT], [BAB])
    A1 = AB[:, 0:8]; B1 = AB[:, 8:16]; A2 = AB[:, 16:24]; B2 = AB[:, 24:32]

    def bcast_vec(dst, Bdst, col0):
        for hf in range(2):
            pb, Bp = nextp()
            for k4 in range(4):
                kc = hf * 4 + k4
                ts(dgt, ident, modT[:, col0 + kc:col0 + kc + 1], None, ALU.mult, None, [Bcst, BmodT], [Bdgt])
                mm(pb[:, k4 * 128:(k4 + 1) * 128], ones[:, 0:128], dgt, True, True, [Bcst, Bdgt], [Bp])
            cp(dst[:, hf * 512:(hf + 1) * 512], pb, [Bp], [Bdst])

    bcast_vec(gmb, Bgmb, 16)
    bcast_vec(gfb, Bgfb, 40)

    def norm_to_T(xsrc, Bx, dstT3, BdstT, c, Acol, Bcol):
        act(xs, xsrc, AF.Square, [Bx], [Bxs, Bst6], accum=st6[:, 0:1])
        act(st6[:, 1:2], st6[:, 0:1], AF.Sqrt, [Bst6, Bsm], [Bst6], bias=epsc, scale=1.0 / D)
        S.op("dve", lambda h: h.reciprocal(st6[:, 2:3], st6[:, 1:2]), [Bst6], [Bst6])
        ts(xs, xsrc, st6[:, 2:3], None, ALU.mult, None, [Bx, Bst6], [Bxs])
        for hf in range(2):
            pb, Bp = nextp()
            for k4 in range(4):
                kc = hf * 4 + k4
                S.op("pe", lambda h, o=pb[:, k4 * 128:(k4 + 1) * 128], i=xs[:, kc * 128:(kc + 1) * 128]: h.transpose(o, i, ident),
                     [Bxs, Bcst], [Bp])
            for k4 in range(4):
                kc = hf * 4 + k4
                act(dstT3[:, kc, c * 128:(c + 1) * 128], pb[:, k4 * 128:(k4 + 1) * 128], AF.Identity, [Bp, BAB], [BdstT],
                    bias=Bcol[:, kc:kc + 1], scale=Acol[:, kc:kc + 1])

    def proj_fm(c0, nft, evac):
        for t in range(nft // 2):
            w3, wb = win_tile(c0 + t * 256)
            for f in range(2):
                pb, Bp = nextp()
                for kc in range(8):
                    mm(pb[:, 0:G], w3[:, kc, f * 128:(f + 1) * 128], h1T3[:, kc, :], kc == 0, kc == 7, [wb, Bh1T], [Bp])
                evac(t * 2 + f, pb[:, 0:G], Bp)

    def proj_tm(c0, ntile, evac, ncols=256):
        for t in range(ntile):
            w3, wb = win_tile(c0 + t * ncols, ncols)
            for c in range(2):
                pb, Bp = nextp()
                for kc in range(8):
                    mm(pb[:, 0:ncols], h1T3[:, kc, c * 128:(c + 1) * 128], w3[:, kc, :], kc == 0, kc == 7, [Bh1T, wb], [Bp])
                evac(t, c, pb[:, 0:ncols], Bp)

    def ln_stages(items):
        for hv, Bh, st, Bst in items:
            S.op("dve", lambda h, st=st, hv=hv: h.bn_stats(st[:, 4:10], hv), [Bh], [Bst])
        yield
        for hv, Bh, st, Bst in items:
            S.op("dve", lambda h, st=st: h.bn_aggr(st[:, 10:12], st[:, 4:10]), [Bst], [Bst])
        yield
        for hv, Bh, st, Bst in items:
            act(st[:, 12:13], st[:, 11:12], AF.Sqrt, [Bst, Bsm], [Bst], bias=epsc, scale=1.0)
        yield
        for hv, Bh, st, Bst in items:
            S.op("dve", lambda h, st=st: h.reciprocal(st[:, 13:14], st[:, 12:13]), [Bst], [Bst])
        yield
        for hv, Bh, st, Bst in items:
            ts(hv, hv, st[:, 10:11], st[:, 13:14], ALU.subtract, ALU.mult, [Bh, Bst], [Bh])
        yield

    GAM = [1.0 - 2.0 ** (-5.0 - h) for h in range(4)]

    for g in range(16):
        own = g >= 8
        xsrc = xo if own else xp
        t0 = (g - 8) * G if own else g * G
        for c in range(2):
            dma("sp", xg[c][0], xsrc[t0 + c * 128:t0 + (c + 1) * 128, :], [], [xg[c][1]], xsem[c])
            norm_to_T(xg[c][0], xg[c][1], h1T3, Bh1T, c, A1, B1)
        ang = rot[:, 0:256]; red = rot[:, 256:512]; kf = rot[:, 512:768]
        for c in range(2):
            pcol = (16 if own else 0) + (g % 8) * 2 + c
            ts(ang, cst[:, C_INVF:C_INVF + 256], posf[:, pcol:pcol + 1], None, ALU.mult, None, [Bcst, Bposf], [Brot])
            for dst_, off in ((sct[c][0][:, 0:256], 0.0), (sct[c][0][:, 256:512], math.pi / 2)):
                ts(red, ang, off, None, ALU.add, None, [Brot], [Brot])
                ts(kint, red, 1.0 / TWO_PI, None, ALU.mult, None, [Brot], [Bkint])
                cp(kf, kint, [Bkint], [Brot])
                stt(red, kf, -TWO_PI, red, ALU.mult, ALU.add, [Brot], [Brot])
                ts(red, red, 3.14159, -3.14159, ALU.min, ALU.max, [Brot], [Brot])
                act(dst_, red, AF.Sin, [Brot], [sct[c][1]])
        if g == 8:
            ts(qkpre, qkpre, flag, None, ALU.mult, None, [Bqkpre, Bsm], [Bqkpre])
            ts(mpr[:, 0:1], mpr[:, 0:1], flag[0:4, :], None, ALU.mult, None, [BrowA, Bsm], [BrowA])
            for h in range(4):
                ts(Cml[h][0], Cml[h][0], flag, None, ALU.mult, None, [Cml[h][1], Bsm], [Cml[h][1]])
                cp(CmlR[h][0][:, 0:257], Cml[h][0], [Cml[h][1]], [CmlR[h][1]])
                ts(Cret[h][0], Cret[h][0], flag, None, ALU.mult, None, [Cret[h][1], Bsm], [Cret[h][1]])
                cp(CretR[h][0], Cret[h][0], [Cret[h][1]], [CretR[h][1]])

        need_q = own or g == 7

        def ev_qk(base):
            def f(ft, ps_, Bp):
                cp(qkpre3[:, base + ft, 3:259], ps_, [Bp], [Bqkpre], eng="act")
            return f
        proj_fm(512, 4, ev_qk(4))
        if need_q:
            proj_fm(0, 4, ev_qk(0))
        for ft in (range(8) if need_q else range(4, 8)):
            ts(cacc, qkpre3[:, ft, 0:G], convw[:, ft * 4:ft * 4 + 1], None, ALU.mult, None, [Bqkpre, Bsm], [Bcacc])
            for i in range(1, 4):
                stt(cacc, qkpre3[:, ft, i:i + G], convw[:, ft * 4 + i:ft * 4 + i + 1], cacc, ALU.mult, ALU.add,
                    [Bqkpre, Bsm, Bcacc], [Bcacc])
            act(qkT3[:, ft, :], cacc, AF.Silu, [Bcacc, Bsm], [BqkT], bias=convb[:, ft:ft + 1])
            cp(qkpre3[:, ft, 0:3], qkpre3[:, ft, G:G + 3], [Bqkpre], [Bqkpre])

        def ev_v(t, c, ps_, Bp):
            cp(vml[c][0][:, t * 258:t * 258 + 256], ps_, [Bp], [vml[c][1]], eng="act")
        proj_tm(1024, 4, ev_v)

        def ev_if(t, c, ps_, Bp):
            tt(ifb[c][0], ps_, bifb, ALU.add, [Bp, Bsm], [ifb[c][1]])
        proj_tm(3072, 1, ev_if, ncols=8)
        if own:
            def ev_o(t, c, ps_, Bp):
                act(oml[c][0][:, t * 256:(t + 1) * 256], ps_, AF.Sigmoid, [Bp], [oml[c][1]])
            proj_tm(2048, 4, ev_o)

        lf = gsm[:, 0:8]; a_ = gsm[:, 8:16]; b_ = gsm[:, 16:24]; tmp8 = gsm[:, 24:32]; bb = gsm[:, 32:40]
        u_ = gsm[:, 40:48]; isc = gsm[:, 48:56]; emt = gsm[:, 56:64]; spv = gsm[:, 64:72]; MT = gsm[:, 72:80]
        MLb = gsm[:, 80:88]; MPb = gsm[:, 88:96]; tmp8b = gsm[:, 96:104]
        for c in range(2):
            fp = ifb[c][0][:, 4:8]
            act(tmp8[:, c * 4:c * 4 + 4], fp, AF.Abs, [ifb[c][1]], [Bgsm])
            act(tmp8[:, c * 4:c * 4 + 4], tmp8[:, c * 4:c * 4 + 4], AF.Exp, [Bgsm], [Bgsm], scale=-1.0)
            act(tmp8[:, c * 4:c * 4 + 4], tmp8[:, c * 4:c * 4 + 4], AF.Ln, [Bgsm], [Bgsm], bias=1.0)
            ts(lf[:, c * 4:c * 4 + 4], fp, 0.0, None, ALU.min, None, [ifb[c][1]], [Bgsm])
            tt(lf[:, c * 4:c * 4 + 4], lf[:, c * 4:c * 4 + 4], tmp8[:, c * 4:c * 4 + 4], ALU.subtract, [Bgsm], [Bgsm])
        pa, Bpa = nextp()
        mm(pa[:, 0:8], tri, lf, True, True, [Bcst, Bgsm], [Bpa])
        for c in range(2):
            mm(pa[0:4, 16 + c:17 + c], lf[:, c * 4:c * 4 + 4], ones[:, 0:1], True, True, [Bgsm, Bcst], [Bpa])
        cp(a_, pa[:, 0:8], [Bpa], [Bgsm])
        cp(Ar, pa[0:4, 16:18], [Bpa], [BrowA])
        for c in range(2):
            tt(b_[:, c * 4:c * 4 + 4], ifb[c][0][:, 0:4], a_[:, c * 4:c * 4 + 4], ALU.subtract, [ifb[c][1], Bgsm], [Bgsm])
        pt_, Bpt = nextp()
        for c in range(2):
            S.op("pe", lambda h, o=pt_[0:4, c * 128:(c + 1) * 128], i=b_[:, c * 4:c * 4 + 4]: h.transpose(o, i, ident),
                 [Bgsm, Bcst], [Bpt])
        cp(bTr, pt_[0:4, 0:256], [Bpt], [BrowA])
        for c in range(2):
            ci = (g % 8) * 2 + c if False else c
            S.op("dve", lambda h, o=Mr[:, c * 128:(c + 1) * 128], d=bTr[:, c * 128:(c + 1) * 128], ini=mpr[:, c:c + 1]:
                 h.tensor_tensor_scan(o, d, d, ini, ALU.max, ALU.max), [BrowA], [BrowA])
            tt(mpr[:, c + 1:c + 2], Ar[:, c:c + 1], Mr[:, c * 128 + 127:c * 128 + 128], ALU.add, [BrowA], [BrowA])
            cp(ext[:, c:c + 1], Mr[:, c * 128 + 127:c * 128 + 128], [BrowA], [BrowA])
            cp(ext[:, 2 + c:3 + c], mpr[:, c:c + 1], [BrowA], [BrowA])
        cp(mpr[:, 0:1], mpr[:, 2:3], [BrowA], [BrowA])
        pe_, Bpe = nextp()
        for h in range(4):
            pb, Bp = nextp()
            mm(pb[:, 0:G], cst[0:4, C_SEL + h * 128:C_SEL + (h + 1) * 128], Mr, True, True, [Bcst, BrowA], [Bp])
            for c in range(2):
                tt(NMh[:, h * G + c * 128:h * G + (c + 1) * 128], mneg, pb[:, c * 128:(c + 1) * 128], ALU.subtract,
                   [Bcst, Bp], [BNM])
            mm(pe_[:, h * 4:h * 4 + 4], cst[0:4, C_SEL + h * 128:C_SEL + (h + 1) * 128], ext[:, 0:4], True, True,
               [Bcst, BrowA], [Bpe])
        for c in range(2):
            S.op("pe", lambda h, o=pe_[:, 32 + c * 4:36 + c * 4], i=Mr[:, c * 128:(c + 1) * 128]: h.transpose(o, i, ident[0:4, 0:4]),
                 [BrowA, Bcst], [Bpe])
        cp(MT, pe_[:, 32:40], [Bpe], [Bgsm])
        pe3 = pe_[:, 0:16].rearrange("p (h f) -> p h f", f=4)
        for c in range(2):
            cp(MLb[:, c * 4:c * 4 + 4], pe3[:, :, c], [Bpe], [Bgsm])
            cp(MPb[:, c * 4:c * 4 + 4], pe3[:, :, 2 + c], [Bpe], [Bgsm])
        ts(bb, b_, LNSCALE, None, ALU.add, None, [Bgsm], [Bgsm])
        tt(tmp8, b_, MLb, ALU.subtract, [Bgsm], [Bgsm])
        act(u_, tmp8, AF.Exp, [Bgsm], [Bgsm])
        tt(tmp8, MPb, MLb, ALU.subtract, [Bgsm], [Bgsm])
        act(spv, tmp8, AF.Exp, [Bgsm], [Bgsm])
        if own:
            tt(tmp8, MPb, MT, ALU.subtract, [Bgsm], [Bgsm])
            act(isc, tmp8, AF.Exp, [Bgsm], [Bgsm], bias=LNSCALE)
            tt(tmp8b, a_, MT, ALU.add, [Bgsm], [Bgsm])
            act(emt, tmp8b, AF.Exp, [Bgsm], [Bgsm], scale=-1.0)

        def ev_rk(t, c, ps_, Bp):
            cp(rk[c][0][:, t * 256:(t + 1) * 256], ps_, [Bp], [rk[c][1]], eng="act")
        proj_tm(3592, 2, ev_rk)

        def ev_rv(t, c, ps_, Bp):
            cp(rv[c][0][:, t * 256:(t + 1) * 256], ps_, [Bp], [rv[c][1]], eng="act")
        proj_tm(4104, 4, ev_rv)
        if own:
            def ev_rq(t, c, ps_, Bp):
                cp(rq[c][0][:, t * 256:(t + 1) * 256], ps_, [Bp], [rq[c][1]], eng="act")
            proj_tm(3080, 2, ev_rq)

            def ev_rg(t, c, ps_, Bp):
                act(rg[c][0][:, t * 256:(t + 1) * 256], ps_, AF.Silu, [Bp], [rg[c][1]])
            proj_tm(5128, 4, ev_rg)

        H4 = range(4)

        def rotary(src, Bsrc, c):
            s3 = sct[c][0][:, 0:256].rearrange("p (h d) -> p h d", d=64)
            c3 = sct[c][0][:, 256:512].rearrange("p (h d) -> p h d", d=64)
            Bsc = sct[c][1]
            x4 = src.rearrange("p (h t d) -> p h t d", t=2, d=64)
            r4 = rtmp[:, 0:512].rearrange("p (h t d) -> p h t d", t=2, d=64)
            q4 = rtmp[:, 512:1024].rearrange("p (h t d) -> p h t d", t=2, d=64)
            tt(r4[:, :, 0, :], x4[:, :, 0, :], c3, ALU.mult, [Bsrc, Bsc], [Brtmp])
            tt(r4[:, :, 1, :], x4[:, :, 1, :], c3, ALU.mult, [Bsrc, Bsc], [Brtmp])
            tt(q4[:, :, 0, :], x4[:, :, 1, :], s3, ALU.mult, [Bsrc, Bsc], [Brtmp])
            tt(q4[:, :, 1, :], x4[:, :, 0, :], s3, ALU.mult, [Bsrc, Bsc], [Brtmp])
            tt(x4[:, :, 0, :], r4[:, :, 0, :], q4[:, :, 0, :], ALU.subtract, [Brtmp], [Bsrc])
            tt(x4[:, :, 1, :], r4[:, :, 1, :], q4[:, :, 1, :], ALU.add, [Brtmp], [Bsrc])
        for c in range(2):
            rotary(rk[c][0], rk[c][1], c)
            if own:
                rotary(rq[c][0], rq[c][1], c)
        def ml_gen(c):
            kTs = [qkT3[:, 4 + h, c * 128:(c + 1) * 128] for h in H4]
            qTs = [qkT3[:, h, c * 128:(c + 1) * 128] for h in H4]
            vxs = [vml[c][0][:, h * 258:h * 258 + 258] for h in H4]
            cols = [c * 4 + h for h in H4]
            if own:
                pS = [nextp() for h in H4]
                for h in H4:
                    mm(pS[h][0][:, 0:128], kTs[h], qTs[h], True, True, [BqkT], [pS[h][1]])
                yield
                for h in H4:
                    act(DTs[h][0], NMh[:, h * G + c * 128:h * G + (c + 1) * 128], AF.Exp, [BNM, Bgsm], [DTs[h][1]],
                        bias=bb[:, cols[h]:cols[h] + 1])
                yield
                for h in H4:
                    tt(PTs[h][0], pS[h][0][:, 0:128], DTs[h][0], ALU.mult, [pS[h][1], DTs[h][1]], [PTs[h][1]])
                yield
                pI = [nextp() for h in H4]
                for h in H4:
                    mm(pI[h][0][:, 0:258], PTs[h][0], vxs[h], True, True, [PTs[h][1], vml[c][1]], [pI[h][1]])
                yield
                pJ = [nextp() for h in H4]
                for h in H4:
                    mm(pJ[h][0][:, 0:258], qTs[h], CmlR[h][0], True, True, [BqkT, CmlR[h][1]], [pJ[h][1]])
                yield
                for h in H4:
                    act(inss[h][0], pJ[h][0][:, 0:258], AF.Copy, [pJ[h][1], Bgsm], [inss[h][1]], scale=isc[:, cols[h]:cols[h] + 1])
                yield
                for h in H4:
                    tt(tots[h][0], pI[h][0][:, 0:258], inss[h][0], ALU.add, [pI[h][1], inss[h][1]], [tots[h][1]])
                yield
                for h in H4:
                    act(sths[h][0][:, 14:15], tots[h][0][:, 256:257], AF.Abs, [tots[h][1]], [sths[h][1]])
                yield
                for h in H4:
                    ts(sths[h][0][:, 14:15], sths[h][0][:, 14:15], emt[:, cols[h]:cols[h] + 1], None, ALU.max, None,
                       [sths[h][1], Bgsm], [sths[h][1]])
                yield
                for h in H4:
                    S.op("dve", lambda hd, st=sths[h][0]: hd.reciprocal(st[:, 15:16], st[:, 14:15]), [sths[h][1]], [sths[h][1]])
                yield
                for h in H4:
                    ts(hhs[h][0], tots[h][0][:, 0:256], sths[h][0][:, 15:16], None, ALU.mult, None, [tots[h][1], sths[h][1]], [hhs[h][1]])
                yield
                yield from ln_stages([(hhs[h][0], hhs[h][1], sths[h][0], sths[h][1]) for h in H4])
                for h in H4:
                    osl = oml[c][0][:, h * 256:(h + 1) * 256]
                    tt(osl, osl, hhs[h][0], ALU.mult, [oml[c][1], hhs[h][1]], [oml[c][1]])
            pK = [nextp() for h in H4]
            for h in H4:
                S.op("pe", lambda hd, o=pK[h][0][:, 0:128], i=kTs[h].bitcast(F32): hd.transpose(o, i, ident), [BqkT, Bcst], [pK[h][1]])
            yield
            for h in H4:
                act(kws[h][0], pK[h][0][:, 0:128], AF.Copy, [pK[h][1], Bgsm], [kws[h][1]], scale=u_[:, cols[h]:cols[h] + 1])
            yield
            pC = [nextp() for h in H4]
            for h in H4:
                mm(pC[h][0][:, 0:258], kws[h][0], vxs[h], True, True, [kws[h][1], vml[c][1]], [pC[h][1]])
            yield
            for h in H4:
                stt(Cml[h][0], Cml[h][0], spv[:, cols[h]:cols[h] + 1], pC[h][0][:, 0:257], ALU.mult, ALU.add,
                    [Cml[h][1], Bgsm, pC[h][1]], [Cml[h][1]])
            yield
            for h in H4:
                cp(CmlR[h][0][:, 0:257], Cml[h][0], [Cml[h][1]], [CmlR[h][1]], eng="act")

            yield

        def ret_gen(c):
            vvs = [rv[c][0][:, h * 256:(h + 1) * 256] for h in H4]
            if own:
                qrT, BqrT = qrTs[c]
                krT, BkrT = krTs[c]
                pq, Bpq = nextp()
                pk_, Bpk = nextp()
                for h in H4:
                    S.op("pe", lambda hd, o=pq[:, h * 128:(h + 1) * 128], i=rq[c][0][:, h * 128:(h + 1) * 128]: hd.transpose(o, i, ident),
                         [rq[c][1], Bcst], [Bpq])
                yield
                for h in H4:
                    S.op("pe", lambda hd, o=pk_[:, h * 128:(h + 1) * 128], i=rk[c][0][:, h * 128:(h + 1) * 128]: hd.transpose(o, i, ident),
                         [rk[c][1], Bcst], [Bpk])
                yield
                cp(qrT, pq, [Bpq], [BqrT], eng="act")
                cp(krT, pk_, [Bpk], [BkrT], eng="act")
                pS = [nextp() for h in H4]
                for h in H4:
                    mm(pS[h][0][:, 0:128], krT[:, h * 128:(h + 1) * 128], qrT[:, h * 128:(h + 1) * 128], True, True, [BkrT, BqrT], [pS[h][1]])
                yield
                for h in H4:
                    tt(PTs[h][0], pS[h][0][:, 0:128], cst[:, C_DECT + h * 128:C_DECT + (h + 1) * 128], ALU.mult, [pS[h][1], Bcst], [PTs[h][1]])
                yield
                for h in H4:
                    tt(qdTs[h][0], qrT[:, h * 128:(h + 1) * 128].bitcast(F32), cst[:, C_QD + h * 128:C_QD + (h + 1) * 128], ALU.mult,
                       [BqrT, Bcst], [qdTs[h][1]])
                yield
                pI = [nextp() for h in H4]
                for h in H4:
                    mm(pI[h][0][:, 0:256], PTs[h][0], vvs[h], True, False, [PTs[h][1], rv[c][1]], [pI[h][1]])
                    mm(pI[h][0][:, 0:256], qdTs[h][0], CretR[h][0], False, True, [qdTs[h][1], CretR[h][1]], [pI[h][1]])
                yield
                for h in H4:
                    cp(hhs[h][0], pI[h][0][:, 0:256], [pI[h][1]], [hhs[h][1]], eng="act")
                yield
                yield from ln_stages([(hhs[h][0], hhs[h][1], sths[h][0], sths[h][1]) for h in H4])
                for h in H4:
                    gsl = rg[c][0][:, h * 256:(h + 1) * 256]
                    tt(gsl, gsl, hhs[h][0], ALU.mult, [rg[c][1], hhs[h][1]], [rg[c][1]])
            for h in H4:
                ts(kws[h][0], rk[c][0][:, h * 128:(h + 1) * 128], cst[:, C_KDEC + h:C_KDEC + h + 1], None, ALU.mult, None,
                   [rk[c][1], Bcst], [kws[h][1]])
            yield
            pC = [nextp() for h in H4]
            for h in H4:
                mm(pC[h][0][:, 0:256], kws[h][0], vvs[h], True, True, [kws[h][1], rv[c][1]], [pC[h][1]])
            yield
            for h in H4:
                stt(Cret[h][0], Cret[h][0], GAM[h] ** 128, pC[h][0][:, 0:256], ALU.mult, ALU.add, [Cret[h][1], pC[h][1]], [Cret[h][1]])
            yield
            for h in H4:
                cp(CretR[h][0], Cret[h][0], [Cret[h][1]], [CretR[h][1]], eng="act")

            yield

        for c in range(2):
            for g_ in (ml_gen(c), ret_gen(c)):
                for _ in g_:
                    pass

        if not own:
            continue
        for c in range(2):
            for (src, Bsrc, dst3, Bdst, gT) in ((oml[c][0], oml[c][1], hmT3, BhmT, mlgT), (rg[c][0], rg[c][1], hrT3, BhrT, retgT)):
                for hf in range(2):
                    pb, Bp = nextp()
                    for k4 in range(4):
                        kc = hf * 4 + k4
                        S.op("pe", lambda hd, o=pb[:, k4 * 128:(k4 + 1) * 128], i=src[:, kc * 128:(kc + 1) * 128]: hd.transpose(o, i, ident),
                             [Bsrc, Bcst], [Bp])
                    for k4 in range(4):
                        kc = hf * 4 + k4
                        act(dst3[:, kc, c * 128:(c + 1) * 128], pb[:, k4 * 128:(k4 + 1) * 128], AF.Copy, [Bp, Bsm], [Bdst],
                            scale=gT[:, kc:kc + 1])
        for t in range(4):
            wm3, wmb = wload(wbml.rearrange("(k p) c -> p k c", p=128)[:, :, t * 256:(t + 1) * 256], 256)
            pbm = []
            for f in range(2):
                pb, Bp = nextp()
                for kc in range(8):
                    mm(pb[:, 0:G], wm3[:, kc, f * 128:(f + 1) * 128], hmT3[:, kc, :], kc == 0, kc == 7, [wmb, BhmT], [Bp])
                pbm.append((pb, Bp))
            wg3, wgb = win_tile(6152 + t * 256)
            for f in range(2):
                pb, Bp = nextp()
                for kc in range(8):
                    mm(pb[:, 0:G], wg3[:, kc, f * 128:(f + 1) * 128], h1T3[:, kc, :], kc == 0, kc == 7, [wgb, Bh1T], [Bp])
                act(sg1, pb[:, 0:G], AF.Sigmoid, [Bp], [Bsg1])
                tt(yT3[:, t * 2 + f, :], pbm[f][0][:, 0:G], sg1, ALU.mult, [pbm[f][1], Bsg1], [ByT])
            wr3, wrb = wload(wbret.rearrange("(k p) c -> p k c", p=128)[:, :, t * 256:(t + 1) * 256], 256)
            pbr = []
            for f in range(2):
                pb, Bp = nextp()
                for kc in range(8):
                    mm(pb[:, 0:G], wr3[:, kc, f * 128:(f + 1) * 128], hrT3[:, kc, :], kc == 0, kc == 7, [wrb, BhrT], [Bp])
                pbr.append((pb, Bp))
            wg3, wgb = win_tile(7176 + t * 256)
            for f in range(2):
                pb, Bp = nextp()
                for kc in range(8):
                    mm(pb[:, 0:G], wg3[:, kc, f * 128:(f + 1) * 128], h1T3[:, kc, :], kc == 0, kc == 7, [wgb, Bh1T], [Bp])
                act(sg1, pb[:, 0:G], AF.Sigmoid, [Bp], [Bsg1])
                tt(sg2, pbr[f][0][:, 0:G], sg1, ALU.mult, [pbr[f][1], Bsg1], [Bsg2])
                tt(yT3[:, t * 2 + f, :], yT3[:, t * 2 + f, :].bitcast(F32), sg2, ALU.add, [ByT, Bsg2], [ByT])
        for t in range(4):
            wo3, wob = wload(wout.rearrange("(k p) c -> p k c", p=128)[:, :, t * 256:(t + 1) * 256], 256)
            for c in range(2):
                pb, Bp = nextp()
                for kc in range(8):
                    mm(pb[:, 0:256], yT3[:, kc, c * 128:(c + 1) * 128], wo3[:, kc, :], kc == 0, kc == 7, [ByT, wob], [Bp])
                xsl = xg[c][0][:, t * 256:(t + 1) * 256]
                tt(sg2, pb[:, 0:256], gmb[:, t * 256:(t + 1) * 256], ALU.mult, [Bp, Bgmb], [Bsg2])
                tt(xsl, xsl, sg2, ALU.add, [xg[c][1], Bsg2], [xg[c][1]])
        for c in range(2):
            dma("sp", x2s[t0 + c * 128:t0 + (c + 1) * 128, :], xg[c][0], [xg[c][1]], [], x2sems[c])

    S.barrier()
    top[0] = PERS_END
    BLK = 512
    NBK = (TOK * 4) // BLK + NE
    NSLOT = NBK * BLK
    Xs = nc.dram_tensor("Xs", [NSLOT, D], F32, kind="Internal").ap()
    Ys = nc.dram_tensor("Ys", [NSLOT, D], F32, kind="Internal").ap()
    H2 = nc.dram_tensor("H2", [TOK, D], F32, kind="Internal").ap()
    A2bc, BA2bc = T(D, name="A2bc")
    B2bc, BB2bc = T(D, name="B2bc")
    xc, Bxc = T(D, name="xc")
    xs2, Bxs2 = T(D, name="xs2")
    h2tm, Bh2tm = T(D, name="h2tm")
    h2Tc, Bh2Tc = T(D, F32R, "h2Tc")
    h2Tc3 = h2Tc.rearrange("p (k n) -> p k n", n=128)
    st2, Bst2 = T(16, name="st2")
    lgt, Blgt = T(NE, name="lgt")
    t8, Bt8 = T(16, name="t8")
    maskall, Bmask = T(16 * NE, name="maskall")
    Gwall, BGw = T(16 * NE, name="Gwall")
    tris, Btris = T(128, name="tristrict")
    cntb, Bcnt = T(NE, name="cnt")
    nbi, Bnbi = T(NE, I32, "nbi")
    nbf, Bnbf = T(NE, name="nbf")
    bend, Bbend = T(NE, name="bend")
    sbase, Bsbase = T(NE, name="sbase")
    ones32, Bones32 = T(NE, name="ones32")
    key, Bkey = T(NE, name="key")
    eqt, Beqt = T(NE, name="eqt")
    s4, Bs4 = T(16, name="s4")
    sidxf, Bsidxf = T(64, name="sidxf")
    w4, Bw4 = T(64, name="w4")
    ebrow, Beb = T(NBK, name="ebrow")
    widxf, Bwidxf = T(NBK * 8, name="widxf")
    didxf, Bdidxf = T(NBK * 4, name="didxf")
    bidxf, Bbidxf = T(NBK * 2, name="bidxf")
    Xtm, BXtm = T(4 * D, name="Xtm")
    Xtm3 = Xtm.rearrange("p (j c) -> p j c", c=D)
    XT, BXT = T(8 * BLK, F32R, "XT")
    XT3 = XT.rearrange("p (k n) -> p k n", n=BLK)
    actT, BactT = T(8 * BLK, F32R, "actT")
    actT3 = actT.rearrange("p (k n) -> p k n", n=BLK)
    NU = 3
    wgt = [T(8 * 256, F32R, "wgu%d" % i) for i in range(NU)]
    wgsem = [S.dmasem("wgu%d" % i) for i in range(NU)]
    wq = [T(2 * 1024, F32R, "wd%d" % q) for q in range(4)]
    wq3 = [wq[q][0].rearrange("p (k n) -> p k n", n=1024) for q in range(4)]
    wqsem = [S.dmasem("wd%d" % q) for q in range(4)]
    bgt = [T(16, name="bgt%d" % i) for i in range(2)]
    bgsem = [S.dmasem("bgt%d" % i) for i in range(2)]
    bdb = [T(D, name="bdb%d" % i) for i in range(2)]
    bdsem = [S.dmasem("bdb%d" % i) for i in range(2)]
    gm_ = [T(512, name="gm%d" % i) for i in range(2)]
    sg_ = [T(512, name="sg%d" % i) for i in range(2)]
    lm_ = [T(512, name="lm%d" % i) for i in range(2)]
    dsb = [T(D, name="dsb%d" % i) for i in range(2)]
    acc, Bacc = T(D, name="acc")
    xcsem = S.dmasem("xc")
    h2sem = S.dmasem("h2st")
    h2lsem = S.dmasem("h2ld")
    scsem = S.dmasem("scat")
    xtsem = S.dmasem("xtm")
    yss = [S.dmasem("ysst%d" % i) for i in range(2)]
    ygsem = S.dmasem("ygat")
    osem = S.dmasem("ost")
    uctr = [0]
    sctr = [0]
    wguh = wgu.rearrange("e (u q) c -> (e u q) c", u=8)
    wdh = wd.rearrange("e (q r) c -> (e q r) c", q=4)

    def ind(ap_):
        return bass.IndirectOffsetOnAxis(ap=ap_, axis=0)

    _bregs = {}

    def bnd(h, v):
        if v not in _bregs:
            _bregs[v] = h.to_reg(v)
        return _bregs[v]

    NIT = 40
    itl = [T(1, I32, "idx%d" % i) for i in range(NIT)]
    ictr = [0]

    def idx_tile(colap, Bsrc):
        i = ictr[0] % NIT
        ictr[0] += 1
        ap_, b_ = itl[i]
        cp(ap_, colap, [Bsrc], [b_])
        return ap_, b_

    def block_idx(b):
        d = {}
        d["bg"] = idx_tile(bidxf[:, b:b + 1], Bbidxf)
        d["bd"] = idx_tile(bidxf[:, NBK + b:NBK + b + 1], Bbidxf)
        for u in range(8):
            d["w%d" % u] = idx_tile(widxf[:, b * 8 + u:b * 8 + u + 1], Bwidxf)
        for q in range(4):
            d["d%d" % q] = idx_tile(didxf[:, b * 4 + q:b * 4 + q + 1], Bdidxf)
        return d

    def bcast_cols(dst, Bdst, colap, Bcol):
        for hf in range(2):
            pb, Bp = nextp()
            for k4 in range(4):
                kc = hf * 4 + k4
                ts(dgt, ident, colap[:, kc:kc + 1], None, ALU.mult, None, [Bcst, Bcol], [Bdgt])
                mm(pb[:, k4 * 128:(k4 + 1) * 128], ones[:, 0:128], dgt, True, True, [Bcst, Bdgt], [Bp])
            cp(dst[:, hf * 512:(hf + 1) * 512], pb, [Bp], [Bdst])
    dgt, Bdgt = T(128, name="dgt2")
    bcast_cols(A2bc, BA2bc, A2, BAB)
    bcast_cols(B2bc, BB2bc, B2, BAB)
    tt(tris, tri, ident, ALU.subtract, [Bcst], [Btris])
    S.op("dve", lambda h: h.memset(ones32, 1.0), [], [Bones32])
    wrt3 = wrt.rearrange("p (k n) -> p k n", n=NE)

    for c in range(16):
        dma("sp", xc, x2s[c * 128:(c + 1) * 128, :], [], [Bxc], xcsem)
        act(xs2, xc, AF.Square, [Bxc], [Bxs2, Bst2], accum=st2[:, 0:1])
        act(st2[:, 1:2], st2[:, 0:1], AF.Sqrt, [Bst2, Bsm], [Bst2], bias=epsc, scale=1.0 / D)
        S.op("dve", lambda h: h.reciprocal(st2[:, 2:3], st2[:, 1:2]), [Bst2], [Bst2])
        ts(xs2, xc, st2[:, 2:3], None, ALU.mult, None, [Bxc, Bst2], [Bxs2])
        tt(h2tm, xs2, A2bc, ALU.mult, [Bxs2, BA2bc], [Bh2tm])
        tt(h2tm, h2tm, B2bc, ALU.add, [Bh2tm, BB2bc], [Bh2tm])
        dma("sp", H2[c * 128:(c + 1) * 128, :], h2tm, [Bh2tm], [], h2sem)
        for hf in range(2):
            pb, Bp = nextp()
            for k4 in range(4):
                kc = hf * 4 + k4
                S.op("pe", lambda h, o=pb[:, k4 * 128:(k4 + 1) * 128], i=h2tm[:, kc * 128:(kc + 1) * 128]: h.transpose(o, i, ident),
                     [Bh2tm, Bcst], [Bp])
            cp(h2Tc[:, hf * 512:(hf + 1) * 512], pb, [Bp], [Bh2Tc], eng="act")
        pl, Bpl = nextp()
        for kc in range(8):
            mm(pl[:, 0:NE], h2Tc3[:, kc, :], wrt3[:, kc, :], kc == 0, kc == 7, [Bh2Tc, Bwrt], [Bpl])
        tt(lgt, pl[:, 0:NE], brb, ALU.add, [Bpl, Bbrb], [Blgt])
        S.op("dve", lambda h: h.max(t8[:, 0:8], lgt), [Blgt], [Bt8])
        ts(maskall[:, c * NE:(c + 1) * NE], lgt, t8[:, 3:4], None, ALU.is_ge, None, [Blgt, Bt8], [Bmask])
        ts(t8[:, 8:9], t8[:, 0:1], -1.0, None, ALU.mult, None, [Bt8], [Bt8])
        act(lgt, lgt, AF.Exp, [Blgt, Bt8], [Blgt], bias=t8[:, 8:9])
        tt(lgt, lgt, maskall[:, c * NE:(c + 1) * NE], ALU.mult, [Blgt, Bmask], [Blgt])
        S.op("dve", lambda h: h.reduce_sum(t8[:, 9:10], lgt, mybir.AxisListType.X), [Blgt], [Bt8])
        S.op("dve", lambda h: h.reciprocal(t8[:, 10:11], t8[:, 9:10]), [Bt8], [Bt8])
        ts(Gwall[:, c * NE:(c + 1) * NE], lgt, t8[:, 10:11], None, ALU.mult, None, [Blgt, Bt8], [BGw])

    pcn, Bpcn = nextp()
    for c in range(16):
        mm(pcn[:, 0:NE], ones[:, 0:128], maskall[:, c * NE:(c + 1) * NE], c == 0, c == 15, [Bcst, Bmask], [Bpcn])
    cp(cntb, pcn[:, 0:NE], [Bpcn], [Bcnt])
    ts(nbi, cntb, 1.0 / BLK, (BLK - 1.0) / BLK - 0.49951171875, ALU.mult, ALU.add, [Bcnt], [Bnbi])
    cp(nbf, nbi, [Bnbi], [Bnbf])
    S.op("dve", lambda h: h.tensor_tensor_scan(bend, ones32, nbf, 0.0, ALU.mult, ALU.add), [Bones32, Bnbf], [Bbend])
    tt(sbase, bend, nbf, ALU.subtract, [Bbend, Bnbf], [Bsbase])
    ts(sbase, sbase, float(BLK), None, ALU.mult, None, [Bsbase], [Bsbase])
    iob = cst[:, C_IOB:C_IOB + NBK]
    S.op("dve", lambda h: h.memset(ebrow, 0.0), [], [Beb])
    for e in range(NE):
        stt(ebrow, iob, bend[:, e:e + 1], ebrow, ALU.is_ge, ALU.add, [Bcst, Bbend, Beb], [Beb])
    ts(ebrow, ebrow, float(NE - 1), None, ALU.min, None, [Beb], [Beb])
    widxf3 = widxf.rearrange("p (b u) -> p b u", u=8)
    for u in range(8):
        ts(widxf3[:, :, u], ebrow, 1024.0, cst[:, C_CU + u:C_CU + u + 1], ALU.mult, ALU.add, [Beb, Bcst], [Bwidxf])
    didxf3 = didxf.rearrange("p (b q) -> p b q", q=4)
    for q in range(4):
        ts(didxf3[:, :, q], ebrow, 512.0, cst[:, C_CU + q:C_CU + q + 1], ALU.mult, ALU.add, [Beb, Bcst], [Bdidxf])
    ts(bidxf[:, 0:NBK], ebrow, 128.0, cst[:, C_CU:C_CU + 1], ALU.mult, ALU.add, [Beb, Bcst], [Bbidxf])
    cp(bidxf[:, NBK:2 * NBK], ebrow, [Beb], [Bbidxf])

    prk, Bprk = nextp()
    for c in range(16):
        for c2 in range(c):
            mm(prk[:, c * NE:(c + 1) * NE], ones[:, 0:128], maskall[:, c2 * NE:(c2 + 1) * NE], c2 == 0, False, [Bcst, Bmask], [Bprk])
        mm(prk[:, c * NE:(c + 1) * NE], tris, maskall[:, c * NE:(c + 1) * NE], c == 0, True, [Btris, Bmask], [Bprk])
    for c in range(16):
        mk = maskall[:, c * NE:(c + 1) * NE]
        tt(key, prk[:, c * NE:(c + 1) * NE], sbase, ALU.add, [Bprk, Bsbase], [Bkey])
        ts(key, key, 1.0, None, ALU.add, None, [Bkey], [Bkey])
        tt(key, key, mk, ALU.mult, [Bkey, Bmask], [Bkey])
        S.op("dve", lambda h: h.max(s4[:, 0:8], key), [Bkey], [Bs4])
        ts(sidxf[:, c * 4:(c + 1) * 4], s4[:, 0:4], -1.0, None, ALU.add, None, [Bs4], [Bsidxf])
        for k in range(4):
            ts(eqt, key, s4[:, k:k + 1], None, ALU.is_equal, None, [Bkey, Bs4], [Beqt])
            tt(eqt, eqt, Gwall[:, c * NE:(c + 1) * NE], ALU.mult, [Beqt, BGw], [Beqt])
            S.op("dve", lambda h, o=w4[:, c * 4 + k:c * 4 + k + 1]: h.reduce_sum(o, eqt, mybir.AxisListType.X), [Beqt], [Bw4])
    lasth2 = S.last[("dma", id(h2sem))]
    for c in range(16):
        S.op("sp", lambda h, c=c: h.dma_start(out=h2tm, in_=H2[c * 128:(c + 1) * 128, :]), [], [Bh2tm], dma=h2lsem, extra=[lasth2])
        for k in range(4):
            ia, Bia = idx_tile(sidxf[:, c * 4 + k:c * 4 + k + 1], Bsidxf)
            S.op("pool", lambda h, ia=ia: h.indirect_dma_start(
                out=Xs[:, :], out_offset=ind(ia), in_=h2tm, in_offset=None, bounds_check=bnd(h, NSLOT - 1), oob_is_err=False),
                [Bh2tm, Bia], [], dma=scsem)

    lastsc = S.last[("dma", id(scsem))]
    nxt_ix = block_idx(0)
    for b in range(NBK):
        bix = nxt_ix
        if b + 1 < NBK:
            nxt_ix = block_idx(b + 1)
        if b == 0:
            S.op("sp", lambda h, b=b: h.dma_start(out=Xtm3, in_=Xs[b * BLK:(b + 1) * BLK, :].rearrange("(j p) c -> p j c", p=128)),
                 [], [BXtm], dma=xtsem, extra=[lastsc])
        for j in range(4):
            for hf in range(2):
                pb, Bp = nextp()
                for k4 in range(4):
                    kc = hf * 4 + k4
                    S.op("pe", lambda h, o=pb[:, k4 * 128:(k4 + 1) * 128], i=Xtm3[:, j, kc * 128:(kc + 1) * 128]: h.transpose(o, i, ident),
                         [BXtm, Bcst], [Bp])
                for k4 in range(4):
                    kc = hf * 4 + k4
                    cp(XT3[:, kc, j * 128:(j + 1) * 128], pb[:, k4 * 128:(k4 + 1) * 128], [Bp], [BXT], eng="act")
        if b + 1 < NBK:
            S.op("sp", lambda h, b=b + 1: h.dma_start(out=Xtm3, in_=Xs[b * BLK:(b + 1) * BLK, :].rearrange("(j p) c -> p j c", p=128)),
                 [], [BXtm], dma=xtsem, extra=[lastsc])
        bi = b % 2
        ia, Bia = bix["bg"]
        S.op("pool", lambda h, o=bgt[bi][0], ia=ia: h.indirect_dma_start(
            out=o, out_offset=None, in_=bguT_d[:, :], in_offset=ind(ia), bounds_check=bnd(h, NE * 128 - 1), oob_is_err=False),
            [Bia], [bgt[bi][1]], dma=bgsem[bi])
        ia, Bia = bix["bd"]
        S.op("pool", lambda h, o=bdb[bi][0], ia=ia: h.indirect_dma_start(
            out=o, out_offset=None, in_=bd_d[:, :], in_offset=ind(ia), bounds_check=bnd(h, NE - 1), oob_is_err=False),
            [Bia], [bdb[bi][1]], dma=bdsem[bi])
        for u in range(8):
            i = uctr[0] % NU
            uctr[0] += 1
            wv, wb = wgt[i]
            w3 = wv.rearrange("p (k n) -> p k n", n=256)
            ia, Bia = bix["w%d" % u]
            S.op("pool", lambda h, o=wv, ia=ia: h.indirect_dma_start(
                out=o, out_offset=None, in_=wguh[:, :], in_offset=ind(ia), bounds_check=bnd(h, NE * 1024 - 1), oob_is_err=False),
                [Bia], [wb], dma=wgsem[i])
            pg, Bpg = nextp()
            for kc in range(8):
                mm(pg, w3[:, kc, 0:128], XT3[:, kc, :], kc == 0, kc == 7, [wb, BXT], [Bpg])
            plin, Bplin = nextp()
            for kc in range(8):
                mm(plin, w3[:, kc, 128:256], XT3[:, kc, :], kc == 0, kc == 7, [wb, BXT], [Bplin])
            si = sctr[0] % 2
            sctr[0] += 1
            gmv, Bgm = gm_[si]; sgv, Bsg = sg_[si]; lmv, Blm = lm_[si]
            bg = bgt[bi][0]
            ts(gmv, pg, bg[:, u * 2:u * 2 + 1], 7.0, ALU.add, ALU.min, [Bpg, bgt[bi][1]], [Bgm])
            act(sgv, gmv, AF.Sigmoid, [Bgm], [Bsg], scale=1.702)
            act(lmv, plin, AF.Identity, [Bplin, bgt[bi][1]], [Blm], bias=bg[:, u * 2 + 1:u * 2 + 2])
            ts(lmv, lmv, 7.0, -7.0, ALU.min, ALU.max, [Blm], [Blm])
            tt(gmv, gmv, sgv, ALU.mult, [Bgm, Bsg], [Bgm])
            stt(actT3[:, u, :], lmv, 1.0, gmv, ALU.add, ALU.mult, [Blm, Bgm], [BactT])
        for q in range(4):
            ia, Bia = bix["d%d" % q]
            S.op("pool", lambda h, o=wq[q][0], ia=ia: h.indirect_dma_start(
                out=o, out_offset=None, in_=wdh[:, :], in_offset=ind(ia), bounds_check=bnd(h, NE * 512 - 1), oob_is_err=False),
                [Bia], [wq[q][1]], dma=wqsem[q])
        for j in range(4):
            dv, Bd = dsb[j % 2]
            for nt in range(2):
                pd_, Bpd = nextp()
                for ft in range(8):
                    mm(pd_, actT3[:, ft, j * 128:(j + 1) * 128], wq3[ft // 2][:, ft % 2, nt * 512:(nt + 1) * 512], ft == 0, ft == 7,
                       [BactT, wq[ft // 2][1]], [Bpd])
                tt(dv[:, nt * 512:(nt + 1) * 512], pd_, bdb[bi][0][:, nt * 512:(nt + 1) * 512], ALU.add, [Bpd, bdb[bi][1]], [Bd])
            dma("sp", Ys[b * BLK + j * 128:b * BLK + (j + 1) * 128, :], dv, [Bd], [], yss[j % 2])

    lastys = [S.last[("dma", id(y_))] for y_ in yss]
    xtm_prev = [BXtm.w] + list(BXtm.r.values())
    BY = [Buf("Yk%d" % k) for k in range(4)]
    ygs = [S.dmasem("ygat%d" % k) for k in range(4)]
    for c in range(16):
        dma("sp", xc, x2s[c * 128:(c + 1) * 128, :], [], [Bxc], xcsem)
        for k in range(4):
            ia, Bia = idx_tile(sidxf[:, c * 4 + k:c * 4 + k + 1], Bsidxf)
            S.op("pool", lambda h, o=Xtm3[:, k, :], ia=ia: h.indirect_dma_start(
                out=o, out_offset=None, in_=Ys[:, :], in_offset=ind(ia), bounds_check=bnd(h, NSLOT - 1), oob_is_err=False),
                [Bia], [BY[k]], dma=ygs[k], extra=lastys + [d_ for d_ in xtm_prev if d_ is not None])
        ts(acc, Xtm3[:, 0, :], w4[:, c * 4:c * 4 + 1], None, ALU.mult, None, [BY[0], Bw4], [Bacc])
        for k in range(1, 4):
            stt(acc, Xtm3[:, k, :], w4[:, c * 4 + k:c * 4 + k + 1], acc, ALU.mult, ALU.add, [BY[k], Bw4, Bacc], [Bacc])
        tt(acc, acc, gfb, ALU.mult, [Bacc, Bgfb], [Bacc])
        tt(xc, xc, acc, ALU.add, [Bxc, Bacc], [Bxc])
        act(xs2, xc, AF.Square, [Bxc], [Bxs2, Bst2], accum=st2[:, 0:1])
        act(st2[:, 1:2], st2[:, 0:1], AF.Sqrt, [Bst2, Bsm], [Bst2], bias=epsc, scale=1.0 / D)
        S.op("dve", lambda h: h.reciprocal(st2[:, 2:3], st2[:, 1:2]), [Bst2], [Bst2])
        stt(xs2, xc, st2[:, 2:3], gfin, ALU.mult, ALU.mult, [Bxc, Bst2, Bgfin], [Bxs2])
        dma("sp", out[c * 128:(c + 1) * 128, :], xs2, [Bxs2], [], osem)

    print("arena cols: pers", PERS_END, "mix", MIX_END, "moe", top[0], "ops", len(S.ops))
    S.emit(nc, es, final_waits=[osem, h2sem, scsem] + x2sems + yss)
    es.close()
    return nc


def _consts():
    c = np.zeros((128, NC), np.float64)
    idx = np.arange(128)
    c[:, C_ID:C_ID + 128] = np.eye(128)
    s = idx[:, None]; j = idx[None, :]
    c[:, C_TRI:C_TRI + 128] = (s <= j)
    c[:, C_MNEG:C_MNEG + 128] = np.where(s <= j, 0.0, -30000.0)
    for h in range(4):
        c[h, C_SEL + h * 128:C_SEL + (h + 1) * 128] = 1.0
        lg = math.log(1.0 - 2.0 ** (-5.0 - h))
        c[:, C_DECT + h * 128:C_DECT + (h + 1) * 128] = np.where(j >= s, np.exp(lg * np.maximum(j - s, 0)), 0.0) * 128.0 ** -0.5
        c[:, C_QD + h * 128:C_QD + (h + 1) * 128] = np.exp(lg * (j + 1.0)) * np.ones((128, 1))
        c[:, C_KDEC + h] = np.exp(lg * (127.0 - idx)) * 128.0 ** -0.5
    c[:, C_ONES:C_ONES + 512] = 1.0
    inv = (10000.0 ** (-np.arange(64, dtype=np.float32) / np.float32(64))).astype(np.float32)
    c[:, C_INVF:C_INVF + 256] = np.tile(inv, 4)[None, :]
    c[:, C_IOB:C_IOB + 64] = np.arange(64)[None, :]
    c[:, C_CU:C_CU + 8] = np.arange(8)[None, :] * 128 + idx[:, None]
    return c.astype(np.float32)


_NC_CACHE = {}


def _colT(v, n):
    return np.ascontiguousarray(np.asarray(v, np.float32).reshape(n, 128).T)


def kernel(x, c, positions, w_ada, b_ada, norm_mix_g, w_in, conv_w, conv_b, b_if, ml_norm_g, ret_norm_g,
           w_branch_ml, w_branch_ret, w_out, norm_ffn_g, w_router, b_router, w_gate_up, b_gate_up, w_down, b_down,
           norm_final_g, _debug=False):
    f = lambda a: np.ascontiguousarray(np.asarray(a, np.float32))
    x = f(x); c = f(c)
    positions = np.asarray(positions).astype(np.int32)
    key = bool(_debug)
    if key not in _NC_CACHE:
        _NC_CACHE[key] = build_nc(debug=key)
    nc = _NC_CACHE[key]
    wgu_p = f(w_gate_up)[0].reshape(NE, 8, 128, 8, 128, 2)
    wgu_p = np.ascontiguousarray(wgu_p.transpose(0, 3, 2, 1, 5, 4)).reshape(NE, D, 2 * D)
    wd_p = f(w_down)[0].reshape(NE, 4, 2, 128, D)
    wd_p = np.ascontiguousarray(wd_p.transpose(0, 1, 3, 2, 4)).reshape(NE, D // 2, 2 * D)
    bgu = f(b_gate_up)[0].reshape(NE, D, 2)
    bguT = np.stack([bgu[..., 0].reshape(NE, 8, 128), bgu[..., 1].reshape(NE, 8, 128)], axis=2)
    bguT = np.ascontiguousarray(bguT.transpose(0, 3, 1, 2).reshape(NE * 128, 16))
    shared = {
        "cst": _consts(),
        "w_ada": f(w_ada)[0], "b_adaT": _colT(f(b_ada)[0], 48),
        "gmixT": _colT(f(norm_mix_g)[0], 8), "gffnT": _colT(f(norm_ffn_g)[0], 8), "gfin": f(norm_final_g).reshape(1, D),
        "w_in": f(w_in)[0],
        "convwT": np.ascontiguousarray(f(conv_w)[0].T.reshape(8, 128, 4).transpose(1, 0, 2).reshape(128, 32)),
        "convbT": _colT(f(conv_b)[0], 8), "bif": f(b_if)[0].reshape(1, 8),
        "mlgT": _colT(f(ml_norm_g)[0], 8), "retgT": _colT(f(ret_norm_g)[0], 8),
        "wbml": f(w_branch_ml)[0], "wbret": f(w_branch_ret)[0], "wout": f(w_out)[0],
        "wr": f(w_router)[0], "br": f(b_router)[0].reshape(1, NE),
        "wgu": wgu_p, "bguT": bguT, "wd": wd_p, "bd": f(b_down)[0],
    }
    in_maps = []
    for i in range(8):
        b, half = i // 2, i % 2
        m = dict(shared)
        m["xo"] = np.ascontiguousarray(x[b, half * TOK:(half + 1) * TOK])
        m["xp"] = np.ascontiguousarray(x[b, 0:TOK])
        m["poso"] = np.ascontiguousarray(positions[b, half * TOK:(half + 1) * TOK].reshape(16, 128).T)
        m["posp"] = np.ascontiguousarray(positions[b, 0:TOK].reshape(16, 128).T)
        m["flag"] = np.full((128, 1), float(half), np.float32)
        m["cT"] = _colT(c[b], 8)
        in_maps.append(m)
    res = run_bass_kernel_spmd(nc, in_maps, core_ids=list(range(8)))
    outp = np.zeros((4, SEQ, D), np.float32)
    for i in range(8):
        b, half = i // 2, i % 2
        outp[b, half * TOK:(half + 1) * TOK] = res.results[i]["out"]
    if _debug:
        dbg = np.zeros((4, SEQ, D), np.float32)
        for i in range(8):
            b, half = i // 2, i % 2
            dbg[b, half * TOK:(half + 1) * TOK] = res.results[i]["x2s"]
        return outp, dbg
    return outp
```

```python
import contextlib
import math
import numpy as np
import concourse.bass as bass
import concourse.mybir as mybir
from concourse.bass_utils import run_bass_kernel_spmd

F32 = mybir.dt.float32
F32R = mybir.dt.float32r
I32 = mybir.dt.int32
ALU = mybir.AluOpType
AF = mybir.ActivationFunctionType

D = 1024
SEQ = 4096
TOK = 2048
G = 256
NE = 32
EPS = 1e-5
LNSCALE = math.log(128.0 ** -0.5)
TWO_PI = 6.283185307179586

C_ID, C_TRI, C_MNEG, C_SEL, C_ONES, C_DECT, C_QD, C_KDEC, C_INVF = 0, 128, 256, 384, 896, 1408, 1920, 2432, 2436
C_IOB, C_CU = 2692, 2756
NC = 2764


class Buf:
    __slots__ = ("name", "w", "r")

    def __init__(self, name=""):
        self.name = name
        self.w = None
        self.r = {}


class DmaSem:
    __slots__ = ("sem", "value", "name")

    def __init__(self, name):
        self.name = name
        self.sem = None
        self.value = 0


class Op:
    __slots__ = ("eng", "fn", "deps", "needed", "tok", "dma")


class Sched:
    ENGS = ("pe", "dve", "act", "pool", "sp")

    def __init__(self, sync_same=True):
        self.ops = []
        self.sync_same = sync_same
        self.dmasems = []
        self.last = {}

    def dmasem(self, name):
        d = DmaSem(name)
        self.dmasems.append(d)
        return d

    def op(self, eng, fn, reads=(), writes=(), dma=None, extra=()):
        o = Op()
        o.eng, o.fn, o.dma = eng, fn, dma
        o.needed = dma is not None
        o.tok = None
        deps = {}
        for b in reads:
            if b.w is not None:
                deps[id(b.w)] = b.w
        for b in writes:
            if b.w is not None:
                deps[id(b.w)] = b.w
            for d in b.r.values():
                deps[id(d)] = d
        for d in extra:
            deps[id(d)] = d
        o.deps = []
        for d in deps.values():
            if d is o:
                continue
            if d.dma is None and d.eng == eng and (eng == "pe" or not self.sync_same):
                continue
            d.needed = True
            o.deps.append(d)
        key = ("dma", id(dma)) if dma is not None else eng
        for b in reads:
            b.r[key] = o
        for b in writes:
            b.w = o
            b.r = {}
        self.ops.append(o)
        self.last[key] = o
        return o

    def barrier(self):
        lasts = list(self.last.values())
        for e in self.ENGS:
            self.op(e, None, extra=[d for d in lasts if d.fn is not None])

    def emit(self, nc, es, final_waits=()):
        esem = {e: es.enter_context(nc.semaphore("s_" + e)) for e in self.ENGS}
        for d in self.dmasems:
            d.sem = es.enter_context(nc.semaphore("d_" + d.name))
            d.value = 0
        cnt = {e: 0 for e in self.ENGS}
        for o in self.ops:
            if o.fn is None:
                continue
            if o.dma is not None:
                o.dma.value += 16
                o.tok = (o.dma.sem, o.dma.value)
            elif o.needed:
                cnt[o.eng] += 1
                o.tok = (esem[o.eng], cnt[o.eng])
        block = es.enter_context(nc.Block())
        per = {e: [o for o in self.ops if o.eng == e] for e in self.ENGS}

        def run(e, h):
            waited = {}
            for o in per[e]:
                need = {}
                for d in o.deps:
                    s, v = d.tok
                    k = id(s)
                    if waited.get(k, 0) >= v:
                        continue
                    if k not in need or need[k][1] < v:
                        need[k] = (s, v)
                for k, (s, v) in need.items():
                    h.wait_ge(s, v)
                    waited[k] = v
                if o.fn is None:
                    continue
                ins = o.fn(h)
                if o.tok is not None:
                    ins.then_inc(o.tok[0], 16 if o.dma is not None else 1)
            if e == "sp":
                for d in final_waits:
                    h.wait_ge(d.sem, d.value)

        @block.tensor
        def _(h):
            run("pe", h)

        @block.vector
        def _(h):
            run("dve", h)

        @block.scalar
        def _(h):
            run("act", h)

        @block.gpsimd
        def _(h):
            run("pool", h)

        @block.sync
        def _(h):
            run("sp", h)


def build_nc(debug=False):
    nc = bass.Bass("TRN2", target_bir_lowering=False)

    def din(name, shape, dt=F32):
        return nc.dram_tensor(name, list(shape), dt, kind="ExternalInput").ap()

    xo = din("xo", [TOK, D]); xp = din("xp", [TOK, D])
    poso = din("poso", [128, 16], I32); posp = din("posp", [128, 16], I32)
    flag_d = din("flag", [128, 1]); cT_d = din("cT", [128, 8]); cst_d = din("cst", [128, NC])
    w_ada = din("w_ada", [D, 6 * D]); b_adaT = din("b_adaT", [128, 48])
    gmixT_d = din("gmixT", [128, 8]); gffnT_d = din("gffnT", [128, 8]); gfin_d = din("gfin", [1, D])
    w_in = din("w_in", [D, 8200]); convwT_d = din("convwT", [128, 32]); convbT_d = din("convbT", [128, 8])
    bif_d = din("bif", [1, 8]); mlgT_d = din("mlgT", [128, 8]); retgT_d = din("retgT", [128, 8])
    wbml = din("wbml", [D, D]); wbret = din("wbret", [D, D]); wout = din("wout", [D, D])
    wr_d = din("wr", [D, NE]); br_d = din("br", [1, NE])
    wgu = din("wgu", [NE, D, 2 * D]); bguT_d = din("bguT", [NE * 128, 16])
    wd = din("wd", [NE, D // 2, 2 * D]); bd_d = din("bd", [NE, D])
    out = nc.dram_tensor("out", [TOK, D], F32, kind="ExternalOutput").ap()
    x2s = nc.dram_tensor("x2s", [TOK, D], F32, kind="ExternalOutput" if debug else "Internal").ap()

    S = Sched()
    es = contextlib.ExitStack()
    NCOL = 53180
    pbanks = [es.enter_context(nc.psum_tensor("pb%d" % i, [128, 512], F32)) for i in range(8)]
    PB = [Buf("pb%d" % i) for i in range(8)]
    pctr = [0]

    def nextp():
        i = pctr[0] % 8
        pctr[0] += 1
        return pbanks[i][:, :], PB[i]

    top = [0]
    tcount = [0]

    def al(n):
        n = (n + 7) // 8 * 8
        a = top[0]
        top[0] += n
        assert top[0] <= NCOL, top[0]
        return a

    def T(n, dt=F32, name=""):
        a = al(n)
        tcount[0] += 1
        t = nc.alloc_sbuf_tensor_at("t%d_%s" % (tcount[0], name), [128, n], dt, offset=16640 + 4 * a)
        return t[:, :], Buf(name)

    def mm(o, lhsT, rhs, st, sp, rd, wr):
        S.op("pe", lambda h: h.matmul(o, lhsT=lhsT, rhs=rhs, start=st, stop=sp), rd, wr)

    def act(o, i, func, rd, wr, bias=None, scale=None, accum=None):
        kw = {}
        if bias is not None:
            kw["bias"] = bias
        if scale is not None:
            kw["scale"] = scale
        if accum is not None:
            kw["accum_out"] = accum
        S.op("act", lambda h: h.activation(o, i, func, **kw), rd, wr)

    def ts(o, i, s1, s2, op0, op1, rd, wr, eng="dve"):
        if op1 is None:
            S.op(eng, lambda h: h.tensor_scalar(o, i, s1, None, op0), rd, wr)
        else:
            S.op(eng, lambda h: h.tensor_scalar(o, i, s1, s2, op0, op1), rd, wr)

    def tt(o, a, b, op, rd, wr, eng="dve"):
        S.op(eng, lambda h: h.tensor_tensor(o, a, b, op), rd, wr)

    def stt(o, a, s, b, op0, op1, rd, wr):
        S.op("dve", lambda h: h.scalar_tensor_tensor(o, a, s, b, op0, op1), rd, wr)

    def cp(o, i, rd, wr, eng="dve"):
        if eng == "act":
            S.op(eng, lambda h: h.copy(o, i), rd, wr)
        else:
            S.op(eng, lambda h: h.tensor_copy(o, i), rd, wr)

    def dma(q, o, i, rd, wr, sem):
        S.op(q, lambda h: h.dma_start(out=o, in_=i), rd, wr, dma=sem)

    cst, Bcst = T(NC, name="cst")
    ident = cst[:, C_ID:C_ID + 128]
    tri = cst[:, C_TRI:C_TRI + 128]
    mneg = cst[:, C_MNEG:C_MNEG + 128]
    ones = cst[:, C_ONES:C_ONES + 512]
    onesR, BonesR = T(512, F32R, "onesR")
    modT, BmodT = T(48, name="modT")
    AB, BAB = T(32, name="AB")
    sm, Bsm = T(160, name="small")
    gmixT = sm[:, 0:8]; gffnT = sm[:, 8:16]; badaT = sm[:, 16:64]; convw = sm[:, 64:96]; convb = sm[:, 96:104]
    bifb = sm[:, 104:112]; flag = sm[:, 112:113]; epsc = sm[:, 113:114]; cTt = sm[:, 114:122]
    mlgT = sm[:, 122:130]; retgT = sm[:, 130:138]
    cact2, Bcact = T(16, F32R, "cact2")
    gfb, Bgfb = T(D, name="gate_f_bc")
    gfin, Bgfin = T(D, name="gfin_bc")
    wrt, Bwrt = T(8 * NE, F32R, "wr")
    brb, Bbrb = T(NE, name="br")
    posf, Bposf = T(32, name="posf")
    posi, Bposi = T(32, I32, "posi")
    PERS_END = top[0]

    dl = {n: S.dmasem(n) for n in ["c0", "c1", "c2", "c3", "c4", "c5", "c6", "c7", "c8", "c9", "c10", "c11", "c12", "c13", "c14", "c15", "c16"]}
    dma("sp", cst, cst_d[:, :], [], [Bcst], dl["c0"])
    dma("sp", gmixT, gmixT_d[:, :], [], [Bsm], dl["c1"])
    dma("sp", gffnT, gffnT_d[:, :], [], [Bsm], dl["c1"])
    dma("sp", badaT, b_adaT[:, :], [], [Bsm], dl["c1"])
    dma("sp", convw, convwT_d[:, :], [], [Bsm], dl["c1"])
    dma("sp", convb, convbT_d[:, :], [], [Bsm], dl["c1"])
    dma("sp", bifb, bif_d[0:1, :].partition_broadcast(128), [], [Bsm], dl["c1"])
    dma("sp", flag, flag_d[:, :], [], [Bsm], dl["c1"])
    dma("sp", cTt, cT_d[:, :], [], [Bsm], dl["c1"])
    dma("sp", mlgT, mlgT_d[:, :], [], [Bsm], dl["c1"])
    dma("sp", retgT, retgT_d[:, :], [], [Bsm], dl["c1"])
    S.op("dve", lambda h: h.memset(epsc, EPS), [], [Bsm])
    cp(onesR, ones, [Bcst], [BonesR])
    dma("sp", gfin, gfin_d[0:1, :].partition_broadcast(128), [], [Bgfin], dl["c2"])
    dma("pool", wrt.rearrange("p (k n) -> p k n", n=NE), wr_d.rearrange("(k p) n -> p k n", p=128), [], [Bwrt], dl["c3"])
    dma("sp", brb, br_d[0:1, :].partition_broadcast(128), [], [Bbrb], dl["c4"])
    dma("sp", posi[:, 0:16], posp[:, :], [], [Bposi], dl["c6"])
    dma("sp", posi[:, 16:32], poso[:, :], [], [Bposi], dl["c6"])
    cp(posf, posi, [Bposi], [Bposf])

    gmb, Bgmb = T(D, name="gate_m_bc")
    Cml = [T(257, F32, "Cml%d" % h) for h in range(4)]
    CmlR = [T(258, F32R, "CmlR%d" % h) for h in range(4)]
    Cret = [T(256, F32, "Cret%d" % h) for h in range(4)]
    CretR = [T(256, F32R, "CretR%d" % h) for h in range(4)]
    rowA, BrowA = T(600, name="rows")
    bTr = rowA[0:4, 0:256]; Mr = rowA[0:4, 256:512]; mpr = rowA[0:4, 512:530]; Ar = rowA[0:4, 530:532]
    ext = rowA[0:4, 532:540]
    xg = [T(D, name="xg%d" % c) for c in range(2)]
    xs, Bxs = T(D, name="xs")
    h1T, Bh1T = T(8 * G, F32R, "h1T")
    h1T3 = h1T.rearrange("p (k n) -> p k n", n=G)
    NW = 3
    wst = [T(8 * 256, F32R, "wst%d" % i) for i in range(NW)]
    wsem = [S.dmasem("wst%d" % i) for i in range(NW)]
    wctr = [0]
    qkpre, Bqkpre = T(8 * 259, name="qkpre")
    qkpre3 = qkpre.rearrange("p (f n) -> p f n", n=259)
    cacc, Bcacc = T(G, name="cacc")
    qkT, BqkT = T(8 * G, F32R, "qkT")
    qkT3 = qkT.rearrange("p (f n) -> p f n", n=G)
    vml = [T(4 * 258, F32R, "vml%d" % c) for c in range(2)]
    oml = [T(D, name="oml%d" % c) for c in range(2)]
    ifb = [T(8, name="if%d" % c) for c in range(2)]
    rq = [T(512, name="rq%d" % c) for c in range(2)]
    rk = [T(512, name="rk%d" % c) for c in range(2)]
    rv = [T(D, F32R, "rv%d" % c) for c in range(2)]
    rg = [T(D, name="rg%d" % c) for c in range(2)]
    gsm, Bgsm = T(104, name="gsm")
    NMh, BNM = T(4 * G, name="NM")
    hmT, BhmT = T(8 * G, F32R, "hmT")
    hrT, BhrT = T(8 * G, F32R, "hrT")
    yT, ByT = T(8 * G, F32R, "yT")
    hmT3 = hmT.rearrange("p (k n) -> p k n", n=G); hrT3 = hrT.rearrange("p (k n) -> p k n", n=G)
    yT3 = yT.rearrange("p (k n) -> p k n", n=G)
    DTs = [T(128, name="DT%d" % h) for h in range(4)]
    PTs = [T(128, F32R, "PT%d" % h) for h in range(4)]
    kws = [T(128, F32R, "kw%d" % h) for h in range(4)]
    inss = [T(258, name="inter_s%d" % h) for h in range(4)]
    tots = [T(258, name="tot%d" % h) for h in range(4)]
    hhs = [(inss[h][0][:, 0:256], inss[h][1]) for h in range(4)]
    sths = [T(16, name="sth%d" % h) for h in range(4)]
    qdTs = kws
    st6, Bst6 = T(16, name="stats")
    rot, Brot = T(768, name="rot")
    sg2, Bsg2 = rot[:, 0:256], Brot
    sct = [T(512, name="sincos%d" % c) for c in range(2)]
    kint, Bkint = T(256, I32, "kint")
    rtmp, Brtmp = xs, Bxs
    qrTs = [T(512, F32R, "qrT%d" % c) for c in range(2)]
    krTs = [T(512, F32R, "krT%d" % c) for c in range(2)]
    sg1, Bsg1 = cacc, Bcacc
    dgt, Bdgt = DTs[0]
    x2sems = [S.dmasem("x2st%d" % c) for c in range(2)]
    xsem = [S.dmasem("xld%d" % c) for c in range(2)]
    MIX_END = top[0]

    for h in range(4):
        S.op("dve", lambda hh_, a=Cml[h][0]: hh_.memset(a, 0.0), [], [Cml[h][1]])
        cp(CmlR[h][0][:, 0:257], Cml[h][0], [Cml[h][1]], [CmlR[h][1]])
        cp(CmlR[h][0][:, 257:258], Cml[h][0][:, 0:1], [Cml[h][1]], [CmlR[h][1]])
        S.op("dve", lambda hh_, a=Cret[h][0]: hh_.memset(a, 0.0), [], [Cret[h][1]])
        cp(CretR[h][0], Cret[h][0], [Cret[h][1]], [CretR[h][1]])
    S.op("dve", lambda h: h.memset(qkpre, 0.0), [], [Bqkpre])
    S.op("dve", lambda h: h.memset(rowA, 0.0), [], [BrowA])
    for c in range(2):
        for q_ in range(0, 1032, 512):
            n_ = min(512, 1032 - q_)
            cp(vml[c][0][:, q_:q_ + n_], ones[:, 0:n_], [Bcst], [vml[c][1]])

    def wload(src3, ncols):
        i = wctr[0] % NW
        wctr[0] += 1
        ap, b = wst[i]
        v3 = ap[:, 0:8 * ncols].rearrange("p (k n) -> p k n", n=ncols)
        dma("pool", v3, src3, [], [b], wsem[i])
        return v3, b

    def win_tile(c0, ncols=256):
        return wload(w_in.rearrange("(k p) c -> p k c", p=128)[:, :, c0:c0 + ncols], ncols)

    act(cact2.rearrange("p (k t) -> p k t", t=2)[:, :, 0], cTt, AF.Silu, [Bsm], [Bcact])
    act(cact2.rearrange("p (k t) -> p k t", t=2)[:, :, 1], cTt, AF.Silu, [Bsm], [Bcact])
    cact3 = cact2.rearrange("p (k t) -> p k t", t=2)
    pm, Bpm = nextp()
    wada3 = w_ada.rearrange("(k p) c -> p k c", p=128)
    for j in range(6):
        for sb_ in range(4):
            w3, wb = wload(wada3[:, :, j * D + sb_ * 256: j * D + (sb_ + 1) * 256], 256)
            for ft in range(2):
                col = (j * 8 + sb_ * 2 + ft) * 2
                for kc in range(8):
                    mm(pm[:, col:col + 2], w3[:, kc, ft * 128:(ft + 1) * 128], cact3[:, kc, :], kc == 0, kc == 7,
                       [wb, Bcact], [Bpm])
    tt(modT, pm[:, 0:96].rearrange("p (c t) -> p c t", t=2)[:, :, 0], badaT, ALU.add, [Bpm, Bsm], [BmodT])
    stt(AB[:, 0:8], modT[:, 8:16], 1.0, gmixT, ALU.add, ALU.mult, [BmodT, Bsm], [BAB])
    cp(AB[:, 8:16], modT[:, 0:8], [BmodT], [BAB])
    stt(AB[:, 16:24], modT[:, 32:40], 1.0, gffnT, ALU.add, ALU.mult, [BmodT, Bsm], [BAB])
    cp(AB[:, 24:32], modT[:, 24:32], [BmodT], [BAB])
    A1 = AB[:, 0:8]; B1 = AB[:, 8:16]; A2 = AB[:, 16:24]; B2 = AB[:, 24:32]

    def bcast_vec(dst, Bdst, col0):
        for hf in range(2):
            pb, Bp = nextp()
            for k4 in range(4):
                kc = hf * 4 + k4
                ts(dgt, ident, modT[:, col0 + kc:col0 + kc + 1], None, ALU.mult, None, [Bcst, BmodT], [Bdgt])
                mm(pb[:, k4 * 128:(k4 + 1) * 128], ones[:, 0:128], dgt, True, True, [Bcst, Bdgt], [Bp])
            cp(dst[:, hf * 512:(hf + 1) * 512], pb, [Bp], [Bdst])

    bcast_vec(gmb, Bgmb, 16)
    bcast_vec(gfb, Bgfb, 40)

    def norm_to_T(xsrc, Bx, dstT3, BdstT, c, Acol, Bcol):
        act(xs, xsrc, AF.Square, [Bx], [Bxs, Bst6], accum=st6[:, 0:1])
        act(st6[:, 1:2], st6[:, 0:1], AF.Sqrt, [Bst6, Bsm], [Bst6], bias=epsc, scale=1.0 / D)
        S.op("dve", lambda h: h.reciprocal(st6[:, 2:3], st6[:, 1:2]), [Bst6], [Bst6])
        ts(xs, xsrc, st6[:, 2:3], None, ALU.mult, None, [Bx, Bst6], [Bxs])
        for hf in range(2):
            pb, Bp = nextp()
            for k4 in range(4):
                kc = hf * 4 + k4
                S.op("pe", lambda h, o=pb[:, k4 * 128:(k4 + 1) * 128], i=xs[:, kc * 128:(kc + 1) * 128]: h.transpose(o, i, ident),
                     [Bxs, Bcst], [Bp])
            for k4 in range(4):
                kc = hf * 4 + k4
                act(dstT3[:, kc, c * 128:(c + 1) * 128], pb[:, k4 * 128:(k4 + 1) * 128], AF.Identity, [Bp, BAB], [BdstT],
                    bias=Bcol[:, kc:kc + 1], scale=Acol[:, kc:kc + 1])

    def proj_fm(c0, nft, evac):
        for t in range(nft // 2):
            w3, wb = win_tile(c0 + t * 256)
            for f in range(2):
                pb, Bp = nextp()
                for kc in range(8):
                    mm(pb[:, 0:G], w3[:, kc, f * 128:(f + 1) * 128], h1T3[:, kc, :], kc == 0, kc == 7, [wb, Bh1T], [Bp])
                evac(t * 2 + f, pb[:, 0:G], Bp)

    def proj_tm(c0, ntile, evac, ncols=256):
        for t in range(ntile):
            w3, wb = win_tile(c0 + t * ncols, ncols)
            for c in range(2):
                pb, Bp = nextp()
                for kc in range(8):
                    mm(pb[:, 0:ncols], h1T3[:, kc, c * 128:(c + 1) * 128], w3[:, kc, :], kc == 0, kc == 7, [Bh1T, wb], [Bp])
                evac(t, c, pb[:, 0:ncols], Bp)

    def ln_stages(items):
        for hv, Bh, st, Bst in items:
            S.op("dve", lambda h, st=st, hv=hv: h.bn_stats(st[:, 4:10], hv), [Bh], [Bst])
        yield
        for hv, Bh, st, Bst in items:
            S.op("dve", lambda h, st=st: h.bn_aggr(st[:, 10:12], st[:, 4:10]), [Bst], [Bst])
        yield
        for hv, Bh, st, Bst in items:
            act(st[:, 12:13], st[:, 11:12], AF.Sqrt, [Bst, Bsm], [Bst], bias=epsc, scale=1.0)
        yield
        for hv, Bh, st, Bst in items:
            S.op("dve", lambda h, st=st: h.reciprocal(st[:, 13:14], st[:, 12:13]), [Bst], [Bst])
        yield
        for hv, Bh, st, Bst in items:
            ts(hv, hv, st[:, 10:11], st[:, 13:14], ALU.subtract, ALU.mult, [Bh, Bst], [Bh])
        yield

    GAM = [1.0 - 2.0 ** (-5.0 - h) for h in range(4)]

    for g in range(16):
        own = g >= 8
        xsrc = xo if own else xp
        t0 = (g - 8) * G if own else g * G
        for c in range(2):
            dma("sp", xg[c][0], xsrc[t0 + c * 128:t0 + (c + 1) * 128, :], [], [xg[c][1]], xsem[c])
            norm_to_T(xg[c][0], xg[c][1], h1T3, Bh1T, c, A1, B1)
        ang = rot[:, 0:256]; red = rot[:, 256:512]; kf = rot[:, 512:768]
        for c in range(2):
            pcol = (16 if own else 0) + (g % 8) * 2 + c
            ts(ang, cst[:, C_INVF:C_INVF + 256], posf[:, pcol:pcol + 1], None, ALU.mult, None, [Bcst, Bposf], [Brot])
            for dst_, off in ((sct[c][0][:, 0:256], 0.0), (sct[c][0][:, 256:512], math.pi / 2)):
                ts(red, ang, off, None, ALU.add, None, [Brot], [Brot])
                ts(kint, red, 1.0 / TWO_PI, None, ALU.mult, None, [Brot], [Bkint])
                cp(kf, kint, [Bkint], [Brot])
                stt(red, kf, -TWO_PI, red, ALU.mult, ALU.add, [Brot], [Brot])
                ts(red, red, 3.14159, -3.14159, ALU.min, ALU.max, [Brot], [Brot])
                act(dst_, red, AF.Sin, [Brot], [sct[c][1]])
        if g == 8:
            ts(qkpre, qkpre, flag, None, ALU.mult, None, [Bqkpre, Bsm], [Bqkpre])
            ts(mpr[:, 0:1], mpr[:, 0:1], flag[0:4, :], None, ALU.mult, None, [BrowA, Bsm], [BrowA])
            for h in range(4):
                ts(Cml[h][0], Cml[h][0], flag, None, ALU.mult, None, [Cml[h][1], Bsm], [Cml[h][1]])
                cp(CmlR[h][0][:, 0:257], Cml[h][0], [Cml[h][1]], [CmlR[h][1]])
                ts(Cret[h][0], Cret[h][0], flag, None, ALU.mult, None, [Cret[h][1], Bsm], [Cret[h][1]])
                cp(CretR[h][0], Cret[h][0], [Cret[h][1]], [CretR[h][1]])

        need_q = own or g == 7

        def ev_qk(base):
            def f(ft, ps_, Bp):
                cp(qkpre3[:, base + ft, 3:259], ps_, [Bp], [Bqkpre], eng="act")
            return f
        proj_fm(512, 4, ev_qk(4))
        if need_q:
            proj_fm(0, 4, ev_qk(0))
        for ft in (range(8) if need_q else range(4, 8)):
            ts(cacc, qkpre3[:, ft, 0:G], convw[:, ft * 4:ft * 4 + 1], None, ALU.mult, None, [Bqkpre, Bsm], [Bcacc])
            for i in range(1, 4):
                stt(cacc, qkpre3[:, ft, i:i + G], convw[:, ft * 4 + i:ft * 4 + i + 1], cacc, ALU.mult, ALU.add,
                    [Bqkpre, Bsm, Bcacc], [Bcacc])
            act(qkT3[:, ft, :], cacc, AF.Silu, [Bcacc, Bsm], [BqkT], bias=convb[:, ft:ft + 1])
            cp(qkpre3[:, ft, 0:3], qkpre3[:, ft, G:G + 3], [Bqkpre], [Bqkpre])

        def ev_v(t, c, ps_, Bp):
            cp(vml[c][0][:, t * 258:t * 258 + 256], ps_, [Bp], [vml[c][1]], eng="act")
        proj_tm(1024, 4, ev_v)

        def ev_if(t, c, ps_, Bp):
            tt(ifb[c][0], ps_, bifb, ALU.add, [Bp, Bsm], [ifb[c][1]])
        proj_tm(3072, 1, ev_if, ncols=8)
        if own:
            def ev_o(t, c, ps_, Bp):
                act(oml[c][0][:, t * 256:(t + 1) * 256], ps_, AF.Sigmoid, [Bp], [oml[c][1]])
            proj_tm(2048, 4, ev_o)

        lf = gsm[:, 0:8]; a_ = gsm[:, 8:16]; b_ = gsm[:, 16:24]; tmp8 = gsm[:, 24:32]; bb = gsm[:, 32:40]
        u_ = gsm[:, 40:48]; isc = gsm[:, 48:56]; emt = gsm[:, 56:64]; spv = gsm[:, 64:72]; MT = gsm[:, 72:80]
        MLb = gsm[:, 80:88]; MPb = gsm[:, 88:96]; tmp8b = gsm[:, 96:104]
        for c in range(2):
            fp = ifb[c][0][:, 4:8]
            act(tmp8[:, c * 4:c * 4 + 4], fp, AF.Abs, [ifb[c][1]], [Bgsm])
            act(tmp8[:, c * 4:c * 4 + 4], tmp8[:, c * 4:c * 4 + 4], AF.Exp, [Bgsm], [Bgsm], scale=-1.0)
            act(tmp8[:, c * 4:c * 4 + 4], tmp8[:, c * 4:c * 4 + 4], AF.Ln, [Bgsm], [Bgsm], bias=1.0)
            ts(lf[:, c * 4:c * 4 + 4], fp, 0.0, None, ALU.min, None, [ifb[c][1]], [Bgsm])
            tt(lf[:, c * 4:c * 4 + 4], lf[:, c * 4:c * 4 + 4], tmp8[:, c * 4:c * 4 + 4], ALU.subtract, [Bgsm], [Bgsm])
        pa, Bpa = nextp()
        mm(pa[:, 0:8], tri, lf, True, True, [Bcst, Bgsm], [Bpa])
        for c in range(2):
            mm(pa[0:4, 16 + c:17 + c], lf[:, c * 4:c * 4 + 4], ones[:, 0:1], True, True, [Bgsm, Bcst], [Bpa])
        cp(a_, pa[:, 0:8], [Bpa], [Bgsm])
        cp(Ar, pa[0:4, 16:18], [Bpa], [BrowA])
        for c in range(2):
            tt(b_[:, c * 4:c * 4 + 4], ifb[c][0][:, 0:4], a_[:, c * 4:c * 4 + 4], ALU.subtract, [ifb[c][1], Bgsm], [Bgsm])
        pt_, Bpt = nextp()
        for c in range(2):
            S.op("pe", lambda h, o=pt_[0:4, c * 128:(c + 1) * 128], i=b_[:, c * 4:c * 4 + 4]: h.transpose(o, i, ident),
                 [Bgsm, Bcst], [Bpt])
        cp(bTr, pt_[0:4, 0:256], [Bpt], [BrowA])
        for c in range(2):
            ci = (g % 8) * 2 + c if False else c
            S.op("dve", lambda h, o=Mr[:, c * 128:(c + 1) * 128], d=bTr[:, c * 128:(c + 1) * 128], ini=mpr[:, c:c + 1]:
                 h.tensor_tensor_scan(o, d, d, ini, ALU.max, ALU.max), [BrowA], [BrowA])
            tt(mpr[:, c + 1:c + 2], Ar[:, c:c + 1], Mr[:, c * 128 + 127:c * 128 + 128], ALU.add, [BrowA], [BrowA])
            cp(ext[:, c:c + 1], Mr[:, c * 128 + 127:c * 128 + 128], [BrowA], [BrowA])
            cp(ext[:, 2 + c:3 + c], mpr[:, c:c + 1], [BrowA], [BrowA])
        cp(mpr[:, 0:1], mpr[:, 2:3], [BrowA], [BrowA])
        pe_, Bpe = nextp()
        for h in range(4):
            pb, Bp = nextp()
            mm(pb[:, 0:G], cst[0:4, C_SEL + h * 128:C_SEL + (h + 1) * 128], Mr, True, True, [Bcst, BrowA], [Bp])
            for c in range(2):
                tt(NMh[:, h * G + c * 128:h * G + (c + 1) * 128], mneg, pb[:, c * 128:(c + 1) * 128], ALU.subtract,
                   [Bcst, Bp], [BNM])
            mm(pe_[:, h * 4:h * 4 + 4], cst[0:4, C_SEL + h * 128:C_SEL + (h + 1) * 128], ext[:, 0:4], True, True,
               [Bcst, BrowA], [Bpe])
        for c in range(2):
            S.op("pe", lambda h, o=pe_[:, 32 + c * 4:36 + c * 4], i=Mr[:, c * 128:(c + 1) * 128]: h.transpose(o, i, ident[0:4, 0:4]),
                 [BrowA, Bcst], [Bpe])
        cp(MT, pe_[:, 32:40], [Bpe], [Bgsm])
        pe3 = pe_[:, 0:16].rearrange("p (h f) -> p h f", f=4)
        for c in range(2):
            cp(MLb[:, c * 4:c * 4 + 4], pe3[:, :, c], [Bpe], [Bgsm])
            cp(MPb[:, c * 4:c * 4 + 4], pe3[:, :, 2 + c], [Bpe], [Bgsm])
        ts(bb, b_, LNSCALE, None, ALU.add, None, [Bgsm], [Bgsm])
        tt(tmp8, b_, MLb, ALU.subtract, [Bgsm], [Bgsm])
        act(u_, tmp8, AF.Exp, [Bgsm], [Bgsm])
        tt(tmp8, MPb, MLb, ALU.subtract, [Bgsm], [Bgsm])
        act(spv, tmp8, AF.Exp, [Bgsm], [Bgsm])
        if own:
            tt(tmp8, MPb, MT, ALU.subtract, [Bgsm], [Bgsm])
            act(isc, tmp8, AF.Exp, [Bgsm], [Bgsm], bias=LNSCALE)
            tt(tmp8b, a_, MT, ALU.add, [Bgsm], [Bgsm])
            act(emt, tmp8b, AF.Exp, [Bgsm], [Bgsm], scale=-1.0)

        def ev_rk(t, c, ps_, Bp):
            cp(rk[c][0][:, t * 256:(t + 1) * 256], ps_, [Bp], [rk[c][1]], eng="act")
        proj_tm(3592, 2, ev_rk)

        def ev_rv(t, c, ps_, Bp):
            cp(rv[c][0][:, t * 256:(t + 1) * 256], ps_, [Bp], [rv[c][1]], eng="act")
        proj_tm(4104, 4, ev_rv)
        if own:
            def ev_rq(t, c, ps_, Bp):
                cp(rq[c][0][:, t * 256:(t + 1) * 256], ps_, [Bp], [rq[c][1]], eng="act")
            proj_tm(3080, 2, ev_rq)

            def ev_rg(t, c, ps_, Bp):
                act(rg[c][0][:, t * 256:(t + 1) * 256], ps_, AF.Silu, [Bp], [rg[c][1]])
            proj_tm(5128, 4, ev_rg)

        H4 = range(4)

        def rotary(src, Bsrc, c):
            s3 = sct[c][0][:, 0:256].rearrange("p (h d) -> p h d", d=64)
            c3 = sct[c][0][:, 256:512].rearrange("p (h d) -> p h d", d=64)
            Bsc = sct[c][1]
            x4 = src.rearrange("p (h t d) -> p h t d", t=2, d=64)
            r4 = rtmp[:, 0:512].rearrange("p (h t d) -> p h t d", t=2, d=64)
            q4 = rtmp[:, 512:1024].rearrange("p (h t d) -> p h t d", t=2, d=64)
            tt(r4[:, :, 0, :], x4[:, :, 0, :], c3, ALU.mult, [Bsrc, Bsc], [Brtmp])
            tt(r4[:, :, 1, :], x4[:, :, 1, :], c3, ALU.mult, [Bsrc, Bsc], [Brtmp])
            tt(q4[:, :, 0, :], x4[:, :, 1, :], s3, ALU.mult, [Bsrc, Bsc], [Brtmp])
            tt(q4[:, :, 1, :], x4[:, :, 0, :], s3, ALU.mult, [Bsrc, Bsc], [Brtmp])
            tt(x4[:, :, 0, :], r4[:, :, 0, :], q4[:, :, 0, :], ALU.subtract, [Brtmp], [Bsrc])
            tt(x4[:, :, 1, :], r4[:, :, 1, :], q4[:, :, 1, :], ALU.add, [Brtmp], [Bsrc])
        for c in range(2):
            rotary(rk[c][0], rk[c][1], c)
            if own:
                rotary(rq[c][0], rq[c][1], c)
        def ml_gen(c):
            kTs = [qkT3[:, 4 + h, c * 128:(c + 1) * 128] for h in H4]
            qTs = [qkT3[:, h, c * 128:(c + 1) * 128] for h in H4]
            vxs = [vml[c][0][:, h * 258:h * 258 + 258] for h in H4]
            cols = [c * 4 + h for h in H4]
            if own:
                pS = [nextp() for h in H4]
                for h in H4:
                    mm(pS[h][0][:, 0:128], kTs[h], qTs[h], True, True, [BqkT], [pS[h][1]])
                yield
                for h in H4:
                    act(DTs[h][0], NMh[:, h * G + c * 128:h * G + (c + 1) * 128], AF.Exp, [BNM, Bgsm], [DTs[h][1]],
                        bias=bb[:, cols[h]:cols[h] + 1])
                yield
                for h in H4:
                    tt(PTs[h][0], pS[h][0][:, 0:128], DTs[h][0], ALU.mult, [pS[h][1], DTs[h][1]], [PTs[h][1]])
                yield
                pI = [nextp() for h in H4]
                for h in H4:
                    mm(pI[h][0][:, 0:258], PTs[h][0], vxs[h], True, True, [PTs[h][1], vml[c][1]], [pI[h][1]])
                yield
                pJ = [nextp() for h in H4]
                for h in H4:
                    mm(pJ[h][0][:, 0:258], qTs[h], CmlR[h][0], True, True, [BqkT, CmlR[h][1]], [pJ[h][1]])
                yield
                for h in H4:
                    act(inss[h][0], pJ[h][0][:, 0:258], AF.Copy, [pJ[h][1], Bgsm], [inss[h][1]], scale=isc[:, cols[h]:cols[h] + 1])
                yield
                for h in H4:
                    tt(tots[h][0], pI[h][0][:, 0:258], inss[h][0], ALU.add, [pI[h][1], inss[h][1]], [tots[h][1]])
                yield
                for h in H4:
                    act(sths[h][0][:, 14:15], tots[h][0][:, 256:257], AF.Abs, [tots[h][1]], [sths[h][1]])
                yield
                for h in H4:
                    ts(sths[h][0][:, 14:15], sths[h][0][:, 14:15], emt[:, cols[h]:cols[h] + 1], None, ALU.max, None,
                       [sths[h][1], Bgsm], [sths[h][1]])
                yield
                for h in H4:
                    S.op("dve", lambda hd, st=sths[h][0]: hd.reciprocal(st[:, 15:16], st[:, 14:15]), [sths[h][1]], [sths[h][1]])
                yield
                for h in H4:
                    ts(hhs[h][0], tots[h][0][:, 0:256], sths[h][0][:, 15:16], None, ALU.mult, None, [tots[h][1], sths[h][1]], [hhs[h][1]])
                yield
                yield from ln_stages([(hhs[h][0], hhs[h][1], sths[h][0], sths[h][1]) for h in H4])
                for h in H4:
                    osl = oml[c][0][:, h * 256:(h + 1) * 256]
                    tt(osl, osl, hhs[h][0], ALU.mult, [oml[c][1], hhs[h][1]], [oml[c][1]])
            pK = [nextp() for h in H4]
            for h in H4:
                S.op("pe", lambda hd, o=pK[h][0][:, 0:128], i=kTs[h].bitcast(F32): hd.transpose(o, i, ident), [BqkT, Bcst], [pK[h][1]])
            yield
            for h in H4:
                act(kws[h][0], pK[h][0][:, 0:128], AF.Copy, [pK[h][1], Bgsm], [kws[h][1]], scale=u_[:, cols[h]:cols[h] + 1])
            yield
            pC = [nextp() for h in H4]
            for h in H4:
                mm(pC[h][0][:, 0:258], kws[h][0], vxs[h], True, True, [kws[h][1], vml[c][1]], [pC[h][1]])
            yield
            for h in H4:
                stt(Cml[h][0], Cml[h][0], spv[:, cols[h]:cols[h] + 1], pC[h][0][:, 0:257], ALU.mult, ALU.add,
                    [Cml[h][1], Bgsm, pC[h][1]], [Cml[h][1]])
            yield
            for h in H4:
                cp(CmlR[h][0][:, 0:257], Cml[h][0], [Cml[h][1]], [CmlR[h][1]], eng="act")

            yield

        def ret_gen(c):
            vvs = [rv[c][0][:, h * 256:(h + 1) * 256] for h in H4]
            if own:
                qrT, BqrT = qrTs[c]
                krT, BkrT = krTs[c]
                pq, Bpq = nextp()
                pk_, Bpk = nextp()
                for h in H4:
                    S.op("pe", lambda hd, o=pq[:, h * 128:(h + 1) * 128], i=rq[c][0][:, h * 128:(h + 1) * 128]: hd.transpose(o, i, ident),
                         [rq[c][1], Bcst], [Bpq])
                yield
                for h in H4:
                    S.op("pe", lambda hd, o=pk_[:, h * 128:(h + 1) * 128], i=rk[c][0][:, h * 128:(h + 1) * 128]: hd.transpose(o, i, ident),
                         [rk[c][1], Bcst], [Bpk])
                yield
                cp(qrT, pq, [Bpq], [BqrT], eng="act")
                cp(krT, pk_, [Bpk], [BkrT], eng="act")
                pS = [nextp() for h in H4]
                for h in H4:
                    mm(pS[h][0][:, 0:128], krT[:, h * 128:(h + 1) * 128], qrT[:, h * 128:(h + 1) * 128], True, True, [BkrT, BqrT], [pS[h][1]])
                yield
                for h in H4:
                    tt(PTs[h][0], pS[h][0][:, 0:128], cst[:, C_DECT + h * 128:C_DECT + (h + 1) * 128], ALU.mult, [pS[h][1], Bcst], [PTs[h][1]])
                yield
                for h in H4:
                    tt(qdTs[h][0], qrT[:, h * 128:(h + 1) * 128].bitcast(F32), cst[:, C_QD + h * 128:C_QD + (h + 1) * 128], ALU.mult,
                       [BqrT, Bcst], [qdTs[h][1]])
                yield
                pI = [nextp() for h in H4]
                for h in H4:
                    mm(pI[h][0][:, 0:256], PTs[h][0], vvs[h], True, False, [PTs[h][1], rv[c][1]], [pI[h][1]])
                    mm(pI[h][0][:, 0:256], qdTs[h][0], CretR[h][0], False, True, [qdTs[h][1], CretR[h][1]], [pI[h][1]])
                yield
                for h in H4:
                    cp(hhs[h][0], pI[h][0][:, 0:256], [pI[h][1]], [hhs[h][1]], eng="act")
                yield
                yield from ln_stages([(hhs[h][0], hhs[h][1], sths[h][0], sths[h][1]) for h in H4])
                for h in H4:
                    gsl = rg[c][0][:, h * 256:(h + 1) * 256]
                    tt(gsl, gsl, hhs[h][0], ALU.mult, [rg[c][1], hhs[h][1]], [rg[c][1]])
            for h in H4:
                ts(kws[h][0], rk[c][0][:, h * 128:(h + 1) * 128], cst[:, C_KDEC + h:C_KDEC + h + 1], None, ALU.mult, None,
                   [rk[c][1], Bcst], [kws[h][1]])
            yield
            pC = [nextp() for h in H4]
            for h in H4:
                mm(pC[h][0][:, 0:256], kws[h][0], vvs[h], True, True, [kws[h][1], rv[c][1]], [pC[h][1]])
            yield
            for h in H4:
                stt(Cret[h][0], Cret[h][0], GAM[h] ** 128, pC[h][0][:, 0:256], ALU.mult, ALU.add, [Cret[h][1], pC[h][1]], [Cret[h][1]])
            yield
            for h in H4:
                cp(CretR[h][0], Cret[h][0], [Cret[h][1]], [CretR[h][1]], eng="act")

            yield

        for c in range(2):
            for g_ in (ml_gen(c), ret_gen(c)):
                for _ in g_:
                    pass

        if not own:
            continue
        for c in range(2):
            for (src, Bsrc, dst3, Bdst, gT) in ((oml[c][0], oml[c][1], hmT3, BhmT, mlgT), (rg[c][0], rg[c][1], hrT3, BhrT, retgT)):
                for hf in range(2):
                    pb, Bp = nextp()
                    for k4 in range(4):
                        kc = hf * 4 + k4
                        S.op("pe", lambda hd, o=pb[:, k4 * 128:(k4 + 1) * 128], i=src[:, kc * 128:(kc + 1) * 128]: hd.transpose(o, i, ident),
                             [Bsrc, Bcst], [Bp])
                    for k4 in range(4):
                        kc = hf * 4 + k4
                        act(dst3[:, kc, c * 128:(c + 1) * 128], pb[:, k4 * 128:(k4 + 1) * 128], AF.Copy, [Bp, Bsm], [Bdst],
                            scale=gT[:, kc:kc + 1])
        for t in range(4):
            wm3, wmb = wload(wbml.rearrange("(k p) c -> p k c", p=128)[:, :, t * 256:(t + 1) * 256], 256)
            pbm = []
            for f in range(2):
                pb, Bp = nextp()
                for kc in range(8):
                    mm(pb[:, 0:G], wm3[:, kc, f * 128:(f + 1) * 128], hmT3[:, kc, :], kc == 0, kc == 7, [wmb, BhmT], [Bp])
                pbm.append((pb, Bp))
            wg3, wgb = win_tile(6152 + t * 256)
            for f in range(2):
                pb, Bp = nextp()
                for kc in range(8):
                    mm(pb[:, 0:G], wg3[:, kc, f * 128:(f + 1) * 128], h1T3[:, kc, :], kc == 0, kc == 7, [wgb, Bh1T], [Bp])
                act(sg1, pb[:, 0:G], AF.Sigmoid, [Bp], [Bsg1])
                tt(yT3[:, t * 2 + f, :], pbm[f][0][:, 0:G], sg1, ALU.mult, [pbm[f][1], Bsg1], [ByT])
            wr3, wrb = wload(wbret.rearrange("(k p) c -> p k c", p=128)[:, :, t * 256:(t + 1) * 256], 256)
            pbr = []
            for f in range(2):
                pb, Bp = nextp()
                for kc in range(8):
                    mm(pb[:, 0:G], wr3[:, kc, f * 128:(f + 1) * 128], hrT3[:, kc, :], kc == 0, kc == 7, [wrb, BhrT], [Bp])
                pbr.append((pb, Bp))
            wg3, wgb = win_tile(7176 + t * 256)
            for f in range(2):
                pb, Bp = nextp()
                for kc in range(8):
                    mm(pb[:, 0:G], wg3[:, kc, f * 128:(f + 1) * 128], h1T3[:, kc, :], kc == 0, kc == 7, [wgb, Bh1T], [Bp])
                act(sg1, pb[:, 0:G], AF.Sigmoid, [Bp], [Bsg1])
                tt(sg2, pbr[f][0][:, 0:G], sg1, ALU.mult, [pbr[f][1], Bsg1], [Bsg2])
                tt(yT3[:, t * 2 + f, :], yT3[:, t * 2 + f, :].bitcast(F32), sg2, ALU.add, [ByT, Bsg2], [ByT])
        for t in range(4):
            wo3, wob = wload(wout.rearrange("(k p) c -> p k c", p=128)[:, :, t * 256:(t + 1) * 256], 256)
            for c in range(2):
                pb, Bp = nextp()
                for kc in range(8):
                    mm(pb[:, 0:256], yT3[:, kc, c * 128:(c + 1) * 128], wo3[:, kc, :], kc == 0, kc == 7, [ByT, wob], [Bp])
                xsl = xg[c][0][:, t * 256:(t + 1) * 256]
                tt(sg2, pb[:, 0:256], gmb[:, t * 256:(t + 1) * 256], ALU.mult, [Bp, Bgmb], [Bsg2])
                tt(xsl, xsl, sg2, ALU.add, [xg[c][1], Bsg2], [xg[c][1]])
        for c in range(2):
            dma("sp", x2s[t0 + c * 128:t0 + (c + 1) * 128, :], xg[c][0], [xg[c][1]], [], x2sems[c])

    S.barrier()
    top[0] = PERS_END
    BLK = 512
    NBK = (TOK * 4) // BLK + NE
    NSLOT = NBK * BLK
    Xs = nc.dram_tensor("Xs", [NSLOT, D], F32, kind="Internal").ap()
    Ys = nc.dram_tensor("Ys", [NSLOT, D], F32, kind="Internal").ap()
    H2 = nc.dram_tensor("H2", [TOK, D], F32, kind="Internal").ap()
    A2bc, BA2bc = T(D, name="A2bc")
    B2bc, BB2bc = T(D, name="B2bc")
    xc, Bxc = T(D, name="xc")
    xs2, Bxs2 = T(D, name="xs2")
    h2tm, Bh2tm = T(D, name="h2tm")
    h2Tc, Bh2Tc = T(D, F32R, "h2Tc")
    h2Tc3 = h2Tc.rearrange("p (k n) -> p k n", n=128)
    st2, Bst2 = T(16, name="st2")
    lgt, Blgt = T(NE, name="lgt")
    t8, Bt8 = T(16, name="t8")
    maskall, Bmask = T(16 * NE, name="maskall")
    Gwall, BGw = T(16 * NE, name="Gwall")
    tris, Btris = T(128, name="tristrict")
    cntb, Bcnt = T(NE, name="cnt")
    nbi, Bnbi = T(NE, I32, "nbi")
    nbf, Bnbf = T(NE, name="nbf")
    bend, Bbend = T(NE, name="bend")
    sbase, Bsbase = T(NE, name="sbase")
    ones32, Bones32 = T(NE, name="ones32")
    key, Bkey = T(NE, name="key")
    eqt, Beqt = T(NE, name="eqt")
    s4, Bs4 = T(16, name="s4")
    sidxf, Bsidxf = T(64, name="sidxf")
    w4, Bw4 = T(64, name="w4")
    ebrow, Beb = T(NBK, name="ebrow")
    widxf, Bwidxf = T(NBK * 8, name="widxf")
    didxf, Bdidxf = T(NBK * 4, name="didxf")
    bidxf, Bbidxf = T(NBK * 2, name="bidxf")
    Xtm, BXtm = T(4 * D, name="Xtm")
    Xtm3 = Xtm.rearrange("p (j c) -> p j c", c=D)
    XT, BXT = T(8 * BLK, F32R, "XT")
    XT3 = XT.rearrange("p (k n) -> p k n", n=BLK)
    actT, BactT = T(8 * BLK, F32R, "actT")
    actT3 = actT.rearrange("p (k n) -> p k n", n=BLK)
    NU = 3
    wgt = [T(8 * 256, F32R, "wgu%d" % i) for i in range(NU)]
    wgsem = [S.dmasem("wgu%d" % i) for i in range(NU)]
    wq = [T(2 * 1024, F32R, "wd%d" % q) for q in range(4)]
    wq3 = [wq[q][0].rearrange("p (k n) -> p k n", n=1024) for q in range(4)]
    wqsem = [S.dmasem("wd%d" % q) for q in range(4)]
    bgt = [T(16, name="bgt%d" % i) for i in range(2)]
    bgsem = [S.dmasem("bgt%d" % i) for i in range(2)]
    bdb = [T(D, name="bdb%d" % i) for i in range(2)]
    bdsem = [S.dmasem("bdb%d" % i) for i in range(2)]
    gm_ = [T(512, name="gm%d" % i) for i in range(2)]
    sg_ = [T(512, name="sg%d" % i) for i in range(2)]
    lm_ = [T(512, name="lm%d" % i) for i in range(2)]
    dsb = [T(D, name="dsb%d" % i) for i in range(2)]
    acc, Bacc = T(D, name="acc")
    xcsem = S.dmasem("xc")
    h2sem = S.dmasem("h2st")
    h2lsem = S.dmasem("h2ld")
    scs = [S.dmasem("scat%d" % k) for k in range(4)]
    xtsem = S.dmasem("xtm")
    yss = [S.dmasem("ysst%d" % i) for i in range(2)]
    ygsem = S.dmasem("ygat")
    osem = S.dmasem("ost")
    uctr = [0]
    sctr = [0]
    wguh = wgu.rearrange("e (u q) c -> (e u q) c", u=8)
    wdh = wd.rearrange("e (q r) c -> (e q r) c", q=4)

    def ind(ap_):
        return bass.IndirectOffsetOnAxis(ap=ap_, axis=0)

    _bregs = {}

    def bnd(h, v):
        if v not in _bregs:
            _bregs[v] = h.to_reg(v)
        return _bregs[v]

    NIT = 40
    itl = [T(1, I32, "idx%d" % i) for i in range(NIT)]
    ictr = [0]

    def idx_tile(colap, Bsrc):
        i = ictr[0] % NIT
        ictr[0] += 1
        ap_, b_ = itl[i]
        cp(ap_, colap, [Bsrc], [b_])
        return ap_, b_

    def block_idx(b):
        d = {}
        d["bg"] = idx_tile(bidxf[:, b:b + 1], Bbidxf)
        d["bd"] = idx_tile(bidxf[:, NBK + b:NBK + b + 1], Bbidxf)
        for u in range(8):
            d["w%d" % u] = idx_tile(widxf[:, b * 8 + u:b * 8 + u + 1], Bwidxf)
        for q in range(4):
            d["d%d" % q] = idx_tile(didxf[:, b * 4 + q:b * 4 + q + 1], Bdidxf)
        return d

    def bcast_cols(dst, Bdst, colap, Bcol):
        for hf in range(2):
            pb, Bp = nextp()
            for k4 in range(4):
                kc = hf * 4 + k4
                ts(dgt, ident, colap[:, kc:kc + 1], None, ALU.mult, None, [Bcst, Bcol], [Bdgt])
                mm(pb[:, k4 * 128:(k4 + 1) * 128], ones[:, 0:128], dgt, True, True, [Bcst, Bdgt], [Bp])
            cp(dst[:, hf * 512:(hf + 1) * 512], pb, [Bp], [Bdst])
    dgt, Bdgt = T(128, name="dgt2")
    bcast_cols(A2bc, BA2bc, A2, BAB)
    bcast_cols(B2bc, BB2bc, B2, BAB)
    tt(tris, tri, ident, ALU.subtract, [Bcst], [Btris])
    S.op("dve", lambda h: h.memset(ones32, 1.0), [], [Bones32])
    wrt3 = wrt.rearrange("p (k n) -> p k n", n=NE)

    for c in range(16):
        dma("sp", xc, x2s[c * 128:(c + 1) * 128, :], [], [Bxc], xcsem)
        act(xs2, xc, AF.Square, [Bxc], [Bxs2, Bst2], accum=st2[:, 0:1])
        act(st2[:, 1:2], st2[:, 0:1], AF.Sqrt, [Bst2, Bsm], [Bst2], bias=epsc, scale=1.0 / D)
        S.op("dve", lambda h: h.reciprocal(st2[:, 2:3], st2[:, 1:2]), [Bst2], [Bst2])
        ts(xs2, xc, st2[:, 2:3], None, ALU.mult, None, [Bxc, Bst2], [Bxs2])
        tt(h2tm, xs2, A2bc, ALU.mult, [Bxs2, BA2bc], [Bh2tm])
        tt(h2tm, h2tm, B2bc, ALU.add, [Bh2tm, BB2bc], [Bh2tm])
        dma("sp", H2[c * 128:(c + 1) * 128, :], h2tm, [Bh2tm], [], h2sem)
        for hf in range(2):
            pb, Bp = nextp()
            for k4 in range(4):
                kc = hf * 4 + k4
                S.op("pe", lambda h, o=pb[:, k4 * 128:(k4 + 1) * 128], i=h2tm[:, kc * 128:(kc + 1) * 128]: h.transpose(o, i, ident),
                     [Bh2tm, Bcst], [Bp])
            cp(h2Tc[:, hf * 512:(hf + 1) * 512], pb, [Bp], [Bh2Tc], eng="act")
        pl, Bpl = nextp()
        for kc in range(8):
            mm(pl[:, 0:NE], h2Tc3[:, kc, :], wrt3[:, kc, :], kc == 0, kc == 7, [Bh2Tc, Bwrt], [Bpl])
        tt(lgt, pl[:, 0:NE], brb, ALU.add, [Bpl, Bbrb], [Blgt])
        S.op("dve", lambda h: h.max(t8[:, 0:8], lgt), [Blgt], [Bt8])
        ts(maskall[:, c * NE:(c + 1) * NE], lgt, t8[:, 3:4], None, ALU.is_ge, None, [Blgt, Bt8], [Bmask])
        ts(t8[:, 8:9], t8[:, 0:1], -1.0, None, ALU.mult, None, [Bt8], [Bt8])
        act(lgt, lgt, AF.Exp, [Blgt, Bt8], [Blgt], bias=t8[:, 8:9])
        tt(lgt, lgt, maskall[:, c * NE:(c + 1) * NE], ALU.mult, [Blgt, Bmask], [Blgt])
        S.op("dve", lambda h: h.reduce_sum(t8[:, 9:10], lgt, mybir.AxisListType.X), [Blgt], [Bt8])
        S.op("dve", lambda h: h.reciprocal(t8[:, 10:11], t8[:, 9:10]), [Bt8], [Bt8])
        ts(Gwall[:, c * NE:(c + 1) * NE], lgt, t8[:, 10:11], None, ALU.mult, None, [Blgt, Bt8], [BGw])

    pcn, Bpcn = nextp()
    for c in range(16):
        mm(pcn[:, 0:NE], ones[:, 0:128], maskall[:, c * NE:(c + 1) * NE], c == 0, c == 15, [Bcst, Bmask], [Bpcn])
    cp(cntb, pcn[:, 0:NE], [Bpcn], [Bcnt])
    ts(nbi, cntb, 1.0 / BLK, (BLK - 1.0) / BLK - 0.49951171875, ALU.mult, ALU.add, [Bcnt], [Bnbi])
    cp(nbf, nbi, [Bnbi], [Bnbf])
    S.op("dve", lambda h: h.tensor_tensor_scan(bend, ones32, nbf, 0.0, ALU.mult, ALU.add), [Bones32, Bnbf], [Bbend])
    tt(sbase, bend, nbf, ALU.subtract, [Bbend, Bnbf], [Bsbase])
    ts(sbase, sbase, float(BLK), None, ALU.mult, None, [Bsbase], [Bsbase])
    iob = cst[:, C_IOB:C_IOB + NBK]
    S.op("dve", lambda h: h.memset(ebrow, 0.0), [], [Beb])
    for e in range(NE):
        stt(ebrow, iob, bend[:, e:e + 1], ebrow, ALU.is_ge, ALU.add, [Bcst, Bbend, Beb], [Beb])
    ts(ebrow, ebrow, float(NE - 1), None, ALU.min, None, [Beb], [Beb])
    widxf3 = widxf.rearrange("p (b u) -> p b u", u=8)
    for u in range(8):
        ts(widxf3[:, :, u], ebrow, 1024.0, cst[:, C_CU + u:C_CU + u + 1], ALU.mult, ALU.add, [Beb, Bcst], [Bwidxf])
    didxf3 = didxf.rearrange("p (b q) -> p b q", q=4)
    for q in range(4):
        ts(didxf3[:, :, q], ebrow, 512.0, cst[:, C_CU + q:C_CU + q + 1], ALU.mult, ALU.add, [Beb, Bcst], [Bdidxf])
    ts(bidxf[:, 0:NBK], ebrow, 128.0, cst[:, C_CU:C_CU + 1], ALU.mult, ALU.add, [Beb, Bcst], [Bbidxf])
    cp(bidxf[:, NBK:2 * NBK], ebrow, [Beb], [Bbidxf])

    prk, Bprk = nextp()
    for c in range(16):
        for c2 in range(c):
            mm(prk[:, c * NE:(c + 1) * NE], ones[:, 0:128], maskall[:, c2 * NE:(c2 + 1) * NE], c2 == 0, False, [Bcst, Bmask], [Bprk])
        mm(prk[:, c * NE:(c + 1) * NE], tris, maskall[:, c * NE:(c + 1) * NE], c == 0, True, [Btris, Bmask], [Bprk])
    for c in range(16):
        mk = maskall[:, c * NE:(c + 1) * NE]
        tt(key, prk[:, c * NE:(c + 1) * NE], sbase, ALU.add, [Bprk, Bsbase], [Bkey])
        ts(key, key, 1.0, None, ALU.add, None, [Bkey], [Bkey])
        tt(key, key, mk, ALU.mult, [Bkey, Bmask], [Bkey])
        S.op("dve", lambda h: h.max(s4[:, 0:8], key), [Bkey], [Bs4])
        ts(sidxf[:, c * 4:(c + 1) * 4], s4[:, 0:4], -1.0, None, ALU.add, None, [Bs4], [Bsidxf])
        for k in range(4):
            ts(eqt, key, s4[:, k:k + 1], None, ALU.is_equal, None, [Bkey, Bs4], [Beqt])
            tt(eqt, eqt, Gwall[:, c * NE:(c + 1) * NE], ALU.mult, [Beqt, BGw], [Beqt])
            S.op("dve", lambda h, o=w4[:, c * 4 + k:c * 4 + k + 1]: h.reduce_sum(o, eqt, mybir.AxisListType.X), [Beqt], [Bw4])
    lasth2 = S.last[("dma", id(h2sem))]
    for c in range(16):
        S.op("sp", lambda h, c=c: h.dma_start(out=h2tm, in_=H2[c * 128:(c + 1) * 128, :]), [], [Bh2tm], dma=h2lsem, extra=[lasth2])
        for k in range(4):
            ia, Bia = idx_tile(sidxf[:, c * 4 + k:c * 4 + k + 1], Bsidxf)
            S.op("pool", lambda h, ia=ia: h.indirect_dma_start(
                out=Xs[:, :], out_offset=ind(ia), in_=h2tm, in_offset=None, bounds_check=bnd(h, NSLOT - 1), oob_is_err=False),
                [Bh2tm, Bia], [], dma=scs[k])

    lastsc = [S.last[("dma", id(s_))] for s_ in scs]
    nxt_ix = block_idx(0)
    for b in range(NBK):
        bix = nxt_ix
        if b + 1 < NBK:
            nxt_ix = block_idx(b + 1)
        if b == 0:
            S.op("sp", lambda h, b=b: h.dma_start(out=Xtm3, in_=Xs[b * BLK:(b + 1) * BLK, :].rearrange("(j p) c -> p j c", p=128)),
                 [], [BXtm], dma=xtsem, extra=lastsc)
        for j in range(4):
            for hf in range(2):
                pb, Bp = nextp()
                for k4 in range(4):
                    kc = hf * 4 + k4
                    S.op("pe", lambda h, o=pb[:, k4 * 128:(k4 + 1) * 128], i=Xtm3[:, j, kc * 128:(kc + 1) * 128]: h.transpose(o, i, ident),
                         [BXtm, Bcst], [Bp])
                for k4 in range(4):
                    kc = hf * 4 + k4
                    cp(XT3[:, kc, j * 128:(j + 1) * 128], pb[:, k4 * 128:(k4 + 1) * 128], [Bp], [BXT], eng="act")
        if b + 1 < NBK:
            S.op("sp", lambda h, b=b + 1: h.dma_start(out=Xtm3, in_=Xs[b * BLK:(b + 1) * BLK, :].rearrange("(j p) c -> p j c", p=128)),
                 [], [BXtm], dma=xtsem, extra=lastsc)
        bi = b % 2
        ia, Bia = bix["bg"]
        S.op("pool", lambda h, o=bgt[bi][0], ia=ia: h.indirect_dma_start(
            out=o, out_offset=None, in_=bguT_d[:, :], in_offset=ind(ia), bounds_check=bnd(h, NE * 128 - 1), oob_is_err=False),
            [Bia], [bgt[bi][1]], dma=bgsem[bi])
        ia, Bia = bix["bd"]
        S.op("pool", lambda h, o=bdb[bi][0], ia=ia: h.indirect_dma_start(
            out=o, out_offset=None, in_=bd_d[:, :], in_offset=ind(ia), bounds_check=bnd(h, NE - 1), oob_is_err=False),
            [Bia], [bdb[bi][1]], dma=bdsem[bi])
        for u in range(8):
            i = uctr[0] % NU
            uctr[0] += 1
            wv, wb = wgt[i]
            w3 = wv.rearrange("p (k n) -> p k n", n=256)
            ia, Bia = bix["w%d" % u]
            S.op("pool", lambda h, o=wv, ia=ia: h.indirect_dma_start(
                out=o, out_offset=None, in_=wguh[:, :], in_offset=ind(ia), bounds_check=bnd(h, NE * 1024 - 1), oob_is_err=False),
                [Bia], [wb], dma=wgsem[i])
            pg, Bpg = nextp()
            for kc in range(8):
                mm(pg, w3[:, kc, 0:128], XT3[:, kc, :], kc == 0, kc == 7, [wb, BXT], [Bpg])
            plin, Bplin = nextp()
            for kc in range(8):
                mm(plin, w3[:, kc, 128:256], XT3[:, kc, :], kc == 0, kc == 7, [wb, BXT], [Bplin])
            si = sctr[0] % 2
            sctr[0] += 1
            gmv, Bgm = gm_[si]; sgv, Bsg = sg_[si]; lmv, Blm = lm_[si]
            bg = bgt[bi][0]
            ts(gmv, pg, bg[:, u * 2:u * 2 + 1], 7.0, ALU.add, ALU.min, [Bpg, bgt[bi][1]], [Bgm])
            act(sgv, gmv, AF.Sigmoid, [Bgm], [Bsg], scale=1.702)
            act(lmv, plin, AF.Identity, [Bplin, bgt[bi][1]], [Blm], bias=bg[:, u * 2 + 1:u * 2 + 2])
            ts(lmv, lmv, 7.0, -7.0, ALU.min, ALU.max, [Blm], [Blm])
            tt(gmv, gmv, sgv, ALU.mult, [Bgm, Bsg], [Bgm])
            stt(actT3[:, u, :], lmv, 1.0, gmv, ALU.add, ALU.mult, [Blm, Bgm], [BactT])
        for q in range(4):
            ia, Bia = bix["d%d" % q]
            S.op("pool", lambda h, o=wq[q][0], ia=ia: h.indirect_dma_start(
                out=o, out_offset=None, in_=wdh[:, :], in_offset=ind(ia), bounds_check=bnd(h, NE * 512 - 1), oob_is_err=False),
                [Bia], [wq[q][1]], dma=wqsem[q])
        for j in range(4):
            dv, Bd = dsb[j % 2]
            for nt in range(2):
                pd_, Bpd = nextp()
                for ft in range(8):
                    mm(pd_, actT3[:, ft, j * 128:(j + 1) * 128], wq3[ft // 2][:, ft % 2, nt * 512:(nt + 1) * 512], ft == 0, ft == 7,
                       [BactT, wq[ft // 2][1]], [Bpd])
                tt(dv[:, nt * 512:(nt + 1) * 512], pd_, bdb[bi][0][:, nt * 512:(nt + 1) * 512], ALU.add, [Bpd, bdb[bi][1]], [Bd])
            dma("sp", Ys[b * BLK + j * 128:b * BLK + (j + 1) * 128, :], dv, [Bd], [], yss[j % 2])

    lastys = [S.last[("dma", id(y_))] for y_ in yss]
    xtm_prev = [BXtm.w] + list(BXtm.r.values())
    BY = [Buf("Yk%d" % k) for k in range(4)]
    ygs = [S.dmasem("ygat%d" % k) for k in range(4)]
    for c in range(16):
        dma("sp", xc, x2s[c * 128:(c + 1) * 128, :], [], [Bxc], xcsem)
        for k in range(4):
            ia, Bia = idx_tile(sidxf[:, c * 4 + k:c * 4 + k + 1], Bsidxf)
            S.op("pool", lambda h, o=Xtm3[:, k, :], ia=ia: h.indirect_dma_start(
                out=o, out_offset=None, in_=Ys[:, :], in_offset=ind(ia), bounds_check=bnd(h, NSLOT - 1), oob_is_err=False),
                [Bia], [BY[k]], dma=ygs[k], extra=lastys + [d_ for d_ in xtm_prev if d_ is not None])
        ts(acc, Xtm3[:, 0, :], w4[:, c * 4:c * 4 + 1], None, ALU.mult, None, [BY[0], Bw4], [Bacc])
        for k in range(1, 4):
            stt(acc, Xtm3[:, k, :], w4[:, c * 4 + k:c * 4 + k + 1], acc, ALU.mult, ALU.add, [BY[k], Bw4, Bacc], [Bacc])
        tt(acc, acc, gfb, ALU.mult, [Bacc, Bgfb], [Bacc])
        tt(xc, xc, acc, ALU.add, [Bxc, Bacc], [Bxc])
        act(xs2, xc, AF.Square, [Bxc], [Bxs2, Bst2], accum=st2[:, 0:1])
        act(st2[:, 1:2], st2[:, 0:1], AF.Sqrt, [Bst2, Bsm], [Bst2], bias=epsc, scale=1.0 / D)
        S.op("dve", lambda h: h.reciprocal(st2[:, 2:3], st2[:, 1:2]), [Bst2], [Bst2])
        stt(xs2, xc, st2[:, 2:3], gfin, ALU.mult, ALU.mult, [Bxc, Bst2, Bgfin], [Bxs2])
        dma("sp", out[c * 128:(c + 1) * 128, :], xs2, [Bxs2], [], osem)

    print("arena cols: pers", PERS_END, "mix", MIX_END, "moe", top[0], "ops", len(S.ops))
    S.emit(nc, es, final_waits=[osem, h2sem] + scs + x2sems + yss)
    es.close()
    return nc


def _consts():
    c = np.zeros((128, NC), np.float64)
    idx = np.arange(128)
    c[:, C_ID:C_ID + 128] = np.eye(128)
    s = idx[:, None]; j = idx[None, :]
    c[:, C_TRI:C_TRI + 128] = (s <= j)
    c[:, C_MNEG:C_MNEG + 128] = np.where(s <= j, 0.0, -30000.0)
    for h in range(4):
        c[h, C_SEL + h * 128:C_SEL + (h + 1) * 128] = 1.0
        lg = math.log(1.0 - 2.0 ** (-5.0 - h))
        c[:, C_DECT + h * 128:C_DECT + (h + 1) * 128] = np.where(j >= s, np.exp(lg * np.maximum(j - s, 0)), 0.0) * 128.0 ** -0.5
        c[:, C_QD + h * 128:C_QD + (h + 1) * 128] = np.exp(lg * (j + 1.0)) * np.ones((128, 1))
        c[:, C_KDEC + h] = np.exp(lg * (127.0 - idx)) * 128.0 ** -0.5
    c[:, C_ONES:C_ONES + 512] = 1.0
    inv = (10000.0 ** (-np.arange(64, dtype=np.float32) / np.float32(64))).astype(np.float32)
    c[:, C_INVF:C_INVF + 256] = np.tile(inv, 4)[None, :]
    c[:, C_IOB:C_IOB + 64] = np.arange(64)[None, :]
    c[:, C_CU:C_CU + 8] = np.arange(8)[None, :] * 128 + idx[:, None]
    return c.astype(np.float32)


_NC_CACHE = {}


def _colT(v, n):
    return np.ascontiguousarray(np.asarray(v, np.float32).reshape(n, 128).T)


def kernel(x, c, positions, w_ada, b_ada, norm_mix_g, w_in, conv_w, conv_b, b_if, ml_norm_g, ret_norm_g,
           w_branch_ml, w_branch_ret, w_out, norm_ffn_g, w_router, b_router, w_gate_up, b_gate_up, w_down, b_down,
           norm_final_g, _debug=False):
    f = lambda a: np.ascontiguousarray(np.asarray(a, np.float32))
    x = f(x); c = f(c)
    positions = np.asarray(positions).astype(np.int32)
    key = bool(_debug)
    if key not in _NC_CACHE:
        _NC_CACHE[key] = build_nc(debug=key)
    nc = _NC_CACHE[key]
    wgu_p = f(w_gate_up)[0].reshape(NE, 8, 128, 8, 128, 2)
    wgu_p = np.ascontiguousarray(wgu_p.transpose(0, 3, 2, 1, 5, 4)).reshape(NE, D, 2 * D)
    wd_p = f(w_down)[0].reshape(NE, 4, 2, 128, D)
    wd_p = np.ascontiguousarray(wd_p.transpose(0, 1, 3, 2, 4)).reshape(NE, D // 2, 2 * D)
    bgu = f(b_gate_up)[0].reshape(NE, D, 2)
    bguT = np.stack([bgu[..., 0].reshape(NE, 8, 128), bgu[..., 1].reshape(NE, 8, 128)], axis=2)
    bguT = np.ascontiguousarray(bguT.transpose(0, 3, 1, 2).reshape(NE * 128, 16))
    shared = {
        "cst": _consts(),
        "w_ada": f(w_ada)[0], "b_adaT": _colT(f(b_ada)[0], 48),
        "gmixT": _colT(f(norm_mix_g)[0], 8), "gffnT": _colT(f(norm_ffn_g)[0], 8), "gfin": f(norm_final_g).reshape(1, D),
        "w_in": f(w_in)[0],
        "convwT": np.ascontiguousarray(f(conv_w)[0].T.reshape(8, 128, 4).transpose(1, 0, 2).reshape(128, 32)),
        "convbT": _colT(f(conv_b)[0], 8), "bif": f(b_if)[0].reshape(1, 8),
        "mlgT": _colT(f(ml_norm_g)[0], 8), "retgT": _colT(f(ret_norm_g)[0], 8),
        "wbml": f(w_branch_ml)[0], "wbret": f(w_branch_ret)[0], "wout": f(w_out)[0],
        "wr": f(w_router)[0], "br": f(b_router)[0].reshape(1, NE),
        "wgu": wgu_p, "bguT": bguT, "wd": wd_p, "bd": f(b_down)[0],
    }
    in_maps = []
    for i in range(8):
        b, half = i // 2, i % 2
        m = dict(shared)
        m["xo"] = np.ascontiguousarray(x[b, half * TOK:(half + 1) * TOK])
        m["xp"] = np.ascontiguousarray(x[b, 0:TOK])
        m["poso"] = np.ascontiguousarray(positions[b, half * TOK:(half + 1) * TOK].reshape(16, 128).T)
        m["posp"] = np.ascontiguousarray(positions[b, 0:TOK].reshape(16, 128).T)
        m["flag"] = np.full((128, 1), float(half), np.float32)
        m["cT"] = _colT(c[b], 8)
        in_maps.append(m)
    res = run_bass_kernel_spmd(nc, in_maps, core_ids=list(range(8)))
    outp = np.zeros((4, SEQ, D), np.float32)
    for i in range(8):
        b, half = i // 2, i % 2
        outp[b, half * TOK:(half + 1) * TOK] = res.results[i]["out"]
    if _debug:
        dbg = np.zeros((4, SEQ, D), np.float32)
        for i in range(8):
            b, half = i // 2, i % 2
            dbg[b, half * TOK:(half + 1) * TOK] = res.results[i]["x2s"]
        return outp, dbg
    return outp
```

```python
import contextlib
import math
import numpy as np
import concourse.bass as bass
import concourse.mybir as mybir
from concourse.bass_utils import run_bass_kernel_spmd

F32 = mybir.dt.float32
F32R = mybir.dt.float32r
I32 = mybir.dt.int32
ALU = mybir.AluOpType
AF = mybir.ActivationFunctionType

D = 1024
SEQ = 4096
TOK = 2048
G = 256
NE = 32
EPS = 1e-5
LNSCALE = math.log(128.0 ** -0.5)
TWO_PI = 6.283185307179586

C_ID, C_TRI, C_MNEG, C_SEL, C_ONES, C_DECT, C_QD, C_KDEC, C_INVF = 0, 128, 256, 384, 896, 1408, 1920, 2432, 2436
C_IOB, C_CU = 2692, 2756
NC = 2764


class Buf:
    __slots__ = ("name", "w", "r")

    def __init__(self, name=""):
        self.name = name
        self.w = None
        self.r = {}


class DmaSem:
    __slots__ = ("sem", "value", "name")

    def __init__(self, name):
        self.name = name
        self.sem = None
        self.value = 0


class Op:
    __slots__ = ("eng", "fn", "deps", "needed", "tok", "dma")


class Sched:
    ENGS = ("pe", "dve", "act", "pool", "sp")

    def __init__(self, sync_same=True):
        self.ops = []
        self.sync_same = sync_same
        self.dmasems = []
        self.last = {}

    def dmasem(self, name):
        d = DmaSem(name)
        self.dmasems.append(d)
        return d

    def op(self, eng, fn, reads=(), writes=(), dma=None, extra=()):
        o = Op()
        o.eng, o.fn, o.dma = eng, fn, dma
        o.needed = dma is not None
        o.tok = None
        deps = {}
        for b in reads:
            if b.w is not None:
                deps[id(b.w)] = b.w
        for b in writes:
            if b.w is not None:
                deps[id(b.w)] = b.w
            for d in b.r.values():
                deps[id(d)] = d
        for d in extra:
            deps[id(d)] = d
        o.deps = []
        for d in deps.values():
            if d is o:
                continue
            if d.dma is None and d.eng == eng and (eng == "pe" or not self.sync_same):
                continue
            d.needed = True
            o.deps.append(d)
        key = ("dma", id(dma)) if dma is not None else eng
        for b in reads:
            b.r[key] = o
        for b in writes:
            b.w = o
            b.r = {}
        self.ops.append(o)
        self.last[key] = o
        return o

    def barrier(self):
        lasts = list(self.last.values())
        for e in self.ENGS:
            self.op(e, None, extra=[d for d in lasts if d.fn is not None])

    def emit(self, nc, es, final_waits=()):
        esem = {e: es.enter_context(nc.semaphore("s_" + e)) for e in self.ENGS}
        for d in self.dmasems:
            d.sem = es.enter_context(nc.semaphore("d_" + d.name))
            d.value = 0
        cnt = {e: 0 for e in self.ENGS}
        for o in self.ops:
            if o.fn is None:
                continue
            if o.dma is not None:
                o.dma.value += 16
                o.tok = (o.dma.sem, o.dma.value)
            elif o.needed:
                cnt[o.eng] += 1
                o.tok = (esem[o.eng], cnt[o.eng])
        block = es.enter_context(nc.Block())
        per = {e: [o for o in self.ops if o.eng == e] for e in self.ENGS}

        def run(e, h):
            waited = {}
            for o in per[e]:
                need = {}
                for d in o.deps:
                    s, v = d.tok
                    k = id(s)
                    if waited.get(k, 0) >= v:
                        continue
                    if k not in need or need[k][1] < v:
                        need[k] = (s, v)
                for k, (s, v) in need.items():
                    h.wait_ge(s, v)
                    waited[k] = v
                if o.fn is None:
                    continue
                ins = o.fn(h)
                if o.tok is not None:
                    ins.then_inc(o.tok[0], 16 if o.dma is not None else 1)
            if e == "sp":
                for d in final_waits:
                    h.wait_ge(d.sem, d.value)

        @block.tensor
        def _(h):
            run("pe", h)

        @block.vector
        def _(h):
            run("dve", h)

        @block.scalar
        def _(h):
            run("act", h)

        @block.gpsimd
        def _(h):
            run("pool", h)

        @block.sync
        def _(h):
            run("sp", h)


def build_nc(debug=False):
    nc = bass.Bass("TRN2", target_bir_lowering=False)

    def din(name, shape, dt=F32):
        return nc.dram_tensor(name, list(shape), dt, kind="ExternalInput").ap()

    xo = din("xo", [TOK, D]); xp = din("xp", [TOK, D])
    poso = din("poso", [128, 16], I32); posp = din("posp", [128, 16], I32)
    flag_d = din("flag", [128, 1]); cT_d = din("cT", [128, 8]); cst_d = din("cst", [128, NC])
    w_ada = din("w_ada", [D, 6 * D]); b_adaT = din("b_adaT", [128, 48])
    gmixT_d = din("gmixT", [128, 8]); gffnT_d = din("gffnT", [128, 8]); gfin_d = din("gfin", [1, D])
    w_in = din("w_in", [D, 8200]); convwT_d = din("convwT", [128, 32]); convbT_d = din("convbT", [128, 8])
    bif_d = din("bif", [1, 8]); mlgT_d = din("mlgT", [128, 8]); retgT_d = din("retgT", [128, 8])
    wbml = din("wbml", [D, D]); wbret = din("wbret", [D, D]); wout = din("wout", [D, D])
    wr_d = din("wr", [D, NE]); br_d = din("br", [1, NE])
    wgu = din("wgu", [NE, D, 2 * D]); bguT_d = din("bguT", [NE * 128, 16])
    wd = din("wd", [NE, D // 2, 2 * D]); bd_d = din("bd", [NE, D])
    out = nc.dram_tensor("out", [TOK, D], F32, kind="ExternalOutput").ap()
    x2s = nc.dram_tensor("x2s", [TOK, D], F32, kind="ExternalOutput" if debug else "Internal").ap()

    S = Sched()
    es = contextlib.ExitStack()
    NCOL = 53180
    pbanks = [es.enter_context(nc.psum_tensor("pb%d" % i, [128, 512], F32)) for i in range(8)]
    PB = [Buf("pb%d" % i) for i in range(8)]
    pctr = [0]

    def nextp():
        i = pctr[0] % 8
        pctr[0] += 1
        return pbanks[i][:, :], PB[i]

    top = [0]
    tcount = [0]

    def al(n):
        n = (n + 7) // 8 * 8
        a = top[0]
        top[0] += n
        assert top[0] <= NCOL, top[0]
        return a

    def T(n, dt=F32, name=""):
        a = al(n)
        tcount[0] += 1
        t = nc.alloc_sbuf_tensor_at("t%d_%s" % (tcount[0], name), [128, n], dt, offset=16640 + 4 * a)
        return t[:, :], Buf(name)

    def mm(o, lhsT, rhs, st, sp, rd, wr):
        S.op("pe", lambda h: h.matmul(o, lhsT=lhsT, rhs=rhs, start=st, stop=sp), rd, wr)

    def act(o, i, func, rd, wr, bias=None, scale=None, accum=None):
        kw = {}
        if bias is not None:
            kw["bias"] = bias
        if scale is not None:
            kw["scale"] = scale
        if accum is not None:
            kw["accum_out"] = accum
        S.op("act", lambda h: h.activation(o, i, func, **kw), rd, wr)

    def ts(o, i, s1, s2, op0, op1, rd, wr, eng="dve"):
        if op1 is None:
            S.op(eng, lambda h: h.tensor_scalar(o, i, s1, None, op0), rd, wr)
        else:
            S.op(eng, lambda h: h.tensor_scalar(o, i, s1, s2, op0, op1), rd, wr)

    def tt(o, a, b, op, rd, wr, eng="dve"):
        S.op(eng, lambda h: h.tensor_tensor(o, a, b, op), rd, wr)

    def stt(o, a, s, b, op0, op1, rd, wr):
        S.op("dve", lambda h: h.scalar_tensor_tensor(o, a, s, b, op0, op1), rd, wr)

    def cp(o, i, rd, wr, eng="dve"):
        if eng == "act":
            S.op(eng, lambda h: h.copy(o, i), rd, wr)
        else:
            S.op(eng, lambda h: h.tensor_copy(o, i), rd, wr)

    def dma(q, o, i, rd, wr, sem):
        S.op(q, lambda h: h.dma_start(out=o, in_=i), rd, wr, dma=sem)

    cst, Bcst = T(NC, name="cst")
    ident = cst[:, C_ID:C_ID + 128]
    tri = cst[:, C_TRI:C_TRI + 128]
    mneg = cst[:, C_MNEG:C_MNEG + 128]
    ones = cst[:, C_ONES:C_ONES + 512]
    onesR, BonesR = T(512, F32R, "onesR")
    modT, BmodT = T(48, name="modT")
    AB, BAB = T(32, name="AB")
    sm, Bsm = T(160, name="small")
    gmixT = sm[:, 0:8]; gffnT = sm[:, 8:16]; badaT = sm[:, 16:64]; convw = sm[:, 64:96]; convb = sm[:, 96:104]
    bifb = sm[:, 104:112]; flag = sm[:, 112:113]; epsc = sm[:, 113:114]; cTt = sm[:, 114:122]
    mlgT = sm[:, 122:130]; retgT = sm[:, 130:138]
    cact2, Bcact = T(16, F32R, "cact2")
    gfb, Bgfb = T(D, name="gate_f_bc")
    gfin, Bgfin = T(D, name="gfin_bc")
    wrt, Bwrt = T(8 * NE, F32R, "wr")
    brb, Bbrb = T(NE, name="br")
    posf, Bposf = T(32, name="posf")
    posi, Bposi = T(32, I32, "posi")
    PERS_END = top[0]

    dl = {n: S.dmasem(n) for n in ["c0", "c1", "c2", "c3", "c4", "c5", "c6", "c7", "c8", "c9", "c10", "c11", "c12", "c13", "c14", "c15", "c16"]}
    dma("sp", cst, cst_d[:, :], [], [Bcst], dl["c0"])
    dma("sp", gmixT, gmixT_d[:, :], [], [Bsm], dl["c1"])
    dma("sp", gffnT, gffnT_d[:, :], [], [Bsm], dl["c1"])
    dma("sp", badaT, b_adaT[:, :], [], [Bsm], dl["c1"])
    dma("sp", convw, convwT_d[:, :], [], [Bsm], dl["c1"])
    dma("sp", convb, convbT_d[:, :], [], [Bsm], dl["c1"])
    dma("sp", bifb, bif_d[0:1, :].partition_broadcast(128), [], [Bsm], dl["c1"])
    dma("sp", flag, flag_d[:, :], [], [Bsm], dl["c1"])
    dma("sp", cTt, cT_d[:, :], [], [Bsm], dl["c1"])
    dma("sp", mlgT, mlgT_d[:, :], [], [Bsm], dl["c1"])
    dma("sp", retgT, retgT_d[:, :], [], [Bsm], dl["c1"])
    S.op("dve", lambda h: h.memset(epsc, EPS), [], [Bsm])
    cp(onesR, ones, [Bcst], [BonesR])
    dma("sp", gfin, gfin_d[0:1, :].partition_broadcast(128), [], [Bgfin], dl["c2"])
    dma("pool", wrt.rearrange("p (k n) -> p k n", n=NE), wr_d.rearrange("(k p) n -> p k n", p=128), [], [Bwrt], dl["c3"])
    dma("sp", brb, br_d[0:1, :].partition_broadcast(128), [], [Bbrb], dl["c4"])
    dma("sp", posi[:, 0:16], posp[:, :], [], [Bposi], dl["c6"])
    dma("sp", posi[:, 16:32], poso[:, :], [], [Bposi], dl["c6"])
    cp(posf, posi, [Bposi], [Bposf])

    gmb, Bgmb = T(D, name="gate_m_bc")
    Cml = [T(257, F32, "Cml%d" % h) for h in range(4)]
    CmlR = [T(258, F32R, "CmlR%d" % h) for h in range(4)]
    Cret = [T(256, F32, "Cret%d" % h) for h in range(4)]
    CretR = [T(256, F32R, "CretR%d" % h) for h in range(4)]
    rowA, BrowA = T(600, name="rows")
    bTr = rowA[0:4, 0:256]; Mr = rowA[0:4, 256:512]; mpr = rowA[0:4, 512:530]; Ar = rowA[0:4, 530:532]
    ext = rowA[0:4, 532:540]
    xg = [T(D, name="xg%d" % c) for c in range(2)]
    xs, Bxs = T(D, name="xs")
    h1T, Bh1T = T(8 * G, F32R, "h1T")
    h1T3 = h1T.rearrange("p (k n) -> p k n", n=G)
    NW = 3
    wst = [T(8 * 256, F32R, "wst%d" % i) for i in range(NW)]
    wsem = [S.dmasem("wst%d" % i) for i in range(NW)]
    wctr = [0]
    qkpre, Bqkpre = T(8 * 259, name="qkpre")
    qkpre3 = qkpre.rearrange("p (f n) -> p f n", n=259)
    cacc, Bcacc = T(G, name="cacc")
    qkT, BqkT = T(8 * G, F32R, "qkT")
    qkT3 = qkT.rearrange("p (f n) -> p f n", n=G)
    vml = [T(4 * 258, F32R, "vml%d" % c) for c in range(2)]
    oml = [T(D, name="oml%d" % c) for c in range(2)]
    ifb = [T(8, name="if%d" % c) for c in range(2)]
    rq = [T(512, name="rq%d" % c) for c in range(2)]
    rk = [T(512, name="rk%d" % c) for c in range(2)]
    rv = [T(D, F32R, "rv%d" % c) for c in range(2)]
    rg = [T(D, name="rg%d" % c) for c in range(2)]
    gsm, Bgsm = T(104, name="gsm")
    NMh, BNM = T(4 * G, name="NM")
    hmT, BhmT = T(8 * G, F32R, "hmT")
    hrT, BhrT = T(8 * G, F32R, "hrT")
    yT, ByT = T(8 * G, F32R, "yT")
    hmT3 = hmT.rearrange("p (k n) -> p k n", n=G); hrT3 = hrT.rearrange("p (k n) -> p k n", n=G)
    yT3 = yT.rearrange("p (k n) -> p k n", n=G)
    DTs = [T(128, name="DT%d" % h) for h in range(4)]
    PTs = [T(128, F32R, "PT%d" % h) for h in range(4)]
    kws = [T(128, F32R, "kw%d" % h) for h in range(4)]
    inss = [T(258, name="inter_s%d" % h) for h in range(4)]
    tots = [T(258, name="tot%d" % h) for h in range(4)]
    hhs = [(inss[h][0][:, 0:256], inss[h][1]) for h in range(4)]
    sths = [T(16, name="sth%d" % h) for h in range(4)]
    qdTs = kws
    st6, Bst6 = T(16, name="stats")
    rot, Brot = T(768, name="rot")
    sg2, Bsg2 = rot[:, 0:256], Brot
    sct = [T(512, name="sincos%d" % c) for c in range(2)]
    kint, Bkint = T(256, I32, "kint")
    rtmp, Brtmp = xs, Bxs
    qrTs = [T(512, F32R, "qrT%d" % c) for c in range(2)]
    krTs = [T(512, F32R, "krT%d" % c) for c in range(2)]
    sg1, Bsg1 = cacc, Bcacc
    dgt, Bdgt = DTs[0]
    x2sems = [S.dmasem("x2st%d" % c) for c in range(2)]
    xsem = [S.dmasem("xld%d" % c) for c in range(2)]
    MIX_END = top[0]

    for h in range(4):
        S.op("dve", lambda hh_, a=Cml[h][0]: hh_.memset(a, 0.0), [], [Cml[h][1]])
        cp(CmlR[h][0][:, 0:257], Cml[h][0], [Cml[h][1]], [CmlR[h][1]])
        cp(CmlR[h][0][:, 257:258], Cml[h][0][:, 0:1], [Cml[h][1]], [CmlR[h][1]])
        S.op("dve", lambda hh_, a=Cret[h][0]: hh_.memset(a, 0.0), [], [Cret[h][1]])
        cp(CretR[h][0], Cret[h][0], [Cret[h][1]], [CretR[h][1]])
    S.op("dve", lambda h: h.memset(qkpre, 0.0), [], [Bqkpre])
    S.op("dve", lambda h: h.memset(rowA, 0.0), [], [BrowA])
    for c in range(2):
        for q_ in range(0, 1032, 512):
            n_ = min(512, 1032 - q_)
            cp(vml[c][0][:, q_:q_ + n_], ones[:, 0:n_], [Bcst], [vml[c][1]])

    def wload(src3, ncols):
        i = wctr[0] % NW
        wctr[0] += 1
        ap, b = wst[i]
        v3 = ap[:, 0:8 * ncols].rearrange("p (k n) -> p k n", n=ncols)
        dma("pool", v3, src3, [], [b], wsem[i])
        return v3, b

    def win_tile(c0, ncols=256):
        return wload(w_in.rearrange("(k p) c -> p k c", p=128)[:, :, c0:c0 + ncols], ncols)

    act(cact2.rearrange("p (k t) -> p k t", t=2)[:, :, 0], cTt, AF.Silu, [Bsm], [Bcact])
    act(cact2.rearrange("p (k t) -> p k t", t=2)[:, :, 1], cTt, AF.Silu, [Bsm], [Bcact])
    cact3 = cact2.rearrange("p (k t) -> p k t", t=2)
    pm, Bpm = nextp()
    wada3 = w_ada.rearrange("(k p) c -> p k c", p=128)
    for j in range(6):
        for sb_ in range(4):
            w3, wb = wload(wada3[:, :, j * D + sb_ * 256: j * D + (sb_ + 1) * 256], 256)
            for ft in range(2):
                col = (j * 8 + sb_ * 2 + ft) * 2
                for kc in range(8):
                    mm(pm[:, col:col + 2], w3[:, kc, ft * 128:(ft + 1) * 128], cact3[:, kc, :], kc == 0, kc == 7,
                       [wb, Bcact], [Bpm])
    tt(modT, pm[:, 0:96].rearrange("p (c t) -> p c t", t=2)[:, :, 0], badaT, ALU.add, [Bpm, Bsm], [BmodT])
    stt(AB[:, 0:8], modT[:, 8:16], 1.0, gmixT, ALU.add, ALU.mult, [BmodT, Bsm], [BAB])
    cp(AB[:, 8:16], modT[:, 0:8], [BmodT], [BAB])
    stt(AB[:, 16:24], modT[:, 32:40], 1.0, gffnT, ALU.add, ALU.mult, [BmodT, Bsm], [BAB])
    cp(AB[:, 24:32], modT[:, 24:32], [BmodT], [BAB])
    A1 = AB[:, 0:8]; B1 = AB[:, 8:16]; A2 = AB[:, 16:24]; B2 = AB[:, 24:32]

    def bcast_vec(dst, Bdst, col0):
        for hf in range(2):
            pb, Bp = nextp()
            for k4 in range(4):
                kc = hf * 4 + k4
                ts(dgt, ident, modT[:, col0 + kc:col0 + kc + 1], None, ALU.mult, None, [Bcst, BmodT], [Bdgt])
                mm(pb[:, k4 * 128:(k4 + 1) * 128], ones[:, 0:128], dgt, True, True, [Bcst, Bdgt], [Bp])
            cp(dst[:, hf * 512:(hf + 1) * 512], pb, [Bp], [Bdst])

    bcast_vec(gmb, Bgmb, 16)
    bcast_vec(gfb, Bgfb, 40)

    def norm_to_T(xsrc, Bx, dstT3, BdstT, c, Acol, Bcol):
        act(xs, xsrc, AF.Square, [Bx], [Bxs, Bst6], accum=st6[:, 0:1])
        act(st6[:, 1:2], st6[:, 0:1], AF.Sqrt, [Bst6, Bsm], [Bst6], bias=epsc, scale=1.0 / D)
        S.op("dve", lambda h: h.reciprocal(st6[:, 2:3], st6[:, 1:2]), [Bst6], [Bst6])
        ts(xs, xsrc, st6[:, 2:3], None, ALU.mult, None, [Bx, Bst6], [Bxs])
        for hf in range(2):
            pb, Bp = nextp()
            for k4 in range(4):
                kc = hf * 4 + k4
                S.op("pe", lambda h, o=pb[:, k4 * 128:(k4 + 1) * 128], i=xs[:, kc * 128:(kc + 1) * 128]: h.transpose(o, i, ident),
                     [Bxs, Bcst], [Bp])
            for k4 in range(4):
                kc = hf * 4 + k4
                act(dstT3[:, kc, c * 128:(c + 1) * 128], pb[:, k4 * 128:(k4 + 1) * 128], AF.Identity, [Bp, BAB], [BdstT],
                    bias=Bcol[:, kc:kc + 1], scale=Acol[:, kc:kc + 1])

    def proj_fm(c0, nft, evac):
        for t in range(nft // 2):
            w3, wb = win_tile(c0 + t * 256)
            for f in range(2):
                pb, Bp = nextp()
                for kc in range(8):
                    mm(pb[:, 0:G], w3[:, kc, f * 128:(f + 1) * 128], h1T3[:, kc, :], kc == 0, kc == 7, [wb, Bh1T], [Bp])
                evac(t * 2 + f, pb[:, 0:G], Bp)

    def proj_tm(c0, ntile, evac, ncols=256):
        for t in range(ntile):
            w3, wb = win_tile(c0 + t * ncols, ncols)
            for c in range(2):
                pb, Bp = nextp()
                for kc in range(8):
                    mm(pb[:, 0:ncols], h1T3[:, kc, c * 128:(c + 1) * 128], w3[:, kc, :], kc == 0, kc == 7, [Bh1T, wb], [Bp])
                evac(t, c, pb[:, 0:ncols], Bp)

    def ln_stages(items):
        for hv, Bh, st, Bst in items:
            S.op("dve", lambda h, st=st, hv=hv: h.bn_stats(st[:, 4:10], hv), [Bh], [Bst])
        yield
        for hv, Bh, st, Bst in items:
            S.op("dve", lambda h, st=st: h.bn_aggr(st[:, 10:12], st[:, 4:10]), [Bst], [Bst])
        yield
        for hv, Bh, st, Bst in items:
            act(st[:, 12:13], st[:, 11:12], AF.Sqrt, [Bst, Bsm], [Bst], bias=epsc, scale=1.0)
        yield
        for hv, Bh, st, Bst in items:
            S.op("dve", lambda h, st=st: h.reciprocal(st[:, 13:14], st[:, 12:13]), [Bst], [Bst])
        yield
        for hv, Bh, st, Bst in items:
            ts(hv, hv, st[:, 10:11], st[:, 13:14], ALU.subtract, ALU.mult, [Bh, Bst], [Bh])
        yield

    GAM = [1.0 - 2.0 ** (-5.0 - h) for h in range(4)]

    for g in range(16):
        own = g >= 8
        xsrc = xo if own else xp
        t0 = (g - 8) * G if own else g * G
        for c in range(2):
            dma("sp", xg[c][0], xsrc[t0 + c * 128:t0 + (c + 1) * 128, :], [], [xg[c][1]], xsem[c])
            norm_to_T(xg[c][0], xg[c][1], h1T3, Bh1T, c, A1, B1)
        ang = rot[:, 0:256]; red = rot[:, 256:512]; kf = rot[:, 512:768]
        for c in range(2):
            pcol = (16 if own else 0) + (g % 8) * 2 + c
            ts(ang, cst[:, C_INVF:C_INVF + 256], posf[:, pcol:pcol + 1], None, ALU.mult, None, [Bcst, Bposf], [Brot])
            for dst_, off in ((sct[c][0][:, 0:256], 0.0), (sct[c][0][:, 256:512], math.pi / 2)):
                ts(red, ang, off, None, ALU.add, None, [Brot], [Brot])
                ts(kint, red, 1.0 / TWO_PI, None, ALU.mult, None, [Brot], [Bkint])
                cp(kf, kint, [Bkint], [Brot])
                stt(red, kf, -TWO_PI, red, ALU.mult, ALU.add, [Brot], [Brot])
                ts(red, red, 3.14159, -3.14159, ALU.min, ALU.max, [Brot], [Brot])
                act(dst_, red, AF.Sin, [Brot], [sct[c][1]])
        if g == 8:
            ts(qkpre, qkpre, flag, None, ALU.mult, None, [Bqkpre, Bsm], [Bqkpre])
            ts(mpr[:, 0:1], mpr[:, 0:1], flag[0:4, :], None, ALU.mult, None, [BrowA, Bsm], [BrowA])
            for h in range(4):
                ts(Cml[h][0], Cml[h][0], flag, None, ALU.mult, None, [Cml[h][1], Bsm], [Cml[h][1]])
                cp(CmlR[h][0][:, 0:257], Cml[h][0], [Cml[h][1]], [CmlR[h][1]])
                ts(Cret[h][0], Cret[h][0], flag, None, ALU.mult, None, [Cret[h][1], Bsm], [Cret[h][1]])
                cp(CretR[h][0], Cret[h][0], [Cret[h][1]], [CretR[h][1]])

        need_q = own or g == 7

        def ev_qk(base):
            def f(ft, ps_, Bp):
                cp(qkpre3[:, base + ft, 3:259], ps_, [Bp], [Bqkpre], eng="act")
            return f
        proj_fm(512, 4, ev_qk(4))
        if need_q:
            proj_fm(0, 4, ev_qk(0))
        for ft in (range(8) if need_q else range(4, 8)):
            ts(cacc, qkpre3[:, ft, 0:G], convw[:, ft * 4:ft * 4 + 1], None, ALU.mult, None, [Bqkpre, Bsm], [Bcacc])
            for i in range(1, 4):
                stt(cacc, qkpre3[:, ft, i:i + G], convw[:, ft * 4 + i:ft * 4 + i + 1], cacc, ALU.mult, ALU.add,
                    [Bqkpre, Bsm, Bcacc], [Bcacc])
            act(qkT3[:, ft, :], cacc, AF.Silu, [Bcacc, Bsm], [BqkT], bias=convb[:, ft:ft + 1])
            cp(qkpre3[:, ft, 0:3], qkpre3[:, ft, G:G + 3], [Bqkpre], [Bqkpre])

        def ev_v(t, c, ps_, Bp):
            cp(vml[c][0][:, t * 258:t * 258 + 256], ps_, [Bp], [vml[c][1]], eng="act")
        proj_tm(1024, 4, ev_v)

        def ev_if(t, c, ps_, Bp):
            tt(ifb[c][0], ps_, bifb, ALU.add, [Bp, Bsm], [ifb[c][1]])
        proj_tm(3072, 1, ev_if, ncols=8)
        if own:
            def ev_o(t, c, ps_, Bp):
                act(oml[c][0][:, t * 256:(t + 1) * 256], ps_, AF.Sigmoid, [Bp], [oml[c][1]])
            proj_tm(2048, 4, ev_o)

        lf = gsm[:, 0:8]; a_ = gsm[:, 8:16]; b_ = gsm[:, 16:24]; tmp8 = gsm[:, 24:32]; bb = gsm[:, 32:40]
        u_ = gsm[:, 40:48]; isc = gsm[:, 48:56]; emt = gsm[:, 56:64]; spv = gsm[:, 64:72]; MT = gsm[:, 72:80]
        MLb = gsm[:, 80:88]; MPb = gsm[:, 88:96]; tmp8b = gsm[:, 96:104]
        for c in range(2):
            fp = ifb[c][0][:, 4:8]
            act(tmp8[:, c * 4:c * 4 + 4], fp, AF.Abs, [ifb[c][1]], [Bgsm])
            act(tmp8[:, c * 4:c * 4 + 4], tmp8[:, c * 4:c * 4 + 4], AF.Exp, [Bgsm], [Bgsm], scale=-1.0)
            act(tmp8[:, c * 4:c * 4 + 4], tmp8[:, c * 4:c * 4 + 4], AF.Ln, [Bgsm], [Bgsm], bias=1.0)
            ts(lf[:, c * 4:c * 4 + 4], fp, 0.0, None, ALU.min, None, [ifb[c][1]], [Bgsm])
            tt(lf[:, c * 4:c * 4 + 4], lf[:, c * 4:c * 4 + 4], tmp8[:, c * 4:c * 4 + 4], ALU.subtract, [Bgsm], [Bgsm])
        pa, Bpa = nextp()
        mm(pa[:, 0:8], tri, lf, True, True, [Bcst, Bgsm], [Bpa])
        for c in range(2):
            mm(pa[0:4, 16 + c:17 + c], lf[:, c * 4:c * 4 + 4], ones[:, 0:1], True, True, [Bgsm, Bcst], [Bpa])
        cp(a_, pa[:, 0:8], [Bpa], [Bgsm])
        cp(Ar, pa[0:4, 16:18], [Bpa], [BrowA])
        for c in range(2):
            tt(b_[:, c * 4:c * 4 + 4], ifb[c][0][:, 0:4], a_[:, c * 4:c * 4 + 4], ALU.subtract, [ifb[c][1], Bgsm], [Bgsm])
        pt_, Bpt = nextp()
        for c in range(2):
            S.op("pe", lambda h, o=pt_[0:4, c * 128:(c + 1) * 128], i=b_[:, c * 4:c * 4 + 4]: h.transpose(o, i, ident),
                 [Bgsm, Bcst], [Bpt])
        cp(bTr, pt_[0:4, 0:256], [Bpt], [BrowA])
        for c in range(2):
            ci = (g % 8) * 2 + c if False else c
            S.op("dve", lambda h, o=Mr[:, c * 128:(c + 1) * 128], d=bTr[:, c * 128:(c + 1) * 128], ini=mpr[:, c:c + 1]:
                 h.tensor_tensor_scan(o, d, d, ini, ALU.max, ALU.max), [BrowA], [BrowA])
            tt(mpr[:, c + 1:c + 2], Ar[:, c:c + 1], Mr[:, c * 128 + 127:c * 128 + 128], ALU.add, [BrowA], [BrowA])
            cp(ext[:, c:c + 1], Mr[:, c * 128 + 127:c * 128 + 128], [BrowA], [BrowA])
            cp(ext[:, 2 + c:3 + c], mpr[:, c:c + 1], [BrowA], [BrowA])
        cp(mpr[:, 0:1], mpr[:, 2:3], [BrowA], [BrowA])
        pe_, Bpe = nextp()
        for h in range(4):
            pb, Bp = nextp()
            mm(pb[:, 0:G], cst[0:4, C_SEL + h * 128:C_SEL + (h + 1) * 128], Mr, True, True, [Bcst, BrowA], [Bp])
            for c in range(2):
                tt(NMh[:, h * G + c * 128:h * G + (c + 1) * 128], mneg, pb[:, c * 128:(c + 1) * 128], ALU.subtract,
                   [Bcst, Bp], [BNM])
            mm(pe_[:, h * 4:h * 4 + 4], cst[0:4, C_SEL + h * 128:C_SEL + (h + 1) * 128], ext[:, 0:4], True, True,
               [Bcst, BrowA], [Bpe])
        for c in range(2):
            S.op("pe", lambda h, o=pe_[:, 32 + c * 4:36 + c * 4], i=Mr[:, c * 128:(c + 1) * 128]: h.transpose(o, i, ident[0:4, 0:4]),
                 [BrowA, Bcst], [Bpe])
        cp(MT, pe_[:, 32:40], [Bpe], [Bgsm])
        pe3 = pe_[:, 0:16].rearrange("p (h f) -> p h f", f=4)
        for c in range(2):
            cp(MLb[:, c * 4:c * 4 + 4], pe3[:, :, c], [Bpe], [Bgsm])
            cp(MPb[:, c * 4:c * 4 + 4], pe3[:, :, 2 + c], [Bpe], [Bgsm])
        ts(bb, b_, LNSCALE, None, ALU.add, None, [Bgsm], [Bgsm])
        tt(tmp8, b_, MLb, ALU.subtract, [Bgsm], [Bgsm])
        act(u_, tmp8, AF.Exp, [Bgsm], [Bgsm])
        tt(tmp8, MPb, MLb, ALU.subtract, [Bgsm], [Bgsm])
        act(spv, tmp8, AF.Exp, [Bgsm], [Bgsm])
        if own:
            tt(tmp8, MPb, MT, ALU.subtract, [Bgsm], [Bgsm])
            act(isc, tmp8, AF.Exp, [Bgsm], [Bgsm], bias=LNSCALE)
            tt(tmp8b, a_, MT, ALU.add, [Bgsm], [Bgsm])
            act(emt, tmp8b, AF.Exp, [Bgsm], [Bgsm], scale=-1.0)

        def ev_rk(t, c, ps_, Bp):
            cp(rk[c][0][:, t * 256:(t + 1) * 256], ps_, [Bp], [rk[c][1]], eng="act")
        proj_tm(3592, 2, ev_rk)

        def ev_rv(t, c, ps_, Bp):
            cp(rv[c][0][:, t * 256:(t + 1) * 256], ps_, [Bp], [rv[c][1]], eng="act")
        proj_tm(4104, 4, ev_rv)
        if own:
            def ev_rq(t, c, ps_, Bp):
                cp(rq[c][0][:, t * 256:(t + 1) * 256], ps_, [Bp], [rq[c][1]], eng="act")
            proj_tm(3080, 2, ev_rq)

            def ev_rg(t, c, ps_, Bp):
                act(rg[c][0][:, t * 256:(t + 1) * 256], ps_, AF.Silu, [Bp], [rg[c][1]])
            proj_tm(5128, 4, ev_rg)

        H4 = range(4)

        def rotary(src, Bsrc, c):
            s3 = sct[c][0][:, 0:256].rearrange("p (h d) -> p h d", d=64)
            c3 = sct[c][0][:, 256:512].rearrange("p (h d) -> p h d", d=64)
            Bsc = sct[c][1]
            x4 = src.rearrange("p (h t d) -> p h t d", t=2, d=64)
            r4 = rtmp[:, 0:512].rearrange("p (h t d) -> p h t d", t=2, d=64)
            q4 = rtmp[:, 512:1024].rearrange("p (h t d) -> p h t d", t=2, d=64)
            tt(r4[:, :, 0, :], x4[:, :, 0, :], c3, ALU.mult, [Bsrc, Bsc], [Brtmp])
            tt(r4[:, :, 1, :], x4[:, :, 1, :], c3, ALU.mult, [Bsrc, Bsc], [Brtmp])
            tt(q4[:, :, 0, :], x4[:, :, 1, :], s3, ALU.mult, [Bsrc, Bsc], [Brtmp])
            tt(q4[:, :, 1, :], x4[:, :, 0, :], s3, ALU.mult, [Bsrc, Bsc], [Brtmp])
            tt(x4[:, :, 0, :], r4[:, :, 0, :], q4[:, :, 0, :], ALU.subtract, [Brtmp], [Bsrc])
            tt(x4[:, :, 1, :], r4[:, :, 1, :], q4[:, :, 1, :], ALU.add, [Brtmp], [Bsrc])
        for c in range(2):
            rotary(rk[c][0], rk[c][1], c)
            if own:
                rotary(rq[c][0], rq[c][1], c)
        def ml_gen(c):
            kTs = [qkT3[:, 4 + h, c * 128:(c + 1) * 128] for h in H4]
            qTs = [qkT3[:, h, c * 128:(c + 1) * 128] for h in H4]
            vxs = [vml[c][0][:, h * 258:h * 258 + 258] for h in H4]
            cols = [c * 4 + h for h in H4]
            if own:
                pS = [nextp() for h in H4]
                for h in H4:
                    mm(pS[h][0][:, 0:128], kTs[h], qTs[h], True, True, [BqkT], [pS[h][1]])
                yield
                for h in H4:
                    act(DTs[h][0], NMh[:, h * G + c * 128:h * G + (c + 1) * 128], AF.Exp, [BNM, Bgsm], [DTs[h][1]],
                        bias=bb[:, cols[h]:cols[h] + 1])
                yield
                for h in H4:
                    tt(PTs[h][0], pS[h][0][:, 0:128], DTs[h][0], ALU.mult, [pS[h][1], DTs[h][1]], [PTs[h][1]])
                yield
                pI = [nextp() for h in H4]
                for h in H4:
                    mm(pI[h][0][:, 0:258], PTs[h][0], vxs[h], True, True, [PTs[h][1], vml[c][1]], [pI[h][1]])
                yield
                pJ = [nextp() for h in H4]
                for h in H4:
                    mm(pJ[h][0][:, 0:258], qTs[h], CmlR[h][0], True, True, [BqkT, CmlR[h][1]], [pJ[h][1]])
                yield
                for h in H4:
                    act(inss[h][0], pJ[h][0][:, 0:258], AF.Copy, [pJ[h][1], Bgsm], [inss[h][1]], scale=isc[:, cols[h]:cols[h] + 1])
                yield
                for h in H4:
                    tt(tots[h][0], pI[h][0][:, 0:258], inss[h][0], ALU.add, [pI[h][1], inss[h][1]], [tots[h][1]])
                yield
                for h in H4:
                    act(sths[h][0][:, 14:15], tots[h][0][:, 256:257], AF.Abs, [tots[h][1]], [sths[h][1]])
                yield
                for h in H4:
                    ts(sths[h][0][:, 14:15], sths[h][0][:, 14:15], emt[:, cols[h]:cols[h] + 1], None, ALU.max, None,
                       [sths[h][1], Bgsm], [sths[h][1]])
                yield
                for h in H4:
                    S.op("dve", lambda hd, st=sths[h][0]: hd.reciprocal(st[:, 15:16], st[:, 14:15]), [sths[h][1]], [sths[h][1]])
                yield
                for h in H4:
                    ts(hhs[h][0], tots[h][0][:, 0:256], sths[h][0][:, 15:16], None, ALU.mult, None, [tots[h][1], sths[h][1]], [hhs[h][1]])
                yield
                yield from ln_stages([(hhs[h][0], hhs[h][1], sths[h][0], sths[h][1]) for h in H4])
                for h in H4:
                    osl = oml[c][0][:, h * 256:(h + 1) * 256]
                    tt(osl, osl, hhs[h][0], ALU.mult, [oml[c][1], hhs[h][1]], [oml[c][1]])
            pK = [nextp() for h in H4]
            for h in H4:
                S.op("pe", lambda hd, o=pK[h][0][:, 0:128], i=kTs[h].bitcast(F32): hd.transpose(o, i, ident), [BqkT, Bcst], [pK[h][1]])
            yield
            for h in H4:
                act(kws[h][0], pK[h][0][:, 0:128], AF.Copy, [pK[h][1], Bgsm], [kws[h][1]], scale=u_[:, cols[h]:cols[h] + 1])
            yield
            pC = [nextp() for h in H4]
            for h in H4:
                mm(pC[h][0][:, 0:258], kws[h][0], vxs[h], True, True, [kws[h][1], vml[c][1]], [pC[h][1]])
            yield
            for h in H4:
                stt(Cml[h][0], Cml[h][0], spv[:, cols[h]:cols[h] + 1], pC[h][0][:, 0:257], ALU.mult, ALU.add,
                    [Cml[h][1], Bgsm, pC[h][1]], [Cml[h][1]])
            yield
            for h in H4:
                cp(CmlR[h][0][:, 0:257], Cml[h][0], [Cml[h][1]], [CmlR[h][1]], eng="act")

            yield

        def ret_gen(c):
            vvs = [rv[c][0][:, h * 256:(h + 1) * 256] for h in H4]
            if own:
                qrT, BqrT = qrTs[c]
                krT, BkrT = krTs[c]
                pq, Bpq = nextp()
                pk_, Bpk = nextp()
                for h in H4:
                    S.op("pe", lambda hd, o=pq[:, h * 128:(h + 1) * 128], i=rq[c][0][:, h * 128:(h + 1) * 128]: hd.transpose(o, i, ident),
                         [rq[c][1], Bcst], [Bpq])
                yield
                for h in H4:
                    S.op("pe", lambda hd, o=pk_[:, h * 128:(h + 1) * 128], i=rk[c][0][:, h * 128:(h + 1) * 128]: hd.transpose(o, i, ident),
                         [rk[c][1], Bcst], [Bpk])
                yield
                cp(qrT, pq, [Bpq], [BqrT], eng="act")
                cp(krT, pk_, [Bpk], [BkrT], eng="act")
                pS = [nextp() for h in H4]
                for h in H4:
                    mm(pS[h][0][:, 0:128], krT[:, h * 128:(h + 1) * 128], qrT[:, h * 128:(h + 1) * 128], True, True, [BkrT, BqrT], [pS[h][1]])
                yield
                for h in H4:
                    tt(PTs[h][0], pS[h][0][:, 0:128], cst[:, C_DECT + h * 128:C_DECT + (h + 1) * 128], ALU.mult, [pS[h][1], Bcst], [PTs[h][1]])
                yield
                for h in H4:
                    tt(qdTs[h][0], qrT[:, h * 128:(h + 1) * 128].bitcast(F32), cst[:, C_QD + h * 128:C_QD + (h + 1) * 128], ALU.mult,
                       [BqrT, Bcst], [qdTs[h][1]])
                yield
                pI = [nextp() for h in H4]
                for h in H4:
                    mm(pI[h][0][:, 0:256], PTs[h][0], vvs[h], True, False, [PTs[h][1], rv[c][1]], [pI[h][1]])
                    mm(pI[h][0][:, 0:256], qdTs[h][0], CretR[h][0], False, True, [qdTs[h][1], CretR[h][1]], [pI[h][1]])
                yield
                for h in H4:
                    cp(hhs[h][0], pI[h][0][:, 0:256], [pI[h][1]], [hhs[h][1]], eng="act")
                yield
                yield from ln_stages([(hhs[h][0], hhs[h][1], sths[h][0], sths[h][1]) for h in H4])
                for h in H4:
                    gsl = rg[c][0][:, h * 256:(h + 1) * 256]
                    tt(gsl, gsl, hhs[h][0], ALU.mult, [rg[c][1], hhs[h][1]], [rg[c][1]])
            for h in H4:
                ts(kws[h][0], rk[c][0][:, h * 128:(h + 1) * 128], cst[:, C_KDEC + h:C_KDEC + h + 1], None, ALU.mult, None,
                   [rk[c][1], Bcst], [kws[h][1]])
            yield
            pC = [nextp() for h in H4]
            for h in H4:
                mm(pC[h][0][:, 0:256], kws[h][0], vvs[h], True, True, [kws[h][1], rv[c][1]], [pC[h][1]])
            yield
            for h in H4:
                stt(Cret[h][0], Cret[h][0], GAM[h] ** 128, pC[h][0][:, 0:256], ALU.mult, ALU.add, [Cret[h][1], pC[h][1]], [Cret[h][1]])
            yield
            for h in H4:
                cp(CretR[h][0], Cret[h][0], [Cret[h][1]], [CretR[h][1]], eng="act")

            yield

        for c in range(2):
            for g_ in (ml_gen(c), ret_gen(c)):
                for _ in g_:
                    pass

        if not own:
            continue
        for c in range(2):
            for (src, Bsrc, dst3, Bdst, gT) in ((oml[c][0], oml[c][1], hmT3, BhmT, mlgT), (rg[c][0], rg[c][1], hrT3, BhrT, retgT)):
                for hf in range(2):
                    pb, Bp = nextp()
                    for k4 in range(4):
                        kc = hf * 4 + k4
                        S.op("pe", lambda hd, o=pb[:, k4 * 128:(k4 + 1) * 128], i=src[:, kc * 128:(kc + 1) * 128]: hd.transpose(o, i, ident),
                             [Bsrc, Bcst], [Bp])
                    for k4 in range(4):
                        kc = hf * 4 + k4
                        act(dst3[:, kc, c * 128:(c + 1) * 128], pb[:, k4 * 128:(k4 + 1) * 128], AF.Copy, [Bp, Bsm], [Bdst],
                            scale=gT[:, kc:kc + 1])
        for t in range(4):
            wm3, wmb = wload(wbml.rearrange("(k p) c -> p k c", p=128)[:, :, t * 256:(t + 1) * 256], 256)
            pbm = []
            for f in range(2):
                pb, Bp = nextp()
                for kc in range(8):
                    mm(pb[:, 0:G], wm3[:, kc, f * 128:(f + 1) * 128], hmT3[:, kc, :], kc == 0, kc == 7, [wmb, BhmT], [Bp])
                pbm.append((pb, Bp))
            wg3, wgb = win_tile(6152 + t * 256)
            for f in range(2):
                pb, Bp = nextp()
                for kc in range(8):
                    mm(pb[:, 0:G], wg3[:, kc, f * 128:(f + 1) * 128], h1T3[:, kc, :], kc == 0, kc == 7, [wgb, Bh1T], [Bp])
                act(sg1, pb[:, 0:G], AF.Sigmoid, [Bp], [Bsg1])
                tt(yT3[:, t * 2 + f, :], pbm[f][0][:, 0:G], sg1, ALU.mult, [pbm[f][1], Bsg1], [ByT])
            wr3, wrb = wload(wbret.rearrange("(k p) c -> p k c", p=128)[:, :, t * 256:(t + 1) * 256], 256)
            pbr = []
            for f in range(2):
                pb, Bp = nextp()
                for kc in range(8):
                    mm(pb[:, 0:G], wr3[:, kc, f * 128:(f + 1) * 128], hrT3[:, kc, :], kc == 0, kc == 7, [wrb, BhrT], [Bp])
                pbr.append((pb, Bp))
            wg3, wgb = win_tile(7176 + t * 256)
            for f in range(2):
                pb, Bp = nextp()
                for kc in range(8):
                    mm(pb[:, 0:G], wg3[:, kc, f * 128:(f + 1) * 128], h1T3[:, kc, :], kc == 0, kc == 7, [wgb, Bh1T], [Bp])
                act(sg1, pb[:, 0:G], AF.Sigmoid, [Bp], [Bsg1])
                tt(sg2, pbr[f][0][:, 0:G], sg1, ALU.mult, [pbr[f][1], Bsg1], [Bsg2])
                tt(yT3[:, t * 2 + f, :], yT3[:, t * 2 + f, :].bitcast(F32), sg2, ALU.add, [ByT, Bsg2], [ByT])
        for t in range(4):
            wo3, wob = wload(wout.rearrange("(k p) c -> p k c", p=128)[:, :, t * 256:(t + 1) * 256], 256)
            for c in range(2):
                pb, Bp = nextp()
                for kc in range(8):
                    mm(pb[:, 0:256], yT3[:, kc, c * 128:(c + 1) * 128], wo3[:, kc, :], kc == 0, kc == 7, [ByT, wob], [Bp])
                xsl = xg[c][0][:, t * 256:(t + 1) * 256]
                tt(sg2, pb[:, 0:256], gmb[:, t * 256:(t + 1) * 256], ALU.mult, [Bp, Bgmb], [Bsg2])
                tt(xsl, xsl, sg2, ALU.add, [xg[c][1], Bsg2], [xg[c][1]])
        for c in range(2):
            dma("sp", x2s[t0 + c * 128:t0 + (c + 1) * 128, :], xg[c][0], [xg[c][1]], [], x2sems[c])

    S.barrier()
    top[0] = PERS_END
    BLK = 512
    NBK = (TOK * 4) // BLK + NE
    NSLOT = NBK * BLK
    Xs = nc.dram_tensor("Xs", [NSLOT, D], F32, kind="Internal").ap()
    Ys = nc.dram_tensor("Ys", [NSLOT, D], F32, kind="Internal").ap()
    H2 = nc.dram_tensor("H2", [TOK, D], F32, kind="Internal").ap()
    A2bc, BA2bc = T(D, name="A2bc")
    B2bc, BB2bc = T(D, name="B2bc")
    xc, Bxc = T(D, name="xc")
    xs2, Bxs2 = T(D, name="xs2")
    h2tm, Bh2tm = T(D, name="h2tm")
    h2Tc, Bh2Tc = T(D, F32R, "h2Tc")
    h2Tc3 = h2Tc.rearrange("p (k n) -> p k n", n=128)
    st2, Bst2 = T(16, name="st2")
    lgt, Blgt = T(NE, name="lgt")
    t8, Bt8 = T(16, name="t8")
    maskall, Bmask = T(16 * NE, name="maskall")
    Gwall, BGw = T(16 * NE, name="Gwall")
    tris, Btris = T(128, name="tristrict")
    cntb, Bcnt = T(NE, name="cnt")
    nbi, Bnbi = T(NE, I32, "nbi")
    nbf, Bnbf = T(NE, name="nbf")
    bend, Bbend = T(NE, name="bend")
    sbase, Bsbase = T(NE, name="sbase")
    ones32, Bones32 = T(NE, name="ones32")
    key, Bkey = T(NE, name="key")
    eqt, Beqt = T(NE, name="eqt")
    s4, Bs4 = T(16, name="s4")
    sidxf, Bsidxf = T(64, name="sidxf")
    w4, Bw4 = T(64, name="w4")
    ebrow, Beb = T(NBK, name="ebrow")
    widxf, Bwidxf = T(NBK * 8, name="widxf")
    didxf, Bdidxf = T(NBK * 4, name="didxf")
    bidxf, Bbidxf = T(NBK * 2, name="bidxf")
    Xtm, BXtm = T(4 * D, name="Xtm")
    Xtm3 = Xtm.rearrange("p (j c) -> p j c", c=D)
    XT, BXT = T(8 * BLK, F32R, "XT")
    XT3 = XT.rearrange("p (k n) -> p k n", n=BLK)
    actT, BactT = T(8 * BLK, F32R, "actT")
    actT3 = actT.rearrange("p (k n) -> p k n", n=BLK)
    NU = 3
    wgt = [T(8 * 256, F32R, "wgu%d" % i) for i in range(NU)]
    wgsem = [S.dmasem("wgu%d" % i) for i in range(NU)]
    wq = [T(2 * 1024, F32R, "wd%d" % q) for q in range(4)]
    wq3 = [wq[q][0].rearrange("p (k n) -> p k n", n=1024) for q in range(4)]
    wqsem = [S.dmasem("wd%d" % q) for q in range(4)]
    bgt = [T(16, name="bgt%d" % i) for i in range(2)]
    bgsem = [S.dmasem("bgt%d" % i) for i in range(2)]
    bdb = [T(D, name="bdb%d" % i) for i in range(2)]
    bdsem = [S.dmasem("bdb%d" % i) for i in range(2)]
    gm_ = [T(512, name="gm%d" % i) for i in range(2)]
    sg_ = [T(512, name="sg%d" % i) for i in range(2)]
    lm_ = [T(512, name="lm%d" % i) for i in range(2)]
    dsb = [T(D, name="dsb%d" % i) for i in range(2)]
    acc, Bacc = T(D, name="acc")
    xcsem = S.dmasem("xc")
    h2sem = S.dmasem("h2st")
    h2lsem = S.dmasem("h2ld")
    scs = [S.dmasem("scat%d" % k) for k in range(4)]
    xtsem = S.dmasem("xtm")
    yss = [S.dmasem("ysst%d" % i) for i in range(2)]
    ygsem = S.dmasem("ygat")
    osem = S.dmasem("ost")
    uctr = [0]
    sctr = [0]
    wguh = wgu.rearrange("e (u q) c -> (e u q) c", u=8)
    wdh = wd.rearrange("e (q r) c -> (e q r) c", q=4)

    def ind(ap_):
        return bass.IndirectOffsetOnAxis(ap=ap_, axis=0)

    _bregs = {}

    def bnd(h, v):
        if v not in _bregs:
            _bregs[v] = h.to_reg(v)
        return _bregs[v]

    NIT = 40
    itl = [T(1, I32, "idx%d" % i) for i in range(NIT)]
    ictr = [0]

    def idx_tile(colap, Bsrc):
        i = ictr[0] % NIT
        ictr[0] += 1
        ap_, b_ = itl[i]
        cp(ap_, colap, [Bsrc], [b_])
        return ap_, b_

    def block_idx(b):
        d = {}
        d["bg"] = idx_tile(bidxf[:, b:b + 1], Bbidxf)
        d["bd"] = idx_tile(bidxf[:, NBK + b:NBK + b + 1], Bbidxf)
        for u in range(8):
            d["w%d" % u] = idx_tile(widxf[:, b * 8 + u:b * 8 + u + 1], Bwidxf)
        for q in range(4):
            d["d%d" % q] = idx_tile(didxf[:, b * 4 + q:b * 4 + q + 1], Bdidxf)
        return d

    def bcast_cols(dst, Bdst, colap, Bcol):
        for hf in range(2):
            pb, Bp = nextp()
            for k4 in range(4):
                kc = hf * 4 + k4
                ts(dgt, ident, colap[:, kc:kc + 1], None, ALU.mult, None, [Bcst, Bcol], [Bdgt])
                mm(pb[:, k4 * 128:(k4 + 1) * 128], ones[:, 0:128], dgt, True, True, [Bcst, Bdgt], [Bp])
            cp(dst[:, hf * 512:(hf + 1) * 512], pb, [Bp], [Bdst])
    dgt, Bdgt = T(128, name="dgt2")
    bcast_cols(A2bc, BA2bc, A2, BAB)
    bcast_cols(B2bc, BB2bc, B2, BAB)
    tt(tris, tri, ident, ALU.subtract, [Bcst], [Btris])
    S.op("dve", lambda h: h.memset(ones32, 1.0), [], [Bones32])
    wrt3 = wrt.rearrange("p (k n) -> p k n", n=NE)

    for c in range(16):
        dma("sp", xc, x2s[c * 128:(c + 1) * 128, :], [], [Bxc], xcsem)
        act(xs2, xc, AF.Square, [Bxc], [Bxs2, Bst2], accum=st2[:, 0:1])
        act(st2[:, 1:2], st2[:, 0:1], AF.Sqrt, [Bst2, Bsm], [Bst2], bias=epsc, scale=1.0 / D)
        S.op("dve", lambda h: h.reciprocal(st2[:, 2:3], st2[:, 1:2]), [Bst2], [Bst2])
        ts(xs2, xc, st2[:, 2:3], None, ALU.mult, None, [Bxc, Bst2], [Bxs2])
        tt(h2tm, xs2, A2bc, ALU.mult, [Bxs2, BA2bc], [Bh2tm])
        tt(h2tm, h2tm, B2bc, ALU.add, [Bh2tm, BB2bc], [Bh2tm])
        dma("sp", H2[c * 128:(c + 1) * 128, :], h2tm, [Bh2tm], [], h2sem)
        for hf in range(2):
            pb, Bp = nextp()
            for k4 in range(4):
                kc = hf * 4 + k4
                S.op("pe", lambda h, o=pb[:, k4 * 128:(k4 + 1) * 128], i=h2tm[:, kc * 128:(kc + 1) * 128]: h.transpose(o, i, ident),
                     [Bh2tm, Bcst], [Bp])
            cp(h2Tc[:, hf * 512:(hf + 1) * 512], pb, [Bp], [Bh2Tc], eng="act")
        pl, Bpl = nextp()
        for kc in range(8):
            mm(pl[:, 0:NE], h2Tc3[:, kc, :], wrt3[:, kc, :], kc == 0, kc == 7, [Bh2Tc, Bwrt], [Bpl])
        tt(lgt, pl[:, 0:NE], brb, ALU.add, [Bpl, Bbrb], [Blgt])
        S.op("dve", lambda h: h.max(t8[:, 0:8], lgt), [Blgt], [Bt8])
        ts(maskall[:, c * NE:(c + 1) * NE], lgt, t8[:, 3:4], None, ALU.is_ge, None, [Blgt, Bt8], [Bmask])
        ts(t8[:, 8:9], t8[:, 0:1], -1.0, None, ALU.mult, None, [Bt8], [Bt8])
        act(lgt, lgt, AF.Exp, [Blgt, Bt8], [Blgt], bias=t8[:, 8:9])
        tt(lgt, lgt, maskall[:, c * NE:(c + 1) * NE], ALU.mult, [Blgt, Bmask], [Blgt])
        S.op("dve", lambda h: h.reduce_sum(t8[:, 9:10], lgt, mybir.AxisListType.X), [Blgt], [Bt8])
        S.op("dve", lambda h: h.reciprocal(t8[:, 10:11], t8[:, 9:10]), [Bt8], [Bt8])
        ts(Gwall[:, c * NE:(c + 1) * NE], lgt, t8[:, 10:11], None, ALU.mult, None, [Blgt, Bt8], [BGw])

    pcn, Bpcn = nextp()
    for c in range(16):
        mm(pcn[:, 0:NE], ones[:, 0:128], maskall[:, c * NE:(c + 1) * NE], c == 0, c == 15, [Bcst, Bmask], [Bpcn])
    cp(cntb, pcn[:, 0:NE], [Bpcn], [Bcnt])
    ts(nbi, cntb, 1.0 / BLK, (BLK - 1.0) / BLK - 0.49951171875, ALU.mult, ALU.add, [Bcnt], [Bnbi])
    cp(nbf, nbi, [Bnbi], [Bnbf])
    S.op("dve", lambda h: h.tensor_tensor_scan(bend, ones32, nbf, 0.0, ALU.mult, ALU.add), [Bones32, Bnbf], [Bbend])
    tt(sbase, bend, nbf, ALU.subtract, [Bbend, Bnbf], [Bsbase])
    ts(sbase, sbase, float(BLK), None, ALU.mult, None, [Bsbase], [Bsbase])
    iob = cst[:, C_IOB:C_IOB + NBK]
    S.op("dve", lambda h: h.memset(ebrow, 0.0), [], [Beb])
    for e in range(NE):
        stt(ebrow, iob, bend[:, e:e + 1], ebrow, ALU.is_ge, ALU.add, [Bcst, Bbend, Beb], [Beb])
    ts(ebrow, ebrow, float(NE - 1), None, ALU.min, None, [Beb], [Beb])
    widxf3 = widxf.rearrange("p (b u) -> p b u", u=8)
    for u in range(8):
        ts(widxf3[:, :, u], ebrow, 1024.0, cst[:, C_CU + u:C_CU + u + 1], ALU.mult, ALU.add, [Beb, Bcst], [Bwidxf])
    didxf3 = didxf.rearrange("p (b q) -> p b q", q=4)
    for q in range(4):
        ts(didxf3[:, :, q], ebrow, 512.0, cst[:, C_CU + q:C_CU + q + 1], ALU.mult, ALU.add, [Beb, Bcst], [Bdidxf])
    ts(bidxf[:, 0:NBK], ebrow, 128.0, cst[:, C_CU:C_CU + 1], ALU.mult, ALU.add, [Beb, Bcst], [Bbidxf])
    cp(bidxf[:, NBK:2 * NBK], ebrow, [Beb], [Bbidxf])

    prk, Bprk = nextp()
    for c in range(16):
        for c2 in range(c):
            mm(prk[:, c * NE:(c + 1) * NE], ones[:, 0:128], maskall[:, c2 * NE:(c2 + 1) * NE], c2 == 0, False, [Bcst, Bmask], [Bprk])
        mm(prk[:, c * NE:(c + 1) * NE], tris, maskall[:, c * NE:(c + 1) * NE], c == 0, True, [Btris, Bmask], [Bprk])
    for c in range(16):
        mk = maskall[:, c * NE:(c + 1) * NE]
        tt(key, prk[:, c * NE:(c + 1) * NE], sbase, ALU.add, [Bprk, Bsbase], [Bkey])
        ts(key, key, 1.0, None, ALU.add, None, [Bkey], [Bkey])
        tt(key, key, mk, ALU.mult, [Bkey, Bmask], [Bkey])
        S.op("dve", lambda h: h.max(s4[:, 0:8], key), [Bkey], [Bs4])
        ts(sidxf[:, c * 4:(c + 1) * 4], s4[:, 0:4], -1.0, None, ALU.add, None, [Bs4], [Bsidxf])
        for k in range(4):
            ts(eqt, key, s4[:, k:k + 1], None, ALU.is_equal, None, [Bkey, Bs4], [Beqt])
            tt(eqt, eqt, Gwall[:, c * NE:(c + 1) * NE], ALU.mult, [Beqt, BGw], [Beqt])
            S.op("dve", lambda h, o=w4[:, c * 4 + k:c * 4 + k + 1]: h.reduce_sum(o, eqt, mybir.AxisListType.X), [Beqt], [Bw4])
    lasth2 = S.last[("dma", id(h2sem))]
    for c in range(16):
        S.op("sp", lambda h, c=c: h.dma_start(out=h2tm, in_=H2[c * 128:(c + 1) * 128, :]), [], [Bh2tm], dma=h2lsem, extra=[lasth2])
        for k in range(4):
            ia, Bia = idx_tile(sidxf[:, c * 4 + k:c * 4 + k + 1], Bsidxf)
            S.op("pool", lambda h, ia=ia: h.indirect_dma_start(
                out=Xs[:, :], out_offset=ind(ia), in_=h2tm, in_offset=None, bounds_check=bnd(h, NSLOT - 1), oob_is_err=False),
                [Bh2tm, Bia], [], dma=scs[k])

    lastsc = [S.last[("dma", id(s_))] for s_ in scs]
    nxt_ix = block_idx(0)
    for b in range(NBK):
        bix = nxt_ix
        if b + 1 < NBK:
            nxt_ix = block_idx(b + 1)
        if b == 0:
            S.op("sp", lambda h, b=b: h.dma_start(out=Xtm3, in_=Xs[b * BLK:(b + 1) * BLK, :].rearrange("(j p) c -> p j c", p=128)),
                 [], [BXtm], dma=xtsem, extra=lastsc)
        for j in range(4):
            for hf in range(2):
                pb, Bp = nextp()
                for k4 in range(4):
                    kc = hf * 4 + k4
                    S.op("pe", lambda h, o=pb[:, k4 * 128:(k4 + 1) * 128], i=Xtm3[:, j, kc * 128:(kc + 1) * 128]: h.transpose(o, i, ident),
                         [BXtm, Bcst], [Bp])
                cp(XT3[:, hf * 4:hf * 4 + 4, j * 128:(j + 1) * 128], pb.rearrange("p (k n) -> p k n", n=128), [Bp], [BXT],
                   eng=("act" if hf == 0 else "dve"))
        if b + 1 < NBK:
            S.op("sp", lambda h, b=b + 1: h.dma_start(out=Xtm3, in_=Xs[b * BLK:(b + 1) * BLK, :].rearrange("(j p) c -> p j c", p=128)),
                 [], [BXtm], dma=xtsem, extra=lastsc)
        bi = b % 2
        ia, Bia = bix["bg"]
        S.op("pool", lambda h, o=bgt[bi][0], ia=ia: h.indirect_dma_start(
            out=o, out_offset=None, in_=bguT_d[:, :], in_offset=ind(ia), bounds_check=bnd(h, NE * 128 - 1), oob_is_err=False),
            [Bia], [bgt[bi][1]], dma=bgsem[bi])
        ia, Bia = bix["bd"]
        S.op("pool", lambda h, o=bdb[bi][0], ia=ia: h.indirect_dma_start(
            out=o, out_offset=None, in_=bd_d[:, :], in_offset=ind(ia), bounds_check=bnd(h, NE - 1), oob_is_err=False),
            [Bia], [bdb[bi][1]], dma=bdsem[bi])
        for u in range(8):
            i = uctr[0] % NU
            uctr[0] += 1
            wv, wb = wgt[i]
            w3 = wv.rearrange("p (k n) -> p k n", n=256)
            ia, Bia = bix["w%d" % u]
            S.op("pool", lambda h, o=wv, ia=ia: h.indirect_dma_start(
                out=o, out_offset=None, in_=wguh[:, :], in_offset=ind(ia), bounds_check=bnd(h, NE * 1024 - 1), oob_is_err=False),
                [Bia], [wb], dma=wgsem[i])
            pg, Bpg = nextp()
            for kc in range(8):
                mm(pg, w3[:, kc, 0:128], XT3[:, kc, :], kc == 0, kc == 7, [wb, BXT], [Bpg])
            plin, Bplin = nextp()
            for kc in range(8):
                mm(plin, w3[:, kc, 128:256], XT3[:, kc, :], kc == 0, kc == 7, [wb, BXT], [Bplin])
            si = sctr[0] % 2
            sctr[0] += 1
            gmv, Bgm = gm_[si]; sgv, Bsg = sg_[si]; lmv, Blm = lm_[si]
            bg = bgt[bi][0]
            ts(gmv, pg, bg[:, u * 2:u * 2 + 1], 7.0, ALU.add, ALU.min, [Bpg, bgt[bi][1]], [Bgm])
            act(sgv, gmv, AF.Sigmoid, [Bgm], [Bsg], scale=1.702)
            act(lmv, plin, AF.Identity, [Bplin, bgt[bi][1]], [Blm], bias=bg[:, u * 2 + 1:u * 2 + 2])
            ts(lmv, lmv, 7.0, -7.0, ALU.min, ALU.max, [Blm], [Blm])
            tt(gmv, gmv, sgv, ALU.mult, [Bgm, Bsg], [Bgm])
            stt(actT3[:, u, :], lmv, 1.0, gmv, ALU.add, ALU.mult, [Blm, Bgm], [BactT])
        for q in range(4):
            ia, Bia = bix["d%d" % q]
            S.op("pool", lambda h, o=wq[q][0], ia=ia: h.indirect_dma_start(
                out=o, out_offset=None, in_=wdh[:, :], in_offset=ind(ia), bounds_check=bnd(h, NE * 512 - 1), oob_is_err=False),
                [Bia], [wq[q][1]], dma=wqsem[q])
        for j in range(4):
            dv, Bd = dsb[j % 2]
            for nt in range(2):
                pd_, Bpd = nextp()
                for ft in range(8):
                    mm(pd_, actT3[:, ft, j * 128:(j + 1) * 128], wq3[ft // 2][:, ft % 2, nt * 512:(nt + 1) * 512], ft == 0, ft == 7,
                       [BactT, wq[ft // 2][1]], [Bpd])
                tt(dv[:, nt * 512:(nt + 1) * 512], pd_, bdb[bi][0][:, nt * 512:(nt + 1) * 512], ALU.add, [Bpd, bdb[bi][1]], [Bd])
            dma("sp", Ys[b * BLK + j * 128:b * BLK + (j + 1) * 128, :], dv, [Bd], [], yss[j % 2])

    lastys = [S.last[("dma", id(y_))] for y_ in yss]
    xtm_prev = [BXtm.w] + list(BXtm.r.values())
    BY = [Buf("Yk%d" % k) for k in range(4)]
    ygs = [S.dmasem("ygat%d" % k) for k in range(4)]
    for c in range(16):
        dma("sp", xc, x2s[c * 128:(c + 1) * 128, :], [], [Bxc], xcsem)
        for k in range(4):
            ia, Bia = idx_tile(sidxf[:, c * 4 + k:c * 4 + k + 1], Bsidxf)
            S.op("pool", lambda h, o=Xtm3[:, k, :], ia=ia: h.indirect_dma_start(
                out=o, out_offset=None, in_=Ys[:, :], in_offset=ind(ia), bounds_check=bnd(h, NSLOT - 1), oob_is_err=False),
                [Bia], [BY[k]], dma=ygs[k], extra=lastys + [d_ for d_ in xtm_prev if d_ is not None])
        ts(acc, Xtm3[:, 0, :], w4[:, c * 4:c * 4 + 1], None, ALU.mult, None, [BY[0], Bw4], [Bacc])
        for k in range(1, 4):
            stt(acc, Xtm3[:, k, :], w4[:, c * 4 + k:c * 4 + k + 1], acc, ALU.mult, ALU.add, [BY[k], Bw4, Bacc], [Bacc])
        tt(acc, acc, gfb, ALU.mult, [Bacc, Bgfb], [Bacc])
        tt(xc, xc, acc, ALU.add, [Bxc, Bacc], [Bxc])
        act(xs2, xc, AF.Square, [Bxc], [Bxs2, Bst2], accum=st2[:, 0:1])
        act(st2[:, 1:2], st2[:, 0:1], AF.Sqrt, [Bst2, Bsm], [Bst2], bias=epsc, scale=1.0 / D)
        S.op("dve", lambda h: h.reciprocal(st2[:, 2:3], st2[:, 1:2]), [Bst2], [Bst2])
        stt(xs2, xc, st2[:, 2:3], gfin, ALU.mult, ALU.mult, [Bxc, Bst2, Bgfin], [Bxs2])
        dma("sp", out[c * 128:(c + 1) * 128, :], xs2, [Bxs2], [], osem)

    print("arena cols: pers", PERS_END, "mix", MIX_END, "moe", top[0], "ops", len(S.ops))
    S.emit(nc, es, final_waits=[osem, h2sem] + scs + x2sems + yss)
    es.close()
    return nc


def _consts():
    c = np.zeros((128, NC), np.float64)
    idx = np.arange(128)
    c[:, C_ID:C_ID + 128] = np.eye(128)
    s = idx[:, None]; j = idx[None, :]
    c[:, C_TRI:C_TRI + 128] = (s <= j)
    c[:, C_MNEG:C_MNEG + 128] = np.where(s <= j, 0.0, -30000.0)
    for h in range(4):
        c[h, C_SEL + h * 128:C_SEL + (h + 1) * 128] = 1.0
        lg = math.log(1.0 - 2.0 ** (-5.0 - h))
        c[:, C_DECT + h * 128:C_DECT + (h + 1) * 128] = np.where(j >= s, np.exp(lg * np.maximum(j - s, 0)), 0.0) * 128.0 ** -0.5
        c[:, C_QD + h * 128:C_QD + (h + 1) * 128] = np.exp(lg * (j + 1.0)) * np.ones((128, 1))
        c[:, C_KDEC + h] = np.exp(lg * (127.0 - idx)) * 128.0 ** -0.5
    c[:, C_ONES:C_ONES + 512] = 1.0
    inv = (10000.0 ** (-np.arange(64, dtype=np.float32) / np.float32(64))).astype(np.float32)
    c[:, C_INVF:C_INVF + 256] = np.tile(inv, 4)[None, :]
    c[:, C_IOB:C_IOB + 64] = np.arange(64)[None, :]
    c[:, C_CU:C_CU + 8] = np.arange(8)[None, :] * 128 + idx[:, None]
    return c.astype(np.float32)


_NC_CACHE = {}


def _colT(v, n):
    return np.ascontiguousarray(np.asarray(v, np.float32).reshape(n, 128).T)


def kernel(x, c, positions, w_ada, b_ada, norm_mix_g, w_in, conv_w, conv_b, b_if, ml_norm_g, ret_norm_g,
           w_branch_ml, w_branch_ret, w_out, norm_ffn_g, w_router, b_router, w_gate_up, b_gate_up, w_down, b_down,
           norm_final_g, _debug=False):
    f = lambda a: np.ascontiguousarray(np.asarray(a, np.float32))
    x = f(x); c = f(c)
    positions = np.asarray(positions).astype(np.int32)
    key = bool(_debug)
    if key not in _NC_CACHE:
        _NC_CACHE[key] = build_nc(debug=key)
    nc = _NC_CACHE[key]
    wgu_p = f(w_gate_up)[0].reshape(NE, 8, 128, 8, 128, 2)
    wgu_p = np.ascontiguousarray(wgu_p.transpose(0, 3, 2, 1, 5, 4)).reshape(NE, D, 2 * D)
    wd_p = f(w_down)[0].reshape(NE, 4, 2, 128, D)
    wd_p = np.ascontiguousarray(wd_p.transpose(0, 1, 3, 2, 4)).reshape(NE, D // 2, 2 * D)
    bgu = f(b_gate_up)[0].reshape(NE, D, 2)
    bguT = np.stack([bgu[..., 0].reshape(NE, 8, 128), bgu[..., 1].reshape(NE, 8, 128)], axis=2)
    bguT = np.ascontiguousarray(bguT.transpose(0, 3, 1, 2).reshape(NE * 128, 16))
    shared = {
        "cst": _consts(),
        "w_ada": f(w_ada)[0], "b_adaT": _colT(f(b_ada)[0], 48),
        "gmixT": _colT(f(norm_mix_g)[0], 8), "gffnT": _colT(f(norm_ffn_g)[0], 8), "gfin": f(norm_final_g).reshape(1, D),
        "w_in": f(w_in)[0],
        "convwT": np.ascontiguousarray(f(conv_w)[0].T.reshape(8, 128, 4).transpose(1, 0, 2).reshape(128, 32)),
        "convbT": _colT(f(conv_b)[0], 8), "bif": f(b_if)[0].reshape(1, 8),
        "mlgT": _colT(f(ml_norm_g)[0], 8), "retgT": _colT(f(ret_norm_g)[0], 8),
        "wbml": f(w_branch_ml)[0], "wbret": f(w_branch_ret)[0], "wout": f(w_out)[0],
        "wr": f(w_router)[0], "br": f(b_router)[0].reshape(1, NE),
        "wgu": wgu_p, "bguT": bguT, "wd": wd_p, "bd": f(b_down)[0],
    }
    in_maps = []
    for i in range(8):
        b, half = i // 2, i % 2
        m = dict(shared)
        m["xo"] = np.ascontiguousarray(x[b, half * TOK:(half + 1) * TOK])
        m["xp"] = np.ascontiguousarray(x[b, 0:TOK])
        m["poso"] = np.ascontiguousarray(positions[b, half * TOK:(half + 1) * TOK].reshape(16, 128).T)
        m["posp"] = np.ascontiguousarray(positions[b, 0:TOK].reshape(16, 128).T)
        m["flag"] = np.full((128, 1), float(half), np.float32)
        m["cT"] = _colT(c[b], 8)
        in_maps.append(m)
    res = run_bass_kernel_spmd(nc, in_maps, core_ids=list(range(8)))
    outp = np.zeros((4, SEQ, D), np.float32)
    for i in range(8):
        b, half = i // 2, i % 2
        outp[b, half * TOK:(half + 1) * TOK] = res.results[i]["out"]
    if _debug:
        dbg = np.zeros((4, SEQ, D), np.float32)
        for i in range(8):
            b, half = i // 2, i % 2
            dbg[b, half * TOK:(half + 1) * TOK] = res.results[i]["x2s"]
        return outp, dbg
    return outp
```
